# Optimizing a Trainium2 kernel written in Bass

```python
import math
import jax, jax.numpy as jnp
from jax import lax
import numpy as np

D_MODEL = 2048
BATCH = 4
SEQ = 4096
DEPTH = 4

N_EVEN = (DEPTH + 1) // 2
N_ODD = DEPTH // 2

A_GROUPS = 8
A_DIM = 128
A_CHUNK = 128
A_WIDTH = A_GROUPS * A_DIM

B_HEADS = 4
B_DK = 128
B_DV = 256
B_RANK = 16
B_TAU = 16.0
B_CHUNK = 64
B_WIDTH = B_HEADS * B_DV

AB_OUT = A_WIDTH + B_WIDTH
AB_SPLITS = (A_WIDTH, A_WIDTH, B_HEADS * B_DK, B_HEADS * B_DK, B_WIDTH, B_WIDTH, B_RANK)
AB_IN = 2 * A_WIDTH + 2 * B_HEADS * B_DK + 2 * B_WIDTH + B_RANK

C_HEADS = 16
C_KV_HEADS = 4
C_HEAD_DIM = 128
C_IDX_HEADS = 16
C_IDX_DIM = 64
C_TOPK_MAX = 256
C_QBLOCK = 128
C_WIDTH = C_HEADS * C_HEAD_DIM
C_SPLITS = (C_WIDTH, C_KV_HEADS * C_HEAD_DIM, C_KV_HEADS * C_HEAD_DIM, C_IDX_HEADS * C_IDX_DIM, C_IDX_DIM, C_IDX_HEADS)
C_IN = C_WIDTH + 2 * C_KV_HEADS * C_HEAD_DIM + C_IDX_HEADS * C_IDX_DIM + C_IDX_DIM + C_IDX_HEADS

REL_BUCKETS = 32
REL_MAX_DIST = 128

D_FF = 4 * D_MODEL
EPS = 1e-6
F32 = jnp.float32

kernel_name = 'hybrid_sgu_gla_dsa_trunk'


def _offsets(sizes):
    out, acc = [], 0
    for s in sizes[:-1]:
        acc += s
        out.append(acc)
    return out


def _rmsnorm(x, g):
    xf = x.astype(F32)
    y = xf * lax.rsqrt(jnp.mean(xf * xf, axis=-1, keepdims=True) + EPS)
    return (y * g.astype(F32)).astype(x.dtype)


def _layernorm(x, g):
    xf = x.astype(F32)
    mu = jnp.mean(xf, axis=-1, keepdims=True)
    xc = xf - mu
    var = jnp.mean(xc * xc, axis=-1, keepdims=True)
    return (xc * lax.rsqrt(var + EPS) * g.astype(F32)).astype(x.dtype)


def _t5_bucket(dist):
    max_exact = REL_BUCKETS // 2
    d = jnp.maximum(dist, 1).astype(F32)
    large = max_exact + (jnp.log(d / max_exact) / math.log(REL_MAX_DIST / max_exact)
                         * (REL_BUCKETS - max_exact)).astype(jnp.int32)
    large = jnp.minimum(large, REL_BUCKETS - 1)
    return jnp.where(dist < max_exact, dist, large)


def _spatial_gating(u, v, ln_g, w_s, b_s):
    bsz, L = u.shape[:2]
    nc = L // A_CHUNK
    v = _layernorm(v, ln_g)
    v = v.reshape(bsz, nc, A_CHUNK, A_GROUPS, A_DIM)
    causal = jnp.tril(jnp.ones((A_CHUNK, A_CHUNK), dtype=bool))
    w = jnp.where(causal[None], w_s, jnp.zeros_like(w_s))
    z = jnp.einsum('gts,bnsgc->bntgc', w.astype(v.dtype), v) + b_s.T[:, :, None].astype(v.dtype)
    return u * z.reshape(bsz, L, A_GROUPS, A_DIM)


def _gla(q, k, v, log_a):
    bsz, L = q.shape[:2]
    nc = L // B_CHUNK

    def to_chunks(t):
        return t.astype(F32).reshape(bsz, nc, B_CHUNK, B_HEADS, t.shape[-1]).transpose(1, 0, 3, 2, 4)

    qc = to_chunks(q) * (B_DK ** -0.5)
    kc = to_chunks(k)
    vc = to_chunks(v)
    cum = jnp.cumsum(to_chunks(log_a), axis=3)
    last = cum[:, :, :, -1:, :]
    ref = 0.5 * last
    qe = qc * jnp.exp(cum - ref)
    ke = kc * jnp.exp(ref - cum)
    causal = jnp.tril(jnp.ones((B_CHUNK, B_CHUNK), dtype=bool))
    scores = jnp.where(causal, jnp.einsum('nbhid,nbhjd->nbhij', qe, ke), 0.0)
    o_intra = jnp.einsum('nbhij,nbhjv->nbhiv', scores, vc)
    q_inter = qc * jnp.exp(cum)
    k_state = kc * jnp.exp(last - cum)
    decay = jnp.exp(last[:, :, :, 0, :])

    def step(S, inp):
        qi, ki, vi, di = inp
        o = jnp.einsum('bhid,bhdv->bhiv', qi, S)
        S = di[..., None] * S + jnp.einsum('bhjd,bhjv->bhdv', ki, vi)
        return S, o

    S0 = jnp.zeros((bsz, B_HEADS, B_DK, B_DV), F32)
    _, o_inter = lax.scan(step, S0, (q_inter, k_state, vc, decay))
    o = o_intra + o_inter
    return o.transpose(1, 0, 3, 2, 4).reshape(bsz, L, B_HEADS, B_DV)


def _dsa_attention(q, k, v, iq, ik, iw, rel_bias):
    bsz, L = q.shape[:2]
    k_sel = min(C_TOPK_MAX, L // 4)
    nb = L // C_QBLOCK
    grp = C_HEADS // C_KV_HEADS
    key_pos = jnp.arange(L, dtype=jnp.int32)
    ik32 = ik.astype(F32)

    def blocks(t):
        return t.reshape(bsz, nb, C_QBLOCK, *t.shape[2:]).swapaxes(0, 1)

    def one_block(inp):
        qb, iqb, iwb, start = inp
        qpos = start + jnp.arange(C_QBLOCK, dtype=jnp.int32)
        s = jnp.einsum('bthd,bsd->bths', iqb.astype(F32), ik32) * (C_IDX_DIM ** -0.5)
        score = jnp.einsum('bths,bth->bts', jax.nn.relu(s), iwb.astype(F32)) * (C_IDX_HEADS ** -0.5)
        causal = key_pos[None, :] <= qpos[:, None]
        score = jnp.where(causal[None], score, -jnp.inf)
        _, idx = lax.top_k(score, k_sel)
        valid = idx <= qpos[None, :, None]
        kg = jax.vmap(lambda kb, ib: kb[ib])(k, idx)
        vg = jax.vmap(lambda vb, ib: vb[ib])(v, idx)
        qg = qb.reshape(bsz, C_QBLOCK, C_KV_HEADS, grp, C_HEAD_DIM)
        logits = jnp.einsum('btkgd,btskd->btkgs', qg, kg).astype(F32) * (C_HEAD_DIM ** -0.5)
        dist = jnp.maximum(qpos[None, :, None] - idx, 0)
        bias = rel_bias.astype(F32)[_t5_bucket(dist)]
        bias = bias.reshape(bsz, C_QBLOCK, k_sel, C_KV_HEADS, grp).transpose(0, 1, 3, 4, 2)
        logits = jnp.where(valid[:, :, None, None, :], logits + bias, -jnp.inf)
        p = jax.nn.softmax(logits, axis=-1)
        o = jnp.einsum('btkgs,btskd->btkgd', p.astype(vg.dtype), vg)
        return o.reshape(bsz, C_QBLOCK, C_WIDTH)

    starts = jnp.arange(nb, dtype=jnp.int32) * C_QBLOCK
    out = lax.map(one_block, (blocks(q), blocks(iq), blocks(iw), starts))
    return out.swapaxes(0, 1).reshape(bsz, L, C_WIDTH)


def _even_mixer(h, w_in, v_ln_g, w_s, b_s, gate_w2, gate_b, out_norm_g, w_out):
    bsz, L, _ = h.shape
    p = h @ w_in
    a_u, a_v, q, k, v, r, g_lr = jnp.split(p, _offsets(AB_SPLITS), axis=-1)
    a_u = jax.nn.gelu(a_u, approximate=False).reshape(bsz, L, A_GROUPS, A_DIM)
    a_v = jax.nn.gelu(a_v, approximate=False).reshape(bsz, L, A_GROUPS, A_DIM)
    a_out = _spatial_gating(a_u, a_v, v_ln_g, w_s, b_s).reshape(bsz, L, A_WIDTH)
    log_a = jax.nn.log_sigmoid((g_lr @ gate_w2 + gate_b).astype(F32)) / B_TAU
    o = _gla(q.reshape(bsz, L, B_HEADS, B_DK), k.reshape(bsz, L, B_HEADS, B_DK),
             v.reshape(bsz, L, B_HEADS, B_DV), log_a.reshape(bsz, L, B_HEADS, B_DK))
    o = _rmsnorm(o, out_norm_g) * jax.nn.silu(r.astype(F32)).reshape(bsz, L, B_HEADS, B_DV)
    b_out = o.reshape(bsz, L, B_WIDTH).astype(h.dtype)
    return jnp.concatenate([a_out, b_out], axis=-1) @ w_out


def _odd_mixer(h, w_in, w_out, rel_bias):
    bsz, L, _ = h.shape
    p = h @ w_in
    q, k, v, iq, ik, iw = jnp.split(p, _offsets(C_SPLITS), axis=-1)
    o = _dsa_attention(q.reshape(bsz, L, C_HEADS, C_HEAD_DIM),
                       k.reshape(bsz, L, C_KV_HEADS, C_HEAD_DIM),
                       v.reshape(bsz, L, C_KV_HEADS, C_HEAD_DIM),
                       iq.reshape(bsz, L, C_IDX_HEADS, C_IDX_DIM), ik, iw, rel_bias)
    return o @ w_out


def _sqrelu_mlp(h, w1, w2):
    return jnp.square(jax.nn.relu(h @ w1)) @ w2


def setup_inputs(seed: int = 0) -> dict:
    key = jax.random.key(seed)
    ks = jax.random.split(key, 17)

    def nrm(k, shape, scale):
        return jax.random.normal(k, shape, F32) * scale

    row_scale = (jnp.arange(A_CHUNK, dtype=F32) + 1.0) ** -0.5
    return {
        'x': nrm(ks[0], (BATCH, SEQ, D_MODEL), 1.0),
        'norm_mix_g': 1.0 + nrm(ks[1], (DEPTH, D_MODEL), 0.02),
        'norm_ffn_g': 1.0 + nrm(ks[2], (DEPTH, D_MODEL), 0.02),
        'final_norm_g': 1.0 + nrm(ks[3], (D_MODEL,), 0.02),
        'ab_w_in': nrm(ks[4], (N_EVEN, D_MODEL, AB_IN), D_MODEL ** -0.5),
        'a_v_ln_g': 1.0 + nrm(ks[5], (N_EVEN, A_GROUPS, A_DIM), 0.02),
        'a_w_s': nrm(ks[6], (N_EVEN, A_GROUPS, A_CHUNK, A_CHUNK), 1.0) * row_scale[:, None],
        'a_b_s': nrm(ks[7], (N_EVEN, A_GROUPS, A_CHUNK), 0.02),
        'b_gate_w2': nrm(ks[8], (N_EVEN, B_RANK, B_HEADS * B_DK), B_RANK ** -0.5),
        'b_gate_b': nrm(ks[9], (N_EVEN, B_HEADS * B_DK), 0.02),
        'b_out_norm_g': 1.0 + nrm(ks[10], (N_EVEN, B_DV), 0.02),
        'ab_w_out': nrm(ks[11], (N_EVEN, AB_OUT, D_MODEL), AB_OUT ** -0.5),
        'c_w_in': nrm(ks[12], (N_ODD, D_MODEL, C_IN), D_MODEL ** -0.5),
        'c_w_out': nrm(ks[13], (N_ODD, C_WIDTH, D_MODEL), C_WIDTH ** -0.5),
        'rel_bias': nrm(ks[14], (REL_BUCKETS, C_HEADS), 0.2),
        'ffn_w1': nrm(ks[15], (DEPTH, D_MODEL, D_FF), D_MODEL ** -0.5),
        'ffn_w2': nrm(ks[16], (DEPTH, D_FF, D_MODEL), D_FF ** -0.5),
    }


def reference(x, norm_mix_g, norm_ffn_g, final_norm_g, ab_w_in, a_v_ln_g, a_w_s, a_b_s,
              b_gate_w2, b_gate_b, b_out_norm_g, ab_w_out, c_w_in, c_w_out, rel_bias,
              ffn_w1, ffn_w2):
    h = x
    for layer in range(DEPTH):
        i = layer // 2
        hn = _rmsnorm(h, norm_mix_g[layer])
        if layer % 2 == 0:
            mix = _even_mixer(hn, ab_w_in[i], a_v_ln_g[i], a_w_s[i], a_b_s[i], b_gate_w2[i],
                              b_gate_b[i], b_out_norm_g[i], ab_w_out[i])
        else:
            mix = _odd_mixer(hn, c_w_in[i], c_w_out[i], rel_bias)
        h = h + mix.astype(h.dtype)
        hn = _rmsnorm(h, norm_ffn_g[layer])
        h = h + _sqrelu_mlp(hn, ffn_w1[layer], ffn_w2[layer]).astype(h.dtype)
    return _rmsnorm(h, final_norm_g)
```

```python
import math
from contextlib import ExitStack

import numpy as np
import concourse.bass as bass
import concourse.mybir as mybir
from concourse.bass_utils import run_bass_kernel_spmd

F32 = mybir.dt.float32
BF16 = mybir.dt.bfloat16
AF = mybir.ActivationFunctionType
ALU = mybir.AluOpType
AX = mybir.AxisListType

D = 2048
NDB = D // 128
DFF = 8192
NFB = DFF // 128
SEQ = 4096
BATCH = 4
DEPTH = 4
TS = 512
EPS = 1e-6
AB_IN = 5136
C_IN = 4176


class Buf:
    __slots__ = ("w", "r")

    def __init__(self):
        self.w = {}
        self.r = {}


def _merge(dst, src):
    for s, v in src.items():
        if dst.get(s, 0) < v:
            dst[s] = v


class KB:
    ENG = ("pe", "act", "dve", "pool", "sp")

    def __init__(self, nc, es):
        self.nc = nc
        self.es = es
        self.q = {e: [] for e in self.ENG}
        self.semh = {}
        self.semc = {}
        for e in ("pe", "act", "dve", "pool"):
            self.newsem(e)

    def newsem(self, name):
        if name not in self.semh:
            self.semh[name] = self.es.enter_context(self.nc.semaphore("s_" + name))
            self.semc[name] = 0
        return name

    def op(self, eng, fn, reads=(), writes=(), sem=None, inc=1):
        deps = {}
        for b in reads:
            _merge(deps, b.w)
        for b in writes:
            _merge(deps, b.w)
            _merge(deps, b.r)
        if sem is None:
            sem = eng
        if eng == "pe":
            deps.pop("pe", None)
        self.semc[sem] += inc
        val = self.semc[sem]
        self.q[eng].append((fn, deps, sem, inc))
        for b in reads:
            if b.r.get(sem, 0) < val:
                b.r[sem] = val
        for b in writes:
            b.w = {sem: val}
            b.r = {}
        return (sem, val)

    def dma(self, eng, out, in_, reads=(), writes=(), chan="d0"):
        self.newsem(chan)
        return self.op(eng, lambda e: e.dma_start(out=out, in_=in_), reads, writes,
                       sem=chan, inc=16)

    def wait_all(self, eng, bufs):
        deps = {}
        for b in bufs:
            _merge(deps, b.w)
            _merge(deps, b.r)
        self.q[eng].append((None, deps, None, 0))

    def replay(self, eng, e):
        waited = {}
        for fn, deps, sem, inc in self.q[eng]:
            for s, v in deps.items():
                if waited.get(s, 0) < v:
                    e.wait_ge(self.semh[s], v)
                    waited[s] = v
            if fn is not None:
                ins = fn(e)
                ins.then_inc(self.semh[sem], inc)


def build(cfg):
    T = cfg["T"]
    layers = cfg["layers"]
    parts = cfg.get("parts", ("mix", "ffn"))
    do_final = cfg.get("final", True)
    NST = T // TS
    nc = bass.Bass("TRN2", target_bir_lowering=False)

    def din(name, shape):
        return nc.dram_tensor(name, list(shape), F32, kind="ExternalInput").ap()

    x = din("x", [T, D])
    norm_mix_g = din("norm_mix_g", [DEPTH, D])
    norm_ffn_g = din("norm_ffn_g", [DEPTH, D])
    final_norm_g = din("final_norm_g", [1, D])
    ffn_w1 = din("ffn_w1", [DEPTH, D, DFF])
    ffn_w2 = din("ffn_w2", [DEPTH, DFF, D])
    ab_w_in = din("ab_w_in", [2, D, AB_IN])
    a_v_ln_g = din("a_v_ln_g", [2, 1, 1024])
    a_w_s = din("a_w_s", [2, 8, 128, 128])
    a_b_s = din("a_b_s", [2, 1, 1024])
    b_gate_w2 = din("b_gate_w2", [2, 16, 512])
    b_gate_b = din("b_gate_b", [2, 1, 512])
    b_out_norm_g = din("b_out_norm_g", [2, 1, 256])
    ab_w_out = din("ab_w_out", [2, D, D])
    c_w_in = din("c_w_in", [2, D, C_IN])
    c_w_out = din("c_w_out", [2, D, D])
    rel_bias = din("rel_bias", [32, 16])
    oh_pad = din("oh_pad", [32, 384])
    kT_d = nc.dram_tensor("kT_d", [4, 128, T], BF16, kind="Internal").ap()
    v_d = nc.dram_tensor("v_d", [T, 512], BF16, kind="Internal").ap()
    fpad_d = nc.dram_tensor("fpad_d", [16, 384], F32, kind="Internal").ap()
    out = nc.dram_tensor("out", [T, D], F32, kind="ExternalOutput").ap()
    hscr = nc.dram_tensor("hscr", [T, D], F32, kind="Internal").ap()

    es = ExitStack()
    with es:
        k = KB(nc, es)

        def sb(name, shape, dt):
            return es.enter_context(nc.sbuf_tensor(name, list(shape), dt))

        ps = [es.enter_context(nc.psum_tensor(f"ps{i}", [128, 512], F32)) for i in range(8)]
        psb = [Buf() for _ in range(8)]
        ps_rr = [0]

        reserved = set()

        def next_bank():
            while True:
                i = ps_rr[0]
                ps_rr[0] = (i + 1) % 8
                if i not in reserved:
                    return i

        def reserve_bank():
            i = next_bank()
            reserved.add(i)
            return i

        ident = sb("ident", [128, 128], BF16)
        identf = sb("identf", [128, 128], F32)
        b_ident = Buf()
        hs = sb("hs", [128, TS // 128, D], F32)
        b_hs = [Buf() for _ in range(TS // 128)]
        gB = sb("gB", [128, D], F32)
        b_gB = Buf()
        hn = sb("hn", [128, D], BF16)
        b_hn = Buf()
        junk = sb("junk", [128, 256], BF16)
        b_junk = Buf()
        stat = sb("stat", [128, 8], F32)
        b_stat = Buf()
        hnT = sb("hnT", [128, NDB, TS], BF16)
        b_hnT = [Buf() for _ in range(TS // 128)]
        actT = sb("actT", [128, NFB, TS], BF16)
        b_actT = [Buf() for _ in range(NFB)]
        arena = actT[:, :, :].rearrange("p a b -> p (a b)")

        def carve(off, n, pat=None, **kw):
            v = arena[:, off:off + n]
            return v.rearrange(pat, **kw) if pat else v
        a_uT = carve(0, 4096, "p (g t) -> p g t", g=8)
        v_sgu = carve(4096, 4096, "p (j c) -> p j c", j=4)
        v_gla = carve(8192, 4096, "p (j c) -> p j c", j=4)
        r_s = carve(12288, 4096, "p (j c) -> p j c", j=4)
        catT = carve(16384, 8192, "p (b t) -> p b t", b=16)
        qe = carve(24576, 2048, "p (h t) -> p h t", h=4)
        ke = carve(26624, 2048, "p (h t) -> p h t", h=4)
        ks = carve(28672, 2048, "p (h t) -> p h t", h=4)
        ksT = carve(30720, 2048, "p (h j d) -> p h j d", h=4, j=4)
        b_auT = [Buf() for _ in range(8)]
        b_vsgu = [Buf() for _ in range(4)]
        b_vgla = [Buf() for _ in range(4)]
        b_rs = [Buf() for _ in range(4)]
        b_catT = [Buf() for _ in range(16)]
        b_qe = [Buf() for _ in range(4)]
        b_ke = [Buf() for _ in range(4)]
        b_ks = [Buf() for _ in range(4)]
        b_ksT = [Buf() for _ in range(4)]
        mix_bufs = b_auT + b_vsgu + b_vgla + b_rs + b_catT + b_qe + b_ke + b_ks + b_ksT
        arena2 = sb("arena2", [128, 18432], BF16)

        def carve2(off_b, nbytes, dt, pat=None, **kw):
            v = arena2[:, off_b // 2:(off_b + nbytes) // 2]
            if dt == F32:
                v = v.bitcast(F32)
            return v.rearrange(pat, **kw) if pat else v
        E1 = carve2(0, 4096, BF16, "p (h t) -> p h t", h=4)
        E2 = carve2(4096, 4096, BF16, "p (h t) -> p h t", h=4)
        b_E = [Buf() for _ in range(4)]
        explast = sb("explast", [128, 4, 4], F32)
        spt = [carve2(8192 + i * 2048, 2048, F32) for i in range(2)]
        b_spt = [Buf() for _ in range(2)]
        tmpf = [carve2(12288 + i * 2048, 2048, F32) for i in range(2)]
        b_tmpf = [Buf() for _ in range(2)]
        tmpf_rr = [0]
        scTm = carve2(16384, 1024, BF16, "p (h t) -> p h t", h=4)
        b_scTm = Buf()
        Sst = carve2(17408, 4096, F32, "p (h v) -> p h v", h=4)
        Sbf = carve2(21504, 2048, BF16, "p (h v) -> p h v", h=4)
        b_S = [Buf() for _ in range(4)]
        b_Sbf = [Buf() for _ in range(4)]
        lnB = carve2(23552, 4096, F32)
        ongB = carve2(27648, 1024, F32)
        WT = carve2(28672, 2048, BF16, "p (g t) -> p g t", g=8)
        bout = carve2(30720, 2048, BF16)
        b_bout = Buf()
        bs_row = sb("bs_row", [1, 1024], BF16)
        W2ext = sb("W2ext", [32, 512], BF16)
        even2_bufs = b_E + b_spt + b_tmpf + [b_scTm] + b_S + b_Sbf + [b_bout]
        Iacc = carve2(0, 16384, F32)
        b_I = Buf()
        ikT = carve2(16384, 8192, BF16)
        b_ikT = [Buf() for _ in range(8)]
        BTn = carve2(24576, 8192, BF16, "p (h t) -> p h t", h=16)
        b_BTn = Buf()
        PTb = [carve2(32768 + i * 1024, 1024, BF16) for i in range(2)]
        PMb = [carve2(34816 + i * 1024, 1024, BF16) for i in range(2)]
        b_PT = [Buf() for _ in range(2)]
        b_PM = [Buf() for _ in range(2)]
        odd2_bufs = [b_I] + b_ikT + [b_BTn] + b_PT + b_PM
        b_lc = Buf()
        glr_ext = sb("glr_ext", [32, TS], BF16)
        b_glr = Buf()
        ones_row = sb("ones_row", [1, 128], BF16)
        onec = sb("onec", [128, 1], F32)
        triNeg = sb("triNeg", [128, 128], F32)
        maskT = sb("maskT", [128, 4, 128], BF16)
        cf = sb("cf", [128, 128], F32)
        st2 = sb("st2", [128, 16], F32)
        b_st2 = Buf()

        def alias_fence(src, dst):
            tok = {}
            for b in src:
                _merge(tok, b.w)
                _merge(tok, b.r)
            for b in dst:
                _merge(b.r, tok)
        rtmp = [sb(f"rtmp{i}", [128, TS], F32) for i in range(2)]
        b_rtmp = [Buf() for _ in range(2)]
        NW1 = 2
        W1C = 256
        w1s = [sb(f"w1s{i}", [128, NDB, W1C], BF16) for i in range(NW1)]
        b_w1s = [Buf() for _ in range(NW1)]
        NW2 = 3
        W2R = 4
        w2s = [sb(f"w2s{i}", [128, W2R, 512], BF16) for i in range(NW2)]
        b_w2s = [Buf() for _ in range(NW2)]
        w1_rr = [0]
        w2_rr = [0]
        b_hdram = [Buf() for _ in range(NST)]

        epsc = sb("epsc", [128, 1], F32)
        k.op("pool", lambda e: e.memset(epsc[:], EPS), writes=[b_ident])
        k.op("pool", lambda e: e.memset(identf[:], 0.0), writes=[b_ident])
        k.op("pool", lambda e: e.affine_select(
            out=identf[:], in_=identf[:], pattern=[[-1, 128]], compare_op=ALU.not_equal,
            fill=1.0, base=0, channel_multiplier=1), reads=[b_ident], writes=[b_ident])
        k.op("dve", lambda e: e.tensor_copy(out=ident[:], in_=identf[:]),
             reads=[b_ident], writes=[b_ident])

        k.op("pool", lambda e: e.memset(onec[:], 1.0), writes=[b_ident])
        k.op("pool", lambda e: e.memset(ones_row[:], 1.0), writes=[b_ident])
        k.op("pool", lambda e: e.memset(glr_ext[:], 1.0), writes=[b_glr])
        k.op("pool", lambda e: e.memset(triNeg[:], -1.0 / 16.0), writes=[b_ident])
        k.op("pool", lambda e: e.affine_select(
            out=triNeg[:], in_=triNeg[:], pattern=[[1, 128]], compare_op=ALU.is_ge,
            fill=0.0, base=0, channel_multiplier=-1), reads=[b_ident], writes=[b_ident])
        k.op("pool", lambda e: e.memset(cf[:], 1.0), writes=[b_ident])
        k.op("pool", lambda e: e.affine_select(
            out=cf[:], in_=cf[:], pattern=[[1, 128]], compare_op=ALU.is_ge,
            fill=0.0, base=0, channel_multiplier=-1), reads=[b_ident], writes=[b_ident])
        for hh in range(4):
            k.op("dve", lambda e, hh=hh: e.tensor_copy(out=maskT[:, hh, :], in_=cf[:]),
                 reads=[b_ident], writes=[b_ident])
        def load_gain(g_ap):
            k.dma("sp", gB[:], g_ap.partition_broadcast(128), writes=[b_gB], chan="gB")

        def norm_tile(j, dst_T=True):
            k.op("act", lambda e: e.activation(out=hn[:], in_=hs[:, j, :], func=AF.Square,
                                               accum_out=stat[:, 0:1]),
                 reads=[b_hs[j]], writes=[b_hn, b_stat])
            k.op("act", lambda e: e.activation(out=stat[:, 1:2], in_=stat[:, 0:1], func=AF.Sqrt,
                                               bias=epsc[:, 0:1], scale=1.0 / D),
                 reads=[b_stat, b_ident], writes=[b_stat])
            k.op("dve", lambda e: e.reciprocal(out=stat[:, 2:3], in_=stat[:, 1:2]),
                 reads=[b_stat], writes=[b_stat])

        def norm_apply_T(j):
            k.op("dve", lambda e: e.scalar_tensor_tensor(
                out=hn[:], in0=hs[:, j, :], scalar=stat[:, 2:3], in1=gB[:],
                op0=ALU.mult, op1=ALU.mult),
                reads=[b_hs[j], b_stat, b_gB], writes=[b_hn])
            for half in range(2):
                bi = next_bank()
                pt = ps[bi].bitcast(BF16)
                for q in range(8):
                    db = half * 8 + q
                    k.op("pe", lambda e, db=db, q=q, pt=pt: e.transpose(
                        out=pt[:, q * 128:(q + 1) * 128], in_=hn[:, db * 128:(db + 1) * 128],
                        identity=ident[:]),
                        reads=[b_hn, b_ident], writes=[psb[bi]])
                eng = "act" if half == 0 else "dve"
                src = pt[:, :].rearrange("p (q t) -> p q t", q=8)
                dst = hnT[:, half * 8:(half + 1) * 8, j * 128:(j + 1) * 128]
                if eng == "act":
                    k.op("act", lambda e, src=src, dst=dst: e.copy(out=dst, in_=src),
                         reads=[psb[bi]], writes=[b_hnT[j]])
                else:
                    k.op("dve", lambda e, src=src, dst=dst: e.tensor_copy(out=dst, in_=src),
                         reads=[psb[bi]], writes=[b_hnT[j]])

        def ffn_supertile(l):
            NTT = TS // 128
            for c in range(DFF // W1C):
                s = w1_rr[0]
                w1_rr[0] = (s + 1) % NW1
                src = ffn_w1[l, :, c * W1C:(c + 1) * W1C].rearrange("(b p) c -> p b c", p=128)
                k.dma("pool", w1s[s][:], src, writes=[b_w1s[s]], chan=f"w1_{s}")
                for fb in range(W1C // 128):
                    ffb = c * (W1C // 128) + fb
                    bi = next_bank()
                    for db in range(NDB):
                        k.op("pe", lambda e, s=s, fb=fb, db=db, bi=bi: e.matmul(
                            ps[bi][:, :], lhsT=w1s[s][:, db, fb * 128:(fb + 1) * 128],
                            rhs=hnT[:, db, :], start=(db == 0), stop=(db == NDB - 1)),
                            reads=[b_w1s[s]] + b_hnT, writes=[psb[bi]])
                    r = ffb % 2
                    k.op("act", lambda e, bi=bi, r=r: e.activation(
                        out=rtmp[r][:], in_=ps[bi][:, :], func=AF.Relu),
                        reads=[psb[bi]], writes=[b_rtmp[r]])
                    k.op("dve", lambda e, r=r, ffb=ffb: e.tensor_tensor(
                        out=actT[:, ffb, :], in0=rtmp[r][:], in1=rtmp[r][:], op=ALU.mult),
                        reads=[b_rtmp[r]], writes=[b_actT[ffb]])
            for dq in range(D // 512):
                banks = [next_bank() for _ in range(NTT)]
                for c in range(NFB // W2R):
                    s = w2_rr[0]
                    w2_rr[0] = (s + 1) % NW2
                    src = ffn_w2[l, c * W2R * 128:(c + 1) * W2R * 128,
                                 dq * 512:(dq + 1) * 512].rearrange("(b p) c -> p b c", p=128)
                    k.dma("pool", w2s[s][:], src, writes=[b_w2s[s]], chan=f"w2_{s}")
                    for fr in range(W2R):
                        ffb = c * W2R + fr
                        for j in range(NTT):
                            bi = banks[j]
                            k.op("pe", lambda e, s=s, fr=fr, ffb=ffb, j=j, bi=bi: e.matmul(
                                ps[bi][:, :], lhsT=actT[:, ffb, j * 128:(j + 1) * 128],
                                rhs=w2s[s][:, fr, :], start=(ffb == 0), stop=(ffb == NFB - 1)),
                                reads=[b_w2s[s], b_actT[ffb]], writes=[psb[bi]])
                for j in range(NTT):
                    bi = banks[j]
                    k.op("dve", lambda e, j=j, bi=bi, dq=dq: e.tensor_tensor(
                        out=hs[:, j, dq * 512:(dq + 1) * 512], in0=ps[bi][:, :],
                        in1=hs[:, j, dq * 512:(dq + 1) * 512], op=ALU.add),
                        reads=[psb[bi], b_hs[j]], writes=[b_hs[j]])


        lnsc = sb("lnsc", [128, 1], F32)
        k.op("pool", lambda e: e.memset(lnsc[:], -0.5 * math.log(128.0)), writes=[b_ident])

        def tm_tmp():
            r = tmpf_rr[0]
            tmpf_rr[0] = 1 - r
            return r

        def wchunk(w2d, c0, ncols):
            s_ = w1_rr[0]
            w1_rr[0] = (s_ + 1) % NW1
            k.dma("pool", w1s[s_][:, :, 0:ncols],
                  w2d[:, c0:c0 + ncols].rearrange("(b p) c -> p b c", p=128),
                  writes=[b_w1s[s_]], chan=f"w1_{s_}")
            return s_

        def proj_fm(w2d, c0, ncols, evac):
            s_ = wchunk(w2d, c0, ncols)
            nb = (ncols + 127) // 128
            for fb in range(nb):
                m = min(128, ncols - fb * 128)
                bi = next_bank()
                for db in range(NDB):
                    k.op("pe", lambda e, s_=s_, fb=fb, db=db, bi=bi, m=m: e.matmul(
                        ps[bi][0:m, :], lhsT=w1s[s_][:, db, fb * 128:fb * 128 + m],
                        rhs=hnT[:, db, :], start=(db == 0), stop=(db == NDB - 1)),
                        reads=[b_w1s[s_]] + b_hnT, writes=[psb[bi]])
                evac(fb, bi)

        def proj_tm(w2d, c0, evac):
            s_ = wchunk(w2d, c0, 256)
            for j in range(4):
                bi = next_bank()
                for db in range(NDB):
                    k.op("pe", lambda e, s_=s_, j=j, db=db, bi=bi: e.matmul(
                        ps[bi][:, 0:256], lhsT=hnT[:, db, j * 128:(j + 1) * 128],
                        rhs=w1s[s_][:, db, 0:256], start=(db == 0), stop=(db == NDB - 1)),
                        reads=[b_w1s[s_], b_hnT[j]], writes=[psb[bi]])
                evac(j, bi)

        def even_consts(i):
            k.dma("pool", W2ext[0:16, :], b_gate_w2[i], writes=[b_lc], chan="lc")
            k.dma("pool", W2ext[16:17, :], b_gate_b[i], writes=[b_lc], chan="lc")
            k.dma("sp", lnB[:], a_v_ln_g[i].partition_broadcast(128), writes=[b_lc], chan="lcs")
            k.dma("sp", ongB[:], b_out_norm_g[i].partition_broadcast(128), writes=[b_lc], chan="lcs")
            k.dma("pool", bs_row[:], a_b_s[i], writes=[b_lc], chan="lc")
            for half in range(2):
                k.dma("sp", rtmp[half][:].rearrange("p (g s) -> p g s", g=4),
                      a_w_s[i, half * 4:(half + 1) * 4].rearrange("g t s -> t g s"),
                      writes=[b_rtmp[half]], chan=f"rt{half}")
                bi = next_bank()
                for gq in range(4):
                    k.op("pe", lambda e, half=half, gq=gq, bi=bi: e.transpose(
                        out=ps[bi][:, gq * 128:(gq + 1) * 128],
                        in_=rtmp[half][:, gq * 128:(gq + 1) * 128], identity=identf[:]),
                        reads=[b_rtmp[half], b_ident], writes=[psb[bi]])
                r_ = tm_tmp()
                k.op("act", lambda e, r_=r_, bi=bi: e.copy(out=tmpf[r_][:], in_=ps[bi][:, :]),
                     reads=[psb[bi]], writes=[b_tmpf[r_]])
                v3 = tmpf[r_][:].rearrange("p (g t) -> p g t", g=4)
                k.op("pool", lambda e, v3=v3: e.affine_select(
                    out=v3, in_=v3, pattern=[[0, 4], [1, 128]], compare_op=ALU.is_ge,
                    fill=0.0, base=0, channel_multiplier=-1),
                    reads=[b_tmpf[r_]], writes=[b_tmpf[r_]])
                k.op("dve", lambda e, v3=v3, half=half: e.tensor_copy(
                    out=WT[:, half * 4:(half + 1) * 4, :], in_=v3),
                    reads=[b_tmpf[r_]], writes=[b_lc])
            k.op("pool", lambda e: e.memset(Sst[:], 0.0), writes=b_S)
            k.op("pool", lambda e: e.memset(Sbf[:], 0.0), writes=b_Sbf)

        def even_mixer(i):
            w_in = ab_w_in[i]
            w_out = ab_w_out[i]
            alias_fence(b_actT, mix_bufs)
            def ev_glr(fb, bi):
                k.op("act", lambda e, bi=bi: e.copy(out=glr_ext[0:16, :], in_=ps[bi][0:16, :]),
                     reads=[psb[bi]], writes=[b_glr])
            proj_fm(w_in, 5120, 16, ev_glr)
            cumb = [reserve_bank() for _ in range(4)]
            for j in range(4):
                bg = next_bank()
                k.op("pe", lambda e, j=j, bg=bg: e.matmul(
                    ps[bg][:, :], lhsT=glr_ext[0:17, j * 128:(j + 1) * 128], rhs=W2ext[0:17, :],
                    start=True, stop=True), reads=[b_glr, b_lc], writes=[psb[bg]])
                sj = j % 2
                k.op("act", lambda e, sj=sj, bg=bg: e.activation(
                    out=spt[sj][:], in_=ps[bg][:, :], func=AF.Exp, scale=-1.0),
                    reads=[psb[bg]], writes=[b_spt[sj]])
                k.op("act", lambda e, sj=sj: e.activation(
                    out=spt[sj][:], in_=spt[sj][:], func=AF.Ln, bias=onec[:, 0:1], scale=1.0),
                    reads=[b_spt[sj], b_ident], writes=[b_spt[sj]])
                for hh in range(4):
                    k.op("pe", lambda e, sj=sj, hh=hh, j=j: e.matmul(
                        ps[cumb[hh]][:, j * 128:(j + 1) * 128],
                        lhsT=spt[sj][:, hh * 128:(hh + 1) * 128], rhs=triNeg[:, :],
                        start=True, stop=True),
                        reads=[b_spt[sj], b_ident], writes=[psb[cumb[hh]]])
            for hh in range(4):
                cb = cumb[hh]
                k.op("act", lambda e, hh=hh, cb=cb: e.activation(
                    out=E1[:, hh, :], in_=ps[cb][:, :], func=AF.Exp, bias=lnsc[:, 0:1], scale=1.0),
                    reads=[psb[cb], b_ident], writes=[b_E[hh]])
                k.op("act", lambda e, hh=hh, cb=cb: e.activation(
                    out=E2[:, hh, :], in_=ps[cb][:, :], func=AF.Exp, scale=-1.0),
                    reads=[psb[cb]], writes=[b_E[hh]])
                k.op("act", lambda e, hh=hh, cb=cb: e.activation(
                    out=explast[:, hh, :], in_=ps[cb][:, 127:512:128], func=AF.Exp),
                    reads=[psb[cb]], writes=[b_E[hh]])
                reserved.discard(cb)
            for c in range(2):
                def ev_q(fb, bi, c=c):
                    hh = c * 2 + fb
                    k.op("dve", lambda e, hh=hh, bi=bi: e.tensor_tensor(
                        out=qe[:, hh, :], in0=ps[bi][:, :], in1=E1[:, hh, :], op=ALU.mult),
                        reads=[psb[bi], b_E[hh]], writes=[b_qe[hh]])
                proj_fm(w_in, 2048 + c * 256, 256, ev_q)
            for c in range(2):
                def ev_k(fb, bi, c=c):
                    hh = c * 2 + fb
                    k.op("dve", lambda e, hh=hh, bi=bi: e.tensor_tensor(
                        out=ke[:, hh, :], in0=ps[bi][:, :], in1=E2[:, hh, :], op=ALU.mult),
                        reads=[psb[bi], b_E[hh]], writes=[b_ke[hh]])
                    for j in range(4):
                        k.op("dve", lambda e, hh=hh, bi=bi, j=j: e.scalar_tensor_tensor(
                            out=ks[:, hh, j * 128:(j + 1) * 128], in0=ps[bi][:, j * 128:(j + 1) * 128],
                            scalar=explast[:, hh, j:j + 1], in1=E2[:, hh, j * 128:(j + 1) * 128],
                            op0=ALU.mult, op1=ALU.mult),
                            reads=[psb[bi], b_E[hh]], writes=[b_ks[hh]])
                    bt = next_bank()
                    pt = ps[bt].bitcast(BF16)
                    for j in range(4):
                        k.op("pe", lambda e, hh=hh, j=j, pt=pt: e.transpose(
                            out=pt[:, j * 128:(j + 1) * 128], in_=ks[:, hh, j * 128:(j + 1) * 128],
                            identity=ident[:]), reads=[b_ks[hh], b_ident], writes=[psb[bt]])
                    k.op("act", lambda e, hh=hh, pt=pt: e.copy(
                        out=ksT[:, hh, :, :], in_=pt[:, 0:512].rearrange("p (j d) -> p j d", j=4)),
                        reads=[psb[bt]], writes=[b_ksT[hh]])
                proj_fm(w_in, 2560 + c * 256, 256, ev_k)
            for c in range(4):
                def ev_u(fb, bi, c=c):
                    g = c * 2 + fb
                    k.op("act", lambda e, g=g, bi=bi: e.activation(
                        out=a_uT[:, g, :], in_=ps[bi][:, :], func=AF.Gelu),
                        reads=[psb[bi]], writes=[b_auT[g]])
                proj_fm(w_in, c * 256, 256, ev_u)
            for c in range(4):
                def ev_v(j, bi, c=c):
                    for gg in range(2):
                        g = c * 2 + gg
                        r_ = tm_tmp()
                        k.op("act", lambda e, r_=r_, bi=bi, gg=gg: e.activation(
                            out=tmpf[r_][:, 0:128], in_=ps[bi][:, gg * 128:(gg + 1) * 128],
                            func=AF.Gelu, accum_out=st2[:, 0:1]),
                            reads=[psb[bi]], writes=[b_tmpf[r_], b_st2])
                        k.op("act", lambda e, r_=r_: e.activation(
                            out=junk[:, 0:128], in_=tmpf[r_][:, 0:128], func=AF.Square,
                            accum_out=st2[:, 1:2]),
                            reads=[b_tmpf[r_]], writes=[b_junk, b_st2])
                        k.op("dve", lambda e: e.tensor_scalar(
                            out=st2[:, 2:3], in0=st2[:, 0:1], scalar1=1.0 / 128, scalar2=None,
                            op0=ALU.mult), reads=[b_st2], writes=[b_st2])
                        k.op("dve", lambda e: e.tensor_tensor(
                            out=st2[:, 3:4], in0=st2[:, 2:3], in1=st2[:, 2:3], op=ALU.mult),
                            reads=[b_st2], writes=[b_st2])
                        k.op("dve", lambda e: e.scalar_tensor_tensor(
                            out=st2[:, 4:5], in0=st2[:, 1:2], scalar=1.0 / 128, in1=st2[:, 3:4],
                            op0=ALU.mult, op1=ALU.subtract), reads=[b_st2], writes=[b_st2])
                        k.op("act", lambda e: e.activation(
                            out=st2[:, 5:6], in_=st2[:, 4:5], func=AF.Sqrt, bias=epsc[:, 0:1],
                            scale=1.0), reads=[b_st2, b_ident], writes=[b_st2])
                        k.op("dve", lambda e: e.reciprocal(out=st2[:, 6:7], in_=st2[:, 5:6]),
                             reads=[b_st2], writes=[b_st2])
                        k.op("dve", lambda e, r_=r_: e.tensor_scalar(
                            out=tmpf[r_][:, 0:128], in0=tmpf[r_][:, 0:128], scalar1=st2[:, 2:3],
                            scalar2=st2[:, 6:7], op0=ALU.subtract, op1=ALU.mult),
                            reads=[b_tmpf[r_], b_st2], writes=[b_tmpf[r_]])
                        k.op("dve", lambda e, r_=r_, g=g, j=j: e.tensor_tensor(
                            out=v_sgu[:, j, g * 128:(g + 1) * 128], in0=tmpf[r_][:, 0:128],
                            in1=lnB[:, g * 128:(g + 1) * 128], op=ALU.mult),
                            reads=[b_tmpf[r_], b_lc], writes=[b_vsgu[j]])
                proj_tm(w_in, 1024 + c * 256, ev_v)
            for c in range(4):
                def ev_vg(j, bi, c=c):
                    k.op("act", lambda e, j=j, bi=bi, c=c: e.copy(
                        out=v_gla[:, j, c * 256:(c + 1) * 256], in_=ps[bi][:, 0:256]),
                        reads=[psb[bi]], writes=[b_vgla[j]])
                proj_tm(w_in, 3072 + c * 256, ev_vg)
            for c in range(4):
                def ev_r(j, bi, c=c):
                    k.op("act", lambda e, j=j, bi=bi, c=c: e.activation(
                        out=r_s[:, j, c * 256:(c + 1) * 256], in_=ps[bi][:, 0:256], func=AF.Silu),
                        reads=[psb[bi]], writes=[b_rs[j]])
                proj_tm(w_in, 4096 + c * 256, ev_r)
            for g in range(8):
                bi = next_bank()
                for j in range(4):
                    k.op("pe", lambda e, g=g, j=j, bi=bi: e.matmul(
                        ps[bi][:, j * 128:(j + 1) * 128], lhsT=v_sgu[:, j, g * 128:(g + 1) * 128],
                        rhs=WT[:, g, :], start=True, stop=False),
                        reads=[b_vsgu[j], b_lc], writes=[psb[bi]])
                    k.op("pe", lambda e, g=g, j=j, bi=bi: e.matmul(
                        ps[bi][:, j * 128:(j + 1) * 128], lhsT=ones_row[0:1, :],
                        rhs=bs_row[0:1, g * 128:(g + 1) * 128], start=False, stop=True),
                        reads=[b_ident, b_lc], writes=[psb[bi]])
                k.op("dve", lambda e, g=g, bi=bi: e.tensor_tensor(
                    out=catT[:, g, :], in0=ps[bi][:, :], in1=a_uT[:, g, :], op=ALU.mult),
                    reads=[psb[bi], b_auT[g]], writes=[b_catT[g]])
            for j in range(4):
                jc = slice(j * 128, (j + 1) * 128)
                bs_ = next_bank()
                for hh in range(4):
                    k.op("pe", lambda e, hh=hh, jc=jc, bs_=bs_: e.matmul(
                        ps[bs_][:, hh * 128:(hh + 1) * 128], lhsT=ke[:, hh, jc], rhs=qe[:, hh, jc],
                        start=True, stop=True), reads=[b_ke[hh], b_qe[hh]], writes=[psb[bs_]])
                k.op("dve", lambda e, bs_=bs_: e.tensor_tensor(
                    out=scTm[:, :, :].rearrange("p h t -> p (h t)"), in0=ps[bs_][:, :],
                    in1=maskT[:, :, :].rearrange("p h t -> p (h t)"), op=ALU.mult),
                    reads=[psb[bs_], b_ident], writes=[b_scTm])
                for hp in range(2):
                    bo = next_bank()
                    for hq in range(2):
                        hh = hp * 2 + hq
                        oc = slice(hq * 256, (hq + 1) * 256)
                        vc = slice(hh * 256, (hh + 1) * 256)
                        k.op("pe", lambda e, hh=hh, oc=oc, vc=vc, bo=bo, j=j: e.matmul(
                            ps[bo][:, oc], lhsT=scTm[:, hh, :], rhs=v_gla[:, j, vc],
                            start=True, stop=False),
                            reads=[b_scTm, b_vgla[j]], writes=[psb[bo]])
                        k.op("pe", lambda e, hh=hh, oc=oc, bo=bo, jc=jc: e.matmul(
                            ps[bo][:, oc], lhsT=qe[:, hh, jc], rhs=Sbf[:, hh, :],
                            start=False, stop=True),
                            reads=[b_qe[hh], b_Sbf[hh]], writes=[psb[bo]])
                    for hq in range(2):
                        hh = hp * 2 + hq
                        oc = slice(hq * 256, (hq + 1) * 256)
                        vc = slice(hh * 256, (hh + 1) * 256)
                        k.op("act", lambda e, oc=oc, bo=bo: e.activation(
                            out=junk[:, 0:256], in_=ps[bo][:, oc], func=AF.Square,
                            accum_out=st2[:, 8:9]), reads=[psb[bo]], writes=[b_junk, b_st2])
                        k.op("act", lambda e: e.activation(
                            out=st2[:, 9:10], in_=st2[:, 8:9], func=AF.Sqrt, bias=epsc[:, 0:1],
                            scale=1.0 / 256), reads=[b_st2, b_ident], writes=[b_st2])
                        k.op("dve", lambda e: e.reciprocal(out=st2[:, 10:11], in_=st2[:, 9:10]),
                             reads=[b_st2], writes=[b_st2])
                        r_ = tm_tmp()
                        k.op("dve", lambda e, r_=r_, oc=oc, bo=bo: e.scalar_tensor_tensor(
                            out=tmpf[r_][:, 0:256], in0=ps[bo][:, oc], scalar=st2[:, 10:11],
                            in1=ongB[:, :], op0=ALU.mult, op1=ALU.mult),
                            reads=[psb[bo], b_st2, b_lc], writes=[b_tmpf[r_]])
                        k.op("dve", lambda e, r_=r_, vc=vc, j=j: e.tensor_tensor(
                            out=bout[:, vc], in0=tmpf[r_][:, 0:256], in1=r_s[:, j, vc], op=ALU.mult),
                            reads=[b_tmpf[r_], b_rs[j]], writes=[b_bout])
                for hp in range(2):
                    bk = next_bank()
                    for hq in range(2):
                        hh = hp * 2 + hq
                        oc = slice(hq * 256, (hq + 1) * 256)
                        vc = slice(hh * 256, (hh + 1) * 256)
                        k.op("pe", lambda e, hh=hh, oc=oc, vc=vc, bk=bk, j=j: e.matmul(
                            ps[bk][:, oc], lhsT=ksT[:, hh, j, :], rhs=v_gla[:, j, vc],
                            start=True, stop=True),
                            reads=[b_ksT[hh], b_vgla[j]], writes=[psb[bk]])
                        k.op("dve", lambda e, hh=hh, oc=oc, bk=bk, j=j: e.scalar_tensor_tensor(
                            out=Sst[:, hh, :], in0=Sst[:, hh, :], scalar=explast[:, hh, j:j + 1],
                            in1=ps[bk][:, oc], op0=ALU.mult, op1=ALU.add),
                            reads=[psb[bk], b_S[hh], b_E[hh]], writes=[b_S[hh]])
                        k.op("act", lambda e, hh=hh: e.copy(out=Sbf[:, hh, :], in_=Sst[:, hh, :]),
                             reads=[b_S[hh]], writes=[b_Sbf[hh]])
                bt = next_bank()
                pt = ps[bt].bitcast(BF16)
                for q8 in range(8):
                    k.op("pe", lambda e, q8=q8, pt=pt: e.transpose(
                        out=pt[:, q8 * 128:(q8 + 1) * 128], in_=bout[:, q8 * 128:(q8 + 1) * 128],
                        identity=ident[:]), reads=[b_bout, b_ident], writes=[psb[bt]])
                k.op("act", lambda e, pt=pt, jc=jc: e.copy(
                    out=catT[:, 8:16, jc], in_=pt[:, :].rearrange("p (q t) -> p q t", q=8)),
                    reads=[psb[bt]], writes=b_catT[8:16])
            for cq in range(8):
                s_ = wchunk(w_out, cq * 256, 256)
                for j in range(4):
                    bi = next_bank()
                    for cb in range(16):
                        k.op("pe", lambda e, s_=s_, j=j, cb=cb, bi=bi: e.matmul(
                            ps[bi][:, 0:256], lhsT=catT[:, cb, j * 128:(j + 1) * 128],
                            rhs=w1s[s_][:, cb, 0:256], start=(cb == 0), stop=(cb == 15)),
                            reads=[b_w1s[s_], b_catT[cb]], writes=[psb[bi]])
                    k.op("dve", lambda e, j=j, bi=bi, cq=cq: e.tensor_tensor(
                        out=hs[:, j, cq * 256:(cq + 1) * 256], in0=ps[bi][:, 0:256],
                        in1=hs[:, j, cq * 256:(cq + 1) * 256], op=ALU.add),
                        reads=[psb[bi], b_hs[j]], writes=[b_hs[j]])
            alias_fence(mix_bufs, b_actT)


        qT = carve(0, 8192, "p (h t) -> p h t", h=16)
        iqT = carve(8192, 4096, "p (b t) -> p b t", b=8)
        selT = carve(12288, 16384, "p (k t) -> p k t", k=32)
        vn = carve(28672, 2048, "p (j c) -> p j c", j=4)
        kTn = carve(30720, 2048, "p (g t) -> p g t", g=4)
        b_qT = [Buf() for _ in range(16)]
        b_iqT = [Buf() for _ in range(8)]
        b_selT = [Buf() for _ in range(4)]
        b_vn = [Buf() for _ in range(4)]
        b_kTn = [Buf() for _ in range(4)]
        odd_bufs = b_qT + b_iqT + b_selT + b_vn + b_kTn
        iw_t = sb("iw_t", [128, 4, 16], F32)
        b_iw = [Buf() for _ in range(4)]
        thr = sb("thr", [128, 16], F32)
        b_thr = Buf()
        halfc = sb("halfc", [128, 1], F32)
        negtri = sb("negtri", [128, 128], F32)
        rb = sb("rb", [32, 16], F32)
        OHs = sb("OHs", [32, 384], F32)
        cfarB = sb("cfarB", [128, 16], F32)
        ones128 = sb("ones128", [128, 128], BF16)
        fsb = sb("fsb", [16, 384], F32)
        selq = sb("selq", [128, 512], BF16)
        b_selq = Buf()
        b_oc = Buf()
        b_kvd = [Buf() for _ in range(NST)]
        b_fpad = Buf()
        has_odd = any(l % 2 == 1 for l in layers) and "mix" in parts
        if has_odd:
            k.op("pool", lambda e: e.memset(halfc[:], 0.5), writes=[b_oc])
            k.op("pool", lambda e: e.memset(ones128[:], 1.0), writes=[b_oc])
            k.op("pool", lambda e: e.memset(negtri[:], 0.0), writes=[b_oc])
            k.op("pool", lambda e: e.affine_select(
                out=negtri[:], in_=negtri[:], pattern=[[-1, 128]], compare_op=ALU.is_ge,
                fill=-1.0e30, base=0, channel_multiplier=1), reads=[b_oc], writes=[b_oc])
            k.dma("sp", rb[:], rel_bias, writes=[b_oc], chan="oc")
            k.dma("sp", OHs[:], oh_pad, writes=[b_oc], chan="oc")
            k.dma("sp", cfarB[:], rel_bias[31:32, :].partition_broadcast(128), writes=[b_oc], chan="oc")
            bi = next_bank()
            k.op("pe", lambda e, bi=bi: e.matmul(ps[bi][0:16, 0:384], lhsT=rb[:, :], rhs=OHs[:, :],
                                                 start=True, stop=True),
                 reads=[b_oc], writes=[psb[bi]])
            k.op("act", lambda e, bi=bi: e.copy(out=fsb[:], in_=ps[bi][0:16, 0:384]),
                 reads=[psb[bi]], writes=[b_oc])
            k.dma("sp", fpad_d, fsb[:], reads=[b_oc], writes=[b_fpad], chan="oc2")

        def odd_consts(i):
            alias_fence(even2_bufs + [b_lc], odd2_bufs)
            k.op("pool", lambda e: e.memset(cf[:], 0.0), writes=[b_ident])
            k.op("pool", lambda e: e.affine_select(
                out=cf[:], in_=cf[:], pattern=[[1, 128]], compare_op=ALU.not_equal,
                fill=1.0, base=-127, channel_multiplier=1), reads=[b_ident], writes=[b_ident])
            for hd in range(16):
                r_ = hd % 2
                src = bass.AP(tensor=fpad_d.tensor, offset=fpad_d[hd:hd + 1, :].offset,
                              ap=[[1, 128], [1, 256]])
                k.dma("sp", rtmp[r_][:, 0:256], src, reads=[b_fpad], writes=[b_rtmp[r_]],
                      chan=f"rt{r_}")
                bi = next_bank()
                k.op("pe", lambda e, r_=r_, bi=bi: e.matmul(
                    ps[bi][:, 0:256], lhsT=cf[:, :], rhs=rtmp[r_][:, 0:256], start=True, stop=True),
                    reads=[b_rtmp[r_], b_ident], writes=[psb[bi]])
                k.op("dve", lambda e, hd=hd, bi=bi: e.tensor_scalar(
                    out=BTn[:, hd, :], in0=ps[bi][:, 0:256], scalar1=cfarB[:, hd:hd + 1],
                    scalar2=None, op0=ALU.subtract),
                    reads=[psb[bi], b_oc], writes=[b_BTn])

        def odd_mixer(i, st):
            w_in = c_w_in[i]
            w_out = c_w_out[i]
            t0 = st * TS
            alias_fence(b_actT, odd_bufs)
            for c in range(8):
                def ev_q(fb, bi, c=c):
                    hd = c * 2 + fb
                    k.op("act", lambda e, hd=hd, bi=bi: e.activation(
                        out=qT[:, hd, :], in_=ps[bi][:, :], func=AF.Copy, scale=128.0 ** -0.5),
                        reads=[psb[bi]], writes=[b_qT[hd]])
                proj_fm(w_in, c * 256, 256, ev_q)
            for c in range(2):
                def ev_k(fb, bi, c=c):
                    g = c * 2 + fb
                    k.op("act", lambda e, g=g, bi=bi: e.copy(out=kTn[:, g, :], in_=ps[bi][:, :]),
                         reads=[psb[bi]], writes=[b_kTn[g]])
                    k.dma("sp", kT_d[g, :, t0:t0 + TS], kTn[:, g, :], reads=[b_kTn[g]],
                          writes=[b_kvd[st]], chan=f"kvw{g}")
                proj_fm(w_in, 2048 + c * 256, 256, ev_k)
            for c in range(4):
                def ev_iq(fb, bi, c=c):
                    blk = c * 2 + fb
                    k.op("act", lambda e, blk=blk, bi=bi: e.copy(out=iqT[:, blk, :], in_=ps[bi][:, :]),
                         reads=[psb[bi]], writes=[b_iqT[blk]])
                proj_fm(w_in, 3072 + c * 256, 256, ev_iq)
            s_ = w1_rr[0]
            w1_rr[0] = (s_ + 1) % NW1
            for dup in range(2):
                k.dma("pool", w1s[s_][:, :, dup * 64:(dup + 1) * 64],
                      w_in[:, 4096:4160].rearrange("(b p) c -> p b c", p=128),
                      writes=[b_w1s[s_]], chan=f"w1_{s_}")
            bi = next_bank()
            for db in range(NDB):
                k.op("pe", lambda e, s_=s_, db=db, bi=bi: e.matmul(
                    ps[bi][:, :], lhsT=w1s[s_][:, db, 0:128], rhs=hnT[:, db, :],
                    start=(db == 0), stop=(db == NDB - 1)),
                    reads=[b_w1s[s_]] + b_hnT, writes=[psb[bi]])
            k.op("act", lambda e, bi=bi: e.copy(out=ikT[:, t0:t0 + TS], in_=ps[bi][:, :]),
                 reads=[psb[bi]], writes=[b_ikT[st]])
            for c in range(2):
                def ev_v(j, bi, c=c):
                    k.op("act", lambda e, j=j, bi=bi, c=c: e.copy(
                        out=vn[:, j, c * 256:(c + 1) * 256], in_=ps[bi][:, 0:256]),
                        reads=[psb[bi]], writes=[b_vn[j]])
                    if c == 1:
                        k.dma("sp", v_d[t0 + j * 128:t0 + (j + 1) * 128, :], vn[:, j, :],
                              reads=[b_vn[j]], writes=[b_kvd[st]], chan=f"kvw{j}")
                proj_tm(w_in, 2560 + c * 256, ev_v)
            s_ = wchunk(w_in, 4160, 16)
            for j in range(4):
                bi = next_bank()
                for db in range(NDB):
                    k.op("pe", lambda e, s_=s_, j=j, db=db, bi=bi: e.matmul(
                        ps[bi][:, 0:16], lhsT=hnT[:, db, j * 128:(j + 1) * 128],
                        rhs=w1s[s_][:, db, 0:16], start=(db == 0), stop=(db == NDB - 1)),
                        reads=[b_w1s[s_], b_hnT[j]], writes=[psb[bi]])
                k.op("act", lambda e, j=j, bi=bi: e.copy(out=iw_t[:, j, :], in_=ps[bi][:, 0:16]),
                     reads=[psb[bi]], writes=[b_iw[j]])
            for j in range(4):
                qb = st * 4 + j
                nk = (qb + 1) * 128
                nch = (nk + 511) // 512
                for h in range(16):
                    blk, po = h // 2, (h % 2) * 64
                    for c in range(nch):
                        ncol = min(512, nk - c * 512)
                        bi = next_bank()
                        k.op("pe", lambda e, blk=blk, po=po, j=j, c=c, ncol=ncol, bi=bi: e.matmul(
                            ps[bi][:, 0:ncol], lhsT=iqT[po:po + 64, blk, j * 128:(j + 1) * 128],
                            rhs=ikT[po:po + 64, c * 512:c * 512 + ncol], start=True, stop=True),
                            reads=[b_iqT[blk], b_ikT[c]], writes=[psb[bi]])
                        r_ = (h * nch + c) % 2
                        k.op("act", lambda e, r_=r_, ncol=ncol, bi=bi: e.activation(
                            out=rtmp[r_][:, 0:ncol], in_=ps[bi][:, 0:ncol], func=AF.Relu),
                            reads=[psb[bi]], writes=[b_rtmp[r_]])
                        cs = slice(c * 512, c * 512 + ncol)
                        if h == 0:
                            k.op("dve", lambda e, r_=r_, ncol=ncol, cs=cs, j=j: e.tensor_scalar(
                                out=Iacc[:, cs], in0=rtmp[r_][:, 0:ncol], scalar1=iw_t[:, j, 0:1],
                                scalar2=None, op0=ALU.mult),
                                reads=[b_rtmp[r_], b_iw[j]], writes=[b_I])
                        else:
                            k.op("dve", lambda e, r_=r_, ncol=ncol, cs=cs, j=j, h=h: e.scalar_tensor_tensor(
                                out=Iacc[:, cs], in0=rtmp[r_][:, 0:ncol], scalar=iw_t[:, j, h:h + 1],
                                in1=Iacc[:, cs], op0=ALU.mult, op1=ALU.add),
                                reads=[b_rtmp[r_], b_iw[j], b_I], writes=[b_I])
                k.op("dve", lambda e, nk=nk: e.tensor_reduce(out=thr[:, 1:2], in_=Iacc[:, 0:nk],
                                                            axis=AX.X, op=ALU.max),
                     reads=[b_I], writes=[b_thr])
                k.op("dve", lambda e, nk=nk: e.tensor_reduce(out=thr[:, 0:1], in_=Iacc[:, 0:nk],
                                                            axis=AX.X, op=ALU.min),
                     reads=[b_I], writes=[b_thr])
                k.op("dve", lambda e, qb=qb: e.tensor_tensor(
                    out=Iacc[:, qb * 128:(qb + 1) * 128], in0=Iacc[:, qb * 128:(qb + 1) * 128],
                    in1=negtri[:, :], op=ALU.add), reads=[b_I, b_oc], writes=[b_I])
                if nk > 256:
                    for it in range(22):
                        k.op("dve", lambda e: e.scalar_tensor_tensor(
                            out=thr[:, 2:3], in0=thr[:, 0:1], scalar=thr[:, 1:2], in1=halfc[:, 0:1],
                            op0=ALU.add, op1=ALU.mult), reads=[b_thr, b_oc], writes=[b_thr])
                        n0 = min(nk, 2048)
                        k.op("dve", lambda e, n0=n0: e.tensor_scalar(
                            out=hn[:, 0:n0], in0=Iacc[:, 0:n0], scalar1=thr[:, 2:3], scalar2=None,
                            op0=ALU.is_ge, op1=ALU.add, accum_out=thr[:, 3:4]),
                            reads=[b_I, b_thr], writes=[b_hn, b_thr])
                        if nk > 2048:
                            k.op("dve", lambda e, nk=nk: e.tensor_scalar(
                                out=hn[:, 0:nk - 2048], in0=Iacc[:, 2048:nk], scalar1=thr[:, 2:3],
                                scalar2=None, op0=ALU.is_ge, op1=ALU.add, accum_out=thr[:, 4:5]),
                                reads=[b_I, b_thr], writes=[b_hn, b_thr])
                            k.op("dve", lambda e: e.tensor_tensor(
                                out=thr[:, 3:4], in0=thr[:, 3:4], in1=thr[:, 4:5], op=ALU.add),
                                reads=[b_thr], writes=[b_thr])
                        k.op("dve", lambda e: e.tensor_scalar(
                            out=thr[:, 5:6], in0=thr[:, 3:4], scalar1=255.5, scalar2=None,
                            op0=ALU.is_ge), reads=[b_thr], writes=[b_thr])
                        k.op("dve", lambda e: e.tensor_tensor(
                            out=thr[:, 6:7], in0=thr[:, 2:3], in1=thr[:, 0:1], op=ALU.subtract),
                            reads=[b_thr], writes=[b_thr])
                        k.op("dve", lambda e: e.tensor_tensor(
                            out=thr[:, 7:8], in0=thr[:, 1:2], in1=thr[:, 2:3], op=ALU.subtract),
                            reads=[b_thr], writes=[b_thr])
                        k.op("dve", lambda e: e.scalar_tensor_tensor(
                            out=thr[:, 0:1], in0=thr[:, 6:7], scalar=thr[:, 5:6], in1=thr[:, 0:1],
                            op0=ALU.mult, op1=ALU.add), reads=[b_thr], writes=[b_thr])
                        k.op("dve", lambda e: e.scalar_tensor_tensor(
                            out=thr[:, 1:2], in0=thr[:, 7:8], scalar=thr[:, 5:6], in1=thr[:, 2:3],
                            op0=ALU.mult, op1=ALU.add), reads=[b_thr], writes=[b_thr])
                for c in range(nch):
                    ncol = min(512, nk - c * 512)
                    nb = ncol // 128
                    k.op("dve", lambda e, c=c, ncol=ncol: e.tensor_scalar(
                        out=selq[:, 0:ncol], in0=Iacc[:, c * 512:c * 512 + ncol], scalar1=thr[:, 0:1],
                        scalar2=None, op0=ALU.is_ge), reads=[b_I, b_thr], writes=[b_selq])
                    bt = next_bank()
                    pt = ps[bt].bitcast(BF16)
                    for q in range(nb):
                        k.op("pe", lambda e, q=q, pt=pt: e.transpose(
                            out=pt[:, q * 128:(q + 1) * 128], in_=selq[:, q * 128:(q + 1) * 128],
                            identity=ident[:]), reads=[b_selq, b_ident], writes=[psb[bt]])
                    k.op("act", lambda e, c=c, nb=nb, pt=pt, j=j: e.copy(
                        out=selT[:, c * 4:c * 4 + nb, j * 128:(j + 1) * 128],
                        in_=pt[:, 0:nb * 128].rearrange("p (q t) -> p q t", q=nb)),
                        reads=[psb[bt]], writes=[b_selT[j]])
            for hd in range(16):
                g = hd // 4
                bo = reserve_bank()
                bl = reserve_bank()
                nkb = st * 4 + 4
                for c in range(st + 1):
                    s_ = w2_rr[0]
                    w2_rr[0] = (s_ + 1) % NW2
                    kslot = w2s[s_][:, 0:1, :].rearrange("p a c -> p (a c)")
                    vslot = w2s[s_][:, 1:2, :].rearrange("p a (q v) -> p (a q) v", q=4)
                    k.dma("sp", kslot, kT_d[g, :, c * 512:(c + 1) * 512], reads=[b_kvd[c]],
                          writes=[b_w2s[s_]], chan=f"kv{s_}")
                    k.dma("sp", vslot, v_d[c * 512:(c + 1) * 512, g * 128:(g + 1) * 128].rearrange(
                        "(q p) v -> p q v", p=128), reads=[b_kvd[c]], writes=[b_w2s[s_]], chan=f"kv{s_}")
                    for kq in range(4):
                        kb = c * 4 + kq
                        col0 = kq * 128 if c == st else 0
                        ncols = 512 - col0
                        near = kb >= 4 * st - 1
                        bi = next_bank()
                        k.op("pe", lambda e, kslot=kslot, kq=kq, hd=hd, col0=col0, ncols=ncols, bi=bi, near=near: e.matmul(
                            ps[bi][:, 0:ncols], lhsT=kslot[:, kq * 128:(kq + 1) * 128],
                            rhs=qT[:, hd, col0:512], start=True, stop=(not near)),
                            reads=[b_w2s[s_], b_qT[hd]], writes=[psb[bi]])
                        if near:
                            if c == st:
                                off, nbc = 0, min(256, ncols)
                            else:
                                off, nbc = 128, 128
                            k.op("pe", lambda e, hd=hd, off=off, nbc=nbc, bi=bi: e.matmul(
                                ps[bi][:, 0:nbc], lhsT=ident[:, :], rhs=BTn[:, hd, off:off + nbc],
                                start=False, stop=True),
                                reads=[b_BTn, b_ident], writes=[psb[bi]])
                        pr = kb % 2
                        k.op("act", lambda e, pr=pr, ncols=ncols, bi=bi, hd=hd: e.activation(
                            out=PTb[pr][:, 0:ncols], in_=ps[bi][:, 0:ncols], func=AF.Exp,
                            bias=cfarB[:, hd:hd + 1], scale=1.0),
                            reads=[psb[bi], b_oc], writes=[b_PT[pr]])
                        k.op("dve", lambda e, pr=pr, ncols=ncols, kb=kb, col0=col0: e.tensor_tensor(
                            out=PMb[pr][:, 0:ncols], in0=PTb[pr][:, 0:ncols], in1=selT[:, kb, col0:512],
                            op=ALU.mult), reads=[b_PT[pr]] + b_selT, writes=[b_PM[pr]])
                        k.op("pe", lambda e, vslot=vslot, kq=kq, pr=pr, col0=col0, ncols=ncols, kb=kb, bo=bo, nkb=nkb: e.matmul(
                            ps[bo][:, col0:512], lhsT=vslot[:, kq, :], rhs=PMb[pr][:, 0:ncols],
                            start=(kb == 0), stop=(kb == nkb - 1)),
                            reads=[b_w2s[s_], b_PM[pr]], writes=[psb[bo]])
                        k.op("pe", lambda e, pr=pr, col0=col0, ncols=ncols, kb=kb, bl=bl, nkb=nkb: e.matmul(
                            ps[bl][:, col0:512], lhsT=ones128[:, :], rhs=PMb[pr][:, 0:ncols],
                            start=(kb == 0), stop=(kb == nkb - 1)),
                            reads=[b_oc, b_PM[pr]], writes=[psb[bl]])
                r_ = hd % 2
                k.op("dve", lambda e, r_=r_, bl=bl: e.reciprocal(out=rtmp[r_][:, :], in_=ps[bl][:, :]),
                     reads=[psb[bl]], writes=[b_rtmp[r_]])
                k.op("dve", lambda e, r_=r_, bo=bo, hd=hd: e.tensor_tensor(
                    out=qT[:, hd, :], in0=ps[bo][:, :], in1=rtmp[r_][:, :], op=ALU.mult),
                    reads=[psb[bo], b_rtmp[r_]], writes=[b_qT[hd]])
                reserved.discard(bo)
                reserved.discard(bl)
            for cq in range(8):
                s_ = wchunk(w_out, cq * 256, 256)
                for j in range(4):
                    bi = next_bank()
                    for cb in range(16):
                        k.op("pe", lambda e, s_=s_, j=j, cb=cb, bi=bi: e.matmul(
                            ps[bi][:, 0:256], lhsT=qT[:, cb, j * 128:(j + 1) * 128],
                            rhs=w1s[s_][:, cb, 0:256], start=(cb == 0), stop=(cb == 15)),
                            reads=[b_w1s[s_], b_qT[cb]], writes=[psb[bi]])
                    k.op("dve", lambda e, j=j, bi=bi, cq=cq: e.tensor_tensor(
                        out=hs[:, j, cq * 256:(cq + 1) * 256], in0=ps[bi][:, 0:256],
                        in1=hs[:, j, cq * 256:(cq + 1) * 256], op=ALU.add),
                        reads=[psb[bi], b_hs[j]], writes=[b_hs[j]])
            alias_fence(odd_bufs, b_actT)

        first = True
        for li, l in enumerate(layers):
            last = (li == len(layers) - 1)
            if "mix" in parts and l % 2 == 0:
                alias_fence(odd2_bufs, even2_bufs + [b_lc])
                even_consts(l // 2)
            if "mix" in parts and l % 2 == 1:
                odd_consts(l // 2)
            for st in range(NST):
                rows = slice(st * TS, (st + 1) * TS)
                src_h = x if first else hscr
                for j in range(TS // 128):
                    k.dma("sp", hs[:, j, :], src_h[st * TS + j * 128: st * TS + (j + 1) * 128, :],
                          reads=[b_hdram[st]] if not first else [], writes=[b_hs[j]], chan=f"hs{j}")
                if "mix" in parts:
                    load_gain(norm_mix_g[l:l + 1, :])
                    for j in range(TS // 128):
                        norm_tile(j)
                        norm_apply_T(j)
                    if l % 2 == 0:
                        even_mixer(l // 2)
                    else:
                        odd_mixer(l // 2, st)
                if "ffn" in parts:
                    load_gain(norm_ffn_g[l:l + 1, :])
                    for j in range(TS // 128):
                        norm_tile(j)
                        norm_apply_T(j)
                    ffn_supertile(l)
                if last and do_final:
                    load_gain(final_norm_g[0:1, :])
                    for j in range(TS // 128):
                        norm_tile(j)
                        k.op("dve", lambda e, j=j: e.scalar_tensor_tensor(
                            out=hs[:, j, :], in0=hs[:, j, :], scalar=stat[:, 2:3], in1=gB[:],
                            op0=ALU.mult, op1=ALU.mult),
                            reads=[b_hs[j], b_stat, b_gB], writes=[b_hs[j]])
                dst_h = out if last else hscr
                for j in range(TS // 128):
                    k.dma("sp", dst_h[st * TS + j * 128: st * TS + (j + 1) * 128, :], hs[:, j, :],
                          reads=[b_hs[j]], writes=[b_hdram[st]], chan=f"ho{j}")
            first = False
        k.wait_all("sp", b_hdram)

        with nc.Block() as block:
            @block.tensor
            def _(e):
                k.replay("pe", e)

            @block.scalar
            def _(e):
                k.replay("act", e)

            @block.vector
            def _(e):
                k.replay("dve", e)

            @block.gpsimd
            def _(e):
                k.replay("pool", e)

            @block.sync
            def _(e):
                k.replay("sp", e)
    return nc


def _bucket_table():
    d = np.arange(0, 257)
    dd = np.maximum(d, 1).astype(np.float32)
    large = 16 + (np.log(dd / np.float32(16)) / np.float32(math.log(128 / 16))
                  * np.float32(16)).astype(np.int32)
    large = np.minimum(large, 31)
    return np.where(d < 16, d, large)


def _oh_pad():
    bk = _bucket_table()
    oh = np.zeros((32, 384), np.float32)
    for m in range(127, 384):
        oh[bk[m - 127], m] = 1.0
    return oh


def make_in_map(inp, x):
    f = lambda a: np.ascontiguousarray(np.asarray(a, dtype=np.float32))
    return dict(
        x=f(x), norm_mix_g=f(inp["norm_mix_g"]), norm_ffn_g=f(inp["norm_ffn_g"]),
        final_norm_g=f(inp["final_norm_g"]).reshape(1, -1),
        ffn_w1=f(inp["ffn_w1"]), ffn_w2=f(inp["ffn_w2"]),
        ab_w_in=f(inp["ab_w_in"]), a_v_ln_g=f(inp["a_v_ln_g"]).reshape(2, 1, 1024),
        a_w_s=f(inp["a_w_s"]), a_b_s=f(inp["a_b_s"]).reshape(2, 1, 1024),
        b_gate_w2=f(inp["b_gate_w2"]), b_gate_b=f(inp["b_gate_b"]).reshape(2, 1, 512),
        b_out_norm_g=f(inp["b_out_norm_g"]).reshape(2, 1, 256), ab_w_out=f(inp["ab_w_out"]),
        c_w_in=f(inp["c_w_in"]), c_w_out=f(inp["c_w_out"]), rel_bias=f(inp["rel_bias"]),
        oh_pad=_oh_pad())


def kernel(**inputs):
    x = np.asarray(inputs["x"], dtype=np.float32)
    nc = build(dict(T=SEQ, layers=[0, 1, 2, 3]))
    in_maps = [make_in_map(inputs, x[c % BATCH]) for c in range(8)]
    res = run_bass_kernel_spmd(nc, in_maps, core_ids=list(range(8)))
    out = np.stack([np.asarray(res.results[b]["out"]) for b in range(BATCH)])
    return out.astype(np.float32)
```

```python
import math
from contextlib import ExitStack

import numpy as np
import concourse.bass as bass
import concourse.mybir as mybir
from concourse.bass_utils import run_bass_kernel_spmd

F32 = mybir.dt.float32
BF16 = mybir.dt.bfloat16
AF = mybir.ActivationFunctionType
ALU = mybir.AluOpType
AX = mybir.AxisListType

D = 2048
NDB = D // 128
DFF = 8192
NFB = DFF // 128
SEQ = 4096
BATCH = 4
DEPTH = 4
TS = 512
EPS = 1e-6
AB_IN = 5136
C_IN = 4176


class Buf:
    __slots__ = ("w", "r")

    def __init__(self):
        self.w = {}
        self.r = {}


def _merge(dst, src):
    for s, v in src.items():
        if dst.get(s, 0) < v:
            dst[s] = v


class KB:
    ENG = ("pe", "act", "dve", "pool", "sp")

    def __init__(self, nc, es):
        self.nc = nc
        self.es = es
        self.q = {e: [] for e in self.ENG}
        self.semh = {}
        self.semc = {}
        for e in ("pe", "act", "dve", "pool"):
            self.newsem(e)

    def newsem(self, name):
        if name not in self.semh:
            self.semh[name] = self.es.enter_context(self.nc.semaphore("s_" + name))
            self.semc[name] = 0
        return name

    def op(self, eng, fn, reads=(), writes=(), sem=None, inc=1):
        deps = {}
        for b in reads:
            _merge(deps, b.w)
        for b in writes:
            _merge(deps, b.w)
            _merge(deps, b.r)
        if sem is None:
            sem = eng
        if eng == "pe":
            deps.pop("pe", None)
        self.semc[sem] += inc
        val = self.semc[sem]
        self.q[eng].append((fn, deps, sem, inc))
        for b in reads:
            if b.r.get(sem, 0) < val:
                b.r[sem] = val
        for b in writes:
            b.w = {sem: val}
            b.r = {}
        return (sem, val)

    def dma(self, eng, out, in_, reads=(), writes=(), chan="d0"):
        self.newsem(chan)
        return self.op(eng, lambda e: e.dma_start(out=out, in_=in_), reads, writes,
                       sem=chan, inc=16)

    def wait_all(self, eng, bufs):
        deps = {}
        for b in bufs:
            _merge(deps, b.w)
            _merge(deps, b.r)
        self.q[eng].append((None, deps, None, 0))

    def replay(self, eng, e):
        waited = {}
        for fn, deps, sem, inc in self.q[eng]:
            for s, v in deps.items():
                if waited.get(s, 0) < v:
                    e.wait_ge(self.semh[s], v)
                    waited[s] = v
            if fn is not None:
                ins = fn(e)
                ins.then_inc(self.semh[sem], inc)


def build(cfg):
    T = cfg["T"]
    layers = cfg["layers"]
    parts = cfg.get("parts", ("mix", "ffn"))
    do_final = cfg.get("final", True)
    NST = T // TS
    nc = bass.Bass("TRN2", target_bir_lowering=False)

    def din(name, shape):
        return nc.dram_tensor(name, list(shape), F32, kind="ExternalInput").ap()

    x = din("x", [T, D])
    norm_mix_g = din("norm_mix_g", [DEPTH, D])
    norm_ffn_g = din("norm_ffn_g", [DEPTH, D])
    final_norm_g = din("final_norm_g", [1, D])
    ffn_w1 = din("ffn_w1", [DEPTH, D, DFF])
    ffn_w2 = din("ffn_w2", [DEPTH, DFF, D])
    ab_w_in = din("ab_w_in", [2, D, AB_IN])
    a_v_ln_g = din("a_v_ln_g", [2, 1, 1024])
    a_w_s = din("a_w_s", [2, 8, 128, 128])
    a_b_s = din("a_b_s", [2, 1, 1024])
    b_gate_w2 = din("b_gate_w2", [2, 16, 512])
    b_gate_b = din("b_gate_b", [2, 1, 512])
    b_out_norm_g = din("b_out_norm_g", [2, 1, 256])
    ab_w_out = din("ab_w_out", [2, D, D])
    c_w_in = din("c_w_in", [2, D, C_IN])
    c_w_out = din("c_w_out", [2, D, D])
    rel_bias = din("rel_bias", [32, 16])
    oh_pad = din("oh_pad", [32, 384])
    kT_d = nc.dram_tensor("kT_d", [4, 128, T], BF16, kind="Internal").ap()
    v_d = nc.dram_tensor("v_d", [T, 512], BF16, kind="Internal").ap()
    fpad_d = nc.dram_tensor("fpad_d", [16, 384], F32, kind="Internal").ap()
    out = nc.dram_tensor("out", [T, D], F32, kind="ExternalOutput").ap()
    hscr = nc.dram_tensor("hscr", [T, D], F32, kind="Internal").ap()

    NCHK = 136
    wscr = [nc.dram_tensor(f"wscr{i}", [NCHK, 128, 4096], BF16, kind="Internal").ap()
            for i in range(DEPTH)]
    wreg = {}
    ctx = {"l": 0, "st": 0}

    es = ExitStack()
    with es:
        k = KB(nc, es)

        def cached_load(slot_t, slot_buf, slot_id, key, kind, sub, cast_loads):
            l, st = ctx["l"], ctx["st"]
            if (l, key) not in wreg:
                wreg[(l, key)] = (sum(1 for (ll, _) in wreg if ll == l), Buf())
            idx, wb = wreg[(l, key)]
            assert idx < NCHK
            if kind == "w1":
                scr = wscr[l][idx].rearrange("p (b c) -> p b c", b=16)
            else:
                scr = wscr[l][idx][:, 0:2048].rearrange("p (b c) -> p b c", b=4)
            if st == 0:
                cast_loads()
                k.dma("sp", sub(scr), sub(slot_t), reads=[slot_buf], writes=[wb],
                      chan=f"wo_{kind}{slot_id}")
            else:
                k.dma("sp", sub(slot_t), sub(scr), reads=[wb], writes=[slot_buf],
                      chan=f"wh_{kind}{slot_id}")

        def sb(name, shape, dt):
            return es.enter_context(nc.sbuf_tensor(name, list(shape), dt))

        ps = [es.enter_context(nc.psum_tensor(f"ps{i}", [128, 512], F32)) for i in range(8)]
        psb = [Buf() for _ in range(8)]
        ps_rr = [0]

        reserved = set()

        def next_bank():
            while True:
                i = ps_rr[0]
                ps_rr[0] = (i + 1) % 8
                if i not in reserved:
                    return i

        def reserve_bank():
            i = next_bank()
            reserved.add(i)
            return i

        ident = sb("ident", [128, 128], BF16)
        identf = sb("identf", [128, 128], F32)
        b_ident = Buf()
        hs = sb("hs", [128, TS // 128, D], F32)
        b_hs = [Buf() for _ in range(TS // 128)]
        gB = sb("gB", [128, D], F32)
        b_gB = Buf()
        hn = sb("hn", [128, D], BF16)
        b_hn = Buf()
        junk = sb("junk", [128, 256], BF16)
        b_junk = Buf()
        stat = sb("stat", [128, 8], F32)
        b_stat = Buf()
        hnT = sb("hnT", [128, NDB, TS], BF16)
        b_hnT = [Buf() for _ in range(TS // 128)]
        actT = sb("actT", [128, NFB, TS], BF16)
        b_actT = [Buf() for _ in range(NFB)]
        arena = actT[:, :, :].rearrange("p a b -> p (a b)")

        def carve(off, n, pat=None, **kw):
            v = arena[:, off:off + n]
            return v.rearrange(pat, **kw) if pat else v
        a_uT = carve(0, 4096, "p (g t) -> p g t", g=8)
        v_sgu = carve(4096, 4096, "p (j c) -> p j c", j=4)
        v_gla = carve(8192, 4096, "p (j c) -> p j c", j=4)
        r_s = carve(12288, 4096, "p (j c) -> p j c", j=4)
        catT = carve(16384, 8192, "p (b t) -> p b t", b=16)
        qe = carve(24576, 2048, "p (h t) -> p h t", h=4)
        ke = carve(26624, 2048, "p (h t) -> p h t", h=4)
        ks = carve(28672, 2048, "p (h t) -> p h t", h=4)
        ksT = carve(30720, 2048, "p (h j d) -> p h j d", h=4, j=4)
        b_auT = [Buf() for _ in range(8)]
        b_vsgu = [Buf() for _ in range(4)]
        b_vgla = [Buf() for _ in range(4)]
        b_rs = [Buf() for _ in range(4)]
        b_catT = [Buf() for _ in range(16)]
        b_qe = [Buf() for _ in range(4)]
        b_ke = [Buf() for _ in range(4)]
        b_ks = [Buf() for _ in range(4)]
        b_ksT = [Buf() for _ in range(4)]
        mix_bufs = b_auT + b_vsgu + b_vgla + b_rs + b_catT + b_qe + b_ke + b_ks + b_ksT
        arena2 = sb("arena2", [128, 18432], BF16)

        def carve2(off_b, nbytes, dt, pat=None, **kw):
            v = arena2[:, off_b // 2:(off_b + nbytes) // 2]
            if dt == F32:
                v = v.bitcast(F32)
            return v.rearrange(pat, **kw) if pat else v
        E1 = carve2(0, 4096, BF16, "p (h t) -> p h t", h=4)
        E2 = carve2(4096, 4096, BF16, "p (h t) -> p h t", h=4)
        b_E = [Buf() for _ in range(4)]
        explast = sb("explast", [128, 4, 4], F32)
        spt = [carve2(8192 + i * 2048, 2048, F32) for i in range(2)]
        b_spt = [Buf() for _ in range(2)]
        tmpf = [carve2(12288 + i * 2048, 2048, F32) for i in range(2)]
        b_tmpf = [Buf() for _ in range(2)]
        tmpf_rr = [0]
        scTm = carve2(16384, 1024, BF16, "p (h t) -> p h t", h=4)
        b_scTm = Buf()
        Sst = carve2(17408, 4096, F32, "p (h v) -> p h v", h=4)
        Sbf = carve2(21504, 2048, BF16, "p (h v) -> p h v", h=4)
        b_S = [Buf() for _ in range(4)]
        b_Sbf = [Buf() for _ in range(4)]
        lnB = carve2(23552, 4096, F32)
        ongB = carve2(27648, 1024, F32)
        WT = carve2(28672, 2048, BF16, "p (g t) -> p g t", g=8)
        bout = carve2(30720, 2048, BF16)
        b_bout = Buf()
        bs_row = sb("bs_row", [1, 1024], BF16)
        W2ext = sb("W2ext", [32, 512], BF16)
        even2_bufs = b_E + b_spt + b_tmpf + [b_scTm] + b_S + b_Sbf + [b_bout]
        Iacc = carve2(0, 16384, F32)
        b_I = Buf()
        ikT = carve2(16384, 8192, BF16)
        b_ikT = [Buf() for _ in range(8)]
        BTn = carve2(24576, 8192, BF16, "p (h t) -> p h t", h=16)
        b_BTn = Buf()
        PTb = [carve2(32768 + i * 1024, 1024, BF16) for i in range(2)]
        PMb = [carve2(34816 + i * 1024, 1024, BF16) for i in range(2)]
        b_PT = [Buf() for _ in range(2)]
        b_PM = [Buf() for _ in range(2)]
        odd2_bufs = [b_I] + b_ikT + [b_BTn] + b_PT + b_PM
        b_lc = Buf()
        glr_ext = sb("glr_ext", [32, TS], BF16)
        b_glr = Buf()
        ones_row = sb("ones_row", [1, 128], BF16)
        onec = sb("onec", [128, 1], F32)
        triNeg = sb("triNeg", [128, 128], F32)
        maskT = sb("maskT", [128, 4, 128], BF16)
        cf = sb("cf", [128, 128], F32)
        st2 = sb("st2", [128, 16], F32)
        b_st2 = Buf()

        def alias_fence(src, dst):
            tok = {}
            for b in src:
                _merge(tok, b.w)
                _merge(tok, b.r)
            for b in dst:
                _merge(b.r, tok)
        rtmp = [sb(f"rtmp{i}", [128, TS], F32) for i in range(2)]
        b_rtmp = [Buf() for _ in range(2)]
        NW1 = 2
        W1C = 256
        w1s = [sb(f"w1s{i}", [128, NDB, W1C], BF16) for i in range(NW1)]
        b_w1s = [Buf() for _ in range(NW1)]
        NW2 = 3
        W2R = 4
        w2s = [sb(f"w2s{i}", [128, W2R, 512], BF16) for i in range(NW2)]
        b_w2s = [Buf() for _ in range(NW2)]
        w1_rr = [0]
        w2_rr = [0]
        b_hdram = [Buf() for _ in range(NST)]

        epsc = sb("epsc", [128, 1], F32)
        k.op("pool", lambda e: e.memset(epsc[:], EPS), writes=[b_ident])
        k.op("pool", lambda e: e.memset(identf[:], 0.0), writes=[b_ident])
        k.op("pool", lambda e: e.affine_select(
            out=identf[:], in_=identf[:], pattern=[[-1, 128]], compare_op=ALU.not_equal,
            fill=1.0, base=0, channel_multiplier=1), reads=[b_ident], writes=[b_ident])
        k.op("dve", lambda e: e.tensor_copy(out=ident[:], in_=identf[:]),
             reads=[b_ident], writes=[b_ident])

        k.op("pool", lambda e: e.memset(onec[:], 1.0), writes=[b_ident])
        k.op("pool", lambda e: e.memset(ones_row[:], 1.0), writes=[b_ident])
        k.op("pool", lambda e: e.memset(glr_ext[:], 1.0), writes=[b_glr])
        k.op("pool", lambda e: e.memset(triNeg[:], -1.0 / 16.0), writes=[b_ident])
        k.op("pool", lambda e: e.affine_select(
            out=triNeg[:], in_=triNeg[:], pattern=[[1, 128]], compare_op=ALU.is_ge,
            fill=0.0, base=0, channel_multiplier=-1), reads=[b_ident], writes=[b_ident])
        k.op("pool", lambda e: e.memset(cf[:], 1.0), writes=[b_ident])
        k.op("pool", lambda e: e.affine_select(
            out=cf[:], in_=cf[:], pattern=[[1, 128]], compare_op=ALU.is_ge,
            fill=0.0, base=0, channel_multiplier=-1), reads=[b_ident], writes=[b_ident])
        for hh in range(4):
            k.op("dve", lambda e, hh=hh: e.tensor_copy(out=maskT[:, hh, :], in_=cf[:]),
                 reads=[b_ident], writes=[b_ident])
        def load_gain(g_ap):
            k.dma("sp", gB[:], g_ap.partition_broadcast(128), writes=[b_gB], chan="gB")

        def norm_tile(j, dst_T=True):
            k.op("act", lambda e: e.activation(out=hn[:], in_=hs[:, j, :], func=AF.Square,
                                               accum_out=stat[:, 0:1]),
                 reads=[b_hs[j]], writes=[b_hn, b_stat])
            k.op("act", lambda e: e.activation(out=stat[:, 1:2], in_=stat[:, 0:1], func=AF.Sqrt,
                                               bias=epsc[:, 0:1], scale=1.0 / D),
                 reads=[b_stat, b_ident], writes=[b_stat])
            k.op("dve", lambda e: e.reciprocal(out=stat[:, 2:3], in_=stat[:, 1:2]),
                 reads=[b_stat], writes=[b_stat])

        def norm_apply_T(j):
            k.op("dve", lambda e: e.scalar_tensor_tensor(
                out=hn[:], in0=hs[:, j, :], scalar=stat[:, 2:3], in1=gB[:],
                op0=ALU.mult, op1=ALU.mult),
                reads=[b_hs[j], b_stat, b_gB], writes=[b_hn])
            for half in range(2):
                bi = next_bank()
                pt = ps[bi].bitcast(BF16)
                for q in range(8):
                    db = half * 8 + q
                    k.op("pe", lambda e, db=db, q=q, pt=pt: e.transpose(
                        out=pt[:, q * 128:(q + 1) * 128], in_=hn[:, db * 128:(db + 1) * 128],
                        identity=ident[:]),
                        reads=[b_hn, b_ident], writes=[psb[bi]])
                eng = "act" if half == 0 else "dve"
                src = pt[:, :].rearrange("p (q t) -> p q t", q=8)
                dst = hnT[:, half * 8:(half + 1) * 8, j * 128:(j + 1) * 128]
                if eng == "act":
                    k.op("act", lambda e, src=src, dst=dst: e.copy(out=dst, in_=src),
                         reads=[psb[bi]], writes=[b_hnT[j]])
                else:
                    k.op("dve", lambda e, src=src, dst=dst: e.tensor_copy(out=dst, in_=src),
                         reads=[psb[bi]], writes=[b_hnT[j]])

        def ffn_supertile(l):
            NTT = TS // 128
            for c in range(DFF // W1C):
                s = w1_rr[0]
                w1_rr[0] = (s + 1) % NW1
                src = ffn_w1[l, :, c * W1C:(c + 1) * W1C].rearrange("(b p) c -> p b c", p=128)
                cached_load(w1s[s], b_w1s[s], s, ("w1", c), "w1", lambda v: v[:, :, :],
                            lambda s=s, src=src: k.dma("pool", w1s[s][:], src, writes=[b_w1s[s]],
                                                       chan=f"w1_{s}"))
                for fb in range(W1C // 128):
                    ffb = c * (W1C // 128) + fb
                    bi = next_bank()
                    for db in range(NDB):
                        k.op("pe", lambda e, s=s, fb=fb, db=db, bi=bi: e.matmul(
                            ps[bi][:, :], lhsT=w1s[s][:, db, fb * 128:(fb + 1) * 128],
                            rhs=hnT[:, db, :], start=(db == 0), stop=(db == NDB - 1)),
                            reads=[b_w1s[s]] + b_hnT, writes=[psb[bi]])
                    r = ffb % 2
                    k.op("act", lambda e, bi=bi, r=r: e.activation(
                        out=rtmp[r][:], in_=ps[bi][:, :], func=AF.Relu),
                        reads=[psb[bi]], writes=[b_rtmp[r]])
                    k.op("dve", lambda e, r=r, ffb=ffb: e.tensor_tensor(
                        out=actT[:, ffb, :], in0=rtmp[r][:], in1=rtmp[r][:], op=ALU.mult),
                        reads=[b_rtmp[r]], writes=[b_actT[ffb]])
            for dq in range(D // 512):
                banks = [next_bank() for _ in range(NTT)]
                for c in range(NFB // W2R):
                    s = w2_rr[0]
                    w2_rr[0] = (s + 1) % NW2
                    src = ffn_w2[l, c * W2R * 128:(c + 1) * W2R * 128,
                                 dq * 512:(dq + 1) * 512].rearrange("(b p) c -> p b c", p=128)
                    cached_load(w2s[s], b_w2s[s], s, ("w2", dq, c), "w2", lambda v: v[:, :, :],
                                lambda s=s, src=src: k.dma("pool", w2s[s][:], src, writes=[b_w2s[s]],
                                                           chan=f"w2_{s}"))
                    for fr in range(W2R):
                        ffb = c * W2R + fr
                        for j in range(NTT):
                            bi = banks[j]
                            k.op("pe", lambda e, s=s, fr=fr, ffb=ffb, j=j, bi=bi: e.matmul(
                                ps[bi][:, :], lhsT=actT[:, ffb, j * 128:(j + 1) * 128],
                                rhs=w2s[s][:, fr, :], start=(ffb == 0), stop=(ffb == NFB - 1)),
                                reads=[b_w2s[s], b_actT[ffb]], writes=[psb[bi]])
                for j in range(NTT):
                    bi = banks[j]
                    k.op("dve", lambda e, j=j, bi=bi, dq=dq: e.tensor_tensor(
                        out=hs[:, j, dq * 512:(dq + 1) * 512], in0=ps[bi][:, :],
                        in1=hs[:, j, dq * 512:(dq + 1) * 512], op=ALU.add),
                        reads=[psb[bi], b_hs[j]], writes=[b_hs[j]])


        lnsc = sb("lnsc", [128, 1], F32)
        k.op("pool", lambda e: e.memset(lnsc[:], -0.5 * math.log(128.0)), writes=[b_ident])

        def tm_tmp():
            r = tmpf_rr[0]
            tmpf_rr[0] = 1 - r
            return r

        def wchunk(w2d, c0, ncols, key=None):
            s_ = w1_rr[0]
            w1_rr[0] = (s_ + 1) % NW1
            if key is None:
                key = ("in", c0)
            cached_load(w1s[s_], b_w1s[s_], s_, key, "w1", lambda v: v[:, :, 0:ncols],
                        lambda: k.dma("pool", w1s[s_][:, :, 0:ncols],
                                      w2d[:, c0:c0 + ncols].rearrange("(b p) c -> p b c", p=128),
                                      writes=[b_w1s[s_]], chan=f"w1_{s_}"))
            return s_

        def proj_fm(w2d, c0, ncols, evac):
            s_ = wchunk(w2d, c0, ncols)
            nb = (ncols + 127) // 128
            for fb in range(nb):
                m = min(128, ncols - fb * 128)
                bi = next_bank()
                for db in range(NDB):
                    k.op("pe", lambda e, s_=s_, fb=fb, db=db, bi=bi, m=m: e.matmul(
                        ps[bi][0:m, :], lhsT=w1s[s_][:, db, fb * 128:fb * 128 + m],
                        rhs=hnT[:, db, :], start=(db == 0), stop=(db == NDB - 1)),
                        reads=[b_w1s[s_]] + b_hnT, writes=[psb[bi]])
                evac(fb, bi)

        def proj_tm(w2d, c0, evac):
            s_ = wchunk(w2d, c0, 256)
            for j in range(4):
                bi = next_bank()
                for db in range(NDB):
                    k.op("pe", lambda e, s_=s_, j=j, db=db, bi=bi: e.matmul(
                        ps[bi][:, 0:256], lhsT=hnT[:, db, j * 128:(j + 1) * 128],
                        rhs=w1s[s_][:, db, 0:256], start=(db == 0), stop=(db == NDB - 1)),
                        reads=[b_w1s[s_], b_hnT[j]], writes=[psb[bi]])
                evac(j, bi)

        def even_consts(i):
            k.dma("pool", W2ext[0:16, :], b_gate_w2[i], writes=[b_lc], chan="lc")
            k.dma("pool", W2ext[16:17, :], b_gate_b[i], writes=[b_lc], chan="lc")
            k.dma("sp", lnB[:], a_v_ln_g[i].partition_broadcast(128), writes=[b_lc], chan="lcs")
            k.dma("sp", ongB[:], b_out_norm_g[i].partition_broadcast(128), writes=[b_lc], chan="lcs")
            k.dma("pool", bs_row[:], a_b_s[i], writes=[b_lc], chan="lc")
            for half in range(2):
                k.dma("sp", rtmp[half][:].rearrange("p (g s) -> p g s", g=4),
                      a_w_s[i, half * 4:(half + 1) * 4].rearrange("g t s -> t g s"),
                      writes=[b_rtmp[half]], chan=f"rt{half}")
                bi = next_bank()
                for gq in range(4):
                    k.op("pe", lambda e, half=half, gq=gq, bi=bi: e.transpose(
                        out=ps[bi][:, gq * 128:(gq + 1) * 128],
                        in_=rtmp[half][:, gq * 128:(gq + 1) * 128], identity=identf[:]),
                        reads=[b_rtmp[half], b_ident], writes=[psb[bi]])
                r_ = tm_tmp()
                k.op("act", lambda e, r_=r_, bi=bi: e.copy(out=tmpf[r_][:], in_=ps[bi][:, :]),
                     reads=[psb[bi]], writes=[b_tmpf[r_]])
                v3 = tmpf[r_][:].rearrange("p (g t) -> p g t", g=4)
                k.op("pool", lambda e, v3=v3: e.affine_select(
                    out=v3, in_=v3, pattern=[[0, 4], [1, 128]], compare_op=ALU.is_ge,
                    fill=0.0, base=0, channel_multiplier=-1),
                    reads=[b_tmpf[r_]], writes=[b_tmpf[r_]])
                k.op("dve", lambda e, v3=v3, half=half: e.tensor_copy(
                    out=WT[:, half * 4:(half + 1) * 4, :], in_=v3),
                    reads=[b_tmpf[r_]], writes=[b_lc])
            k.op("pool", lambda e: e.memset(Sst[:], 0.0), writes=b_S)
            k.op("pool", lambda e: e.memset(Sbf[:], 0.0), writes=b_Sbf)

        def even_mixer(i):
            w_in = ab_w_in[i]
            w_out = ab_w_out[i]
            alias_fence(b_actT, mix_bufs)
            def ev_glr(fb, bi):
                k.op("act", lambda e, bi=bi: e.copy(out=glr_ext[0:16, :], in_=ps[bi][0:16, :]),
                     reads=[psb[bi]], writes=[b_glr])
            proj_fm(w_in, 5120, 16, ev_glr)
            cumb = [reserve_bank() for _ in range(4)]
            for j in range(4):
                bg = next_bank()
                k.op("pe", lambda e, j=j, bg=bg: e.matmul(
                    ps[bg][:, :], lhsT=glr_ext[0:17, j * 128:(j + 1) * 128], rhs=W2ext[0:17, :],
                    start=True, stop=True), reads=[b_glr, b_lc], writes=[psb[bg]])
                sj = j % 2
                k.op("act", lambda e, sj=sj, bg=bg: e.activation(
                    out=spt[sj][:], in_=ps[bg][:, :], func=AF.Exp, scale=-1.0),
                    reads=[psb[bg]], writes=[b_spt[sj]])
                k.op("act", lambda e, sj=sj: e.activation(
                    out=spt[sj][:], in_=spt[sj][:], func=AF.Ln, bias=onec[:, 0:1], scale=1.0),
                    reads=[b_spt[sj], b_ident], writes=[b_spt[sj]])
                for hh in range(4):
                    k.op("pe", lambda e, sj=sj, hh=hh, j=j: e.matmul(
                        ps[cumb[hh]][:, j * 128:(j + 1) * 128],
                        lhsT=spt[sj][:, hh * 128:(hh + 1) * 128], rhs=triNeg[:, :],
                        start=True, stop=True),
                        reads=[b_spt[sj], b_ident], writes=[psb[cumb[hh]]])
            for hh in range(4):
                cb = cumb[hh]
                k.op("act", lambda e, hh=hh, cb=cb: e.activation(
                    out=E1[:, hh, :], in_=ps[cb][:, :], func=AF.Exp, bias=lnsc[:, 0:1], scale=1.0),
                    reads=[psb[cb], b_ident], writes=[b_E[hh]])
                k.op("act", lambda e, hh=hh, cb=cb: e.activation(
                    out=E2[:, hh, :], in_=ps[cb][:, :], func=AF.Exp, scale=-1.0),
                    reads=[psb[cb]], writes=[b_E[hh]])
                k.op("act", lambda e, hh=hh, cb=cb: e.activation(
                    out=explast[:, hh, :], in_=ps[cb][:, 127:512:128], func=AF.Exp),
                    reads=[psb[cb]], writes=[b_E[hh]])
                reserved.discard(cb)
            for c in range(2):
                def ev_q(fb, bi, c=c):
                    hh = c * 2 + fb
                    k.op("dve", lambda e, hh=hh, bi=bi: e.tensor_tensor(
                        out=qe[:, hh, :], in0=ps[bi][:, :], in1=E1[:, hh, :], op=ALU.mult),
                        reads=[psb[bi], b_E[hh]], writes=[b_qe[hh]])
                proj_fm(w_in, 2048 + c * 256, 256, ev_q)
            for c in range(2):
                def ev_k(fb, bi, c=c):
                    hh = c * 2 + fb
                    k.op("dve", lambda e, hh=hh, bi=bi: e.tensor_tensor(
                        out=ke[:, hh, :], in0=ps[bi][:, :], in1=E2[:, hh, :], op=ALU.mult),
                        reads=[psb[bi], b_E[hh]], writes=[b_ke[hh]])
                    for j in range(4):
                        k.op("dve", lambda e, hh=hh, bi=bi, j=j: e.scalar_tensor_tensor(
                            out=ks[:, hh, j * 128:(j + 1) * 128], in0=ps[bi][:, j * 128:(j + 1) * 128],
                            scalar=explast[:, hh, j:j + 1], in1=E2[:, hh, j * 128:(j + 1) * 128],
                            op0=ALU.mult, op1=ALU.mult),
                            reads=[psb[bi], b_E[hh]], writes=[b_ks[hh]])
                    bt = next_bank()
                    pt = ps[bt].bitcast(BF16)
                    for j in range(4):
                        k.op("pe", lambda e, hh=hh, j=j, pt=pt: e.transpose(
                            out=pt[:, j * 128:(j + 1) * 128], in_=ks[:, hh, j * 128:(j + 1) * 128],
                            identity=ident[:]), reads=[b_ks[hh], b_ident], writes=[psb[bt]])
                    k.op("act", lambda e, hh=hh, pt=pt: e.copy(
                        out=ksT[:, hh, :, :], in_=pt[:, 0:512].rearrange("p (j d) -> p j d", j=4)),
                        reads=[psb[bt]], writes=[b_ksT[hh]])
                proj_fm(w_in, 2560 + c * 256, 256, ev_k)
            for c in range(4):
                def ev_u(fb, bi, c=c):
                    g = c * 2 + fb
                    k.op("act", lambda e, g=g, bi=bi: e.activation(
                        out=a_uT[:, g, :], in_=ps[bi][:, :], func=AF.Gelu),
                        reads=[psb[bi]], writes=[b_auT[g]])
                proj_fm(w_in, c * 256, 256, ev_u)
            for c in range(4):
                def ev_v(j, bi, c=c):
                    for gg in range(2):
                        g = c * 2 + gg
                        r_ = tm_tmp()
                        k.op("act", lambda e, r_=r_, bi=bi, gg=gg: e.activation(
                            out=tmpf[r_][:, 0:128], in_=ps[bi][:, gg * 128:(gg + 1) * 128],
                            func=AF.Gelu, accum_out=st2[:, 0:1]),
                            reads=[psb[bi]], writes=[b_tmpf[r_], b_st2])
                        k.op("act", lambda e, r_=r_: e.activation(
                            out=junk[:, 0:128], in_=tmpf[r_][:, 0:128], func=AF.Square,
                            accum_out=st2[:, 1:2]),
                            reads=[b_tmpf[r_]], writes=[b_junk, b_st2])
                        k.op("dve", lambda e: e.tensor_scalar(
                            out=st2[:, 2:3], in0=st2[:, 0:1], scalar1=1.0 / 128, scalar2=None,
                            op0=ALU.mult), reads=[b_st2], writes=[b_st2])
                        k.op("dve", lambda e: e.tensor_tensor(
                            out=st2[:, 3:4], in0=st2[:, 2:3], in1=st2[:, 2:3], op=ALU.mult),
                            reads=[b_st2], writes=[b_st2])
                        k.op("dve", lambda e: e.scalar_tensor_tensor(
                            out=st2[:, 4:5], in0=st2[:, 1:2], scalar=1.0 / 128, in1=st2[:, 3:4],
                            op0=ALU.mult, op1=ALU.subtract), reads=[b_st2], writes=[b_st2])
                        k.op("act", lambda e: e.activation(
                            out=st2[:, 5:6], in_=st2[:, 4:5], func=AF.Sqrt, bias=epsc[:, 0:1],
                            scale=1.0), reads=[b_st2, b_ident], writes=[b_st2])
                        k.op("dve", lambda e: e.reciprocal(out=st2[:, 6:7], in_=st2[:, 5:6]),
                             reads=[b_st2], writes=[b_st2])
                        k.op("dve", lambda e, r_=r_: e.tensor_scalar(
                            out=tmpf[r_][:, 0:128], in0=tmpf[r_][:, 0:128], scalar1=st2[:, 2:3],
                            scalar2=st2[:, 6:7], op0=ALU.subtract, op1=ALU.mult),
                            reads=[b_tmpf[r_], b_st2], writes=[b_tmpf[r_]])
                        k.op("dve", lambda e, r_=r_, g=g, j=j: e.tensor_tensor(
                            out=v_sgu[:, j, g * 128:(g + 1) * 128], in0=tmpf[r_][:, 0:128],
                            in1=lnB[:, g * 128:(g + 1) * 128], op=ALU.mult),
                            reads=[b_tmpf[r_], b_lc], writes=[b_vsgu[j]])
                proj_tm(w_in, 1024 + c * 256, ev_v)
            for c in range(4):
                def ev_vg(j, bi, c=c):
                    k.op("act", lambda e, j=j, bi=bi, c=c: e.copy(
                        out=v_gla[:, j, c * 256:(c + 1) * 256], in_=ps[bi][:, 0:256]),
                        reads=[psb[bi]], writes=[b_vgla[j]])
                proj_tm(w_in, 3072 + c * 256, ev_vg)
            for c in range(4):
                def ev_r(j, bi, c=c):
                    k.op("act", lambda e, j=j, bi=bi, c=c: e.activation(
                        out=r_s[:, j, c * 256:(c + 1) * 256], in_=ps[bi][:, 0:256], func=AF.Silu),
                        reads=[psb[bi]], writes=[b_rs[j]])
                proj_tm(w_in, 4096 + c * 256, ev_r)
            for g in range(8):
                bi = next_bank()
                for j in range(4):
                    k.op("pe", lambda e, g=g, j=j, bi=bi: e.matmul(
                        ps[bi][:, j * 128:(j + 1) * 128], lhsT=v_sgu[:, j, g * 128:(g + 1) * 128],
                        rhs=WT[:, g, :], start=True, stop=False),
                        reads=[b_vsgu[j], b_lc], writes=[psb[bi]])
                    k.op("pe", lambda e, g=g, j=j, bi=bi: e.matmul(
                        ps[bi][:, j * 128:(j + 1) * 128], lhsT=ones_row[0:1, :],
                        rhs=bs_row[0:1, g * 128:(g + 1) * 128], start=False, stop=True),
                        reads=[b_ident, b_lc], writes=[psb[bi]])
                k.op("dve", lambda e, g=g, bi=bi: e.tensor_tensor(
                    out=catT[:, g, :], in0=ps[bi][:, :], in1=a_uT[:, g, :], op=ALU.mult),
                    reads=[psb[bi], b_auT[g]], writes=[b_catT[g]])
            for j in range(4):
                jc = slice(j * 128, (j + 1) * 128)
                bs_ = next_bank()
                for hh in range(4):
                    k.op("pe", lambda e, hh=hh, jc=jc, bs_=bs_: e.matmul(
                        ps[bs_][:, hh * 128:(hh + 1) * 128], lhsT=ke[:, hh, jc], rhs=qe[:, hh, jc],
                        start=True, stop=True), reads=[b_ke[hh], b_qe[hh]], writes=[psb[bs_]])
                k.op("dve", lambda e, bs_=bs_: e.tensor_tensor(
                    out=scTm[:, :, :].rearrange("p h t -> p (h t)"), in0=ps[bs_][:, :],
                    in1=maskT[:, :, :].rearrange("p h t -> p (h t)"), op=ALU.mult),
                    reads=[psb[bs_], b_ident], writes=[b_scTm])
                for hp in range(2):
                    bo = next_bank()
                    for hq in range(2):
                        hh = hp * 2 + hq
                        oc = slice(hq * 256, (hq + 1) * 256)
                        vc = slice(hh * 256, (hh + 1) * 256)
                        k.op("pe", lambda e, hh=hh, oc=oc, vc=vc, bo=bo, j=j: e.matmul(
                            ps[bo][:, oc], lhsT=scTm[:, hh, :], rhs=v_gla[:, j, vc],
                            start=True, stop=False),
                            reads=[b_scTm, b_vgla[j]], writes=[psb[bo]])
                        k.op("pe", lambda e, hh=hh, oc=oc, bo=bo, jc=jc: e.matmul(
                            ps[bo][:, oc], lhsT=qe[:, hh, jc], rhs=Sbf[:, hh, :],
                            start=False, stop=True),
                            reads=[b_qe[hh], b_Sbf[hh]], writes=[psb[bo]])
                    for hq in range(2):
                        hh = hp * 2 + hq
                        oc = slice(hq * 256, (hq + 1) * 256)
                        vc = slice(hh * 256, (hh + 1) * 256)
                        k.op("act", lambda e, oc=oc, bo=bo: e.activation(
                            out=junk[:, 0:256], in_=ps[bo][:, oc], func=AF.Square,
                            accum_out=st2[:, 8:9]), reads=[psb[bo]], writes=[b_junk, b_st2])
                        k.op("act", lambda e: e.activation(
                            out=st2[:, 9:10], in_=st2[:, 8:9], func=AF.Sqrt, bias=epsc[:, 0:1],
                            scale=1.0 / 256), reads=[b_st2, b_ident], writes=[b_st2])
                        k.op("dve", lambda e: e.reciprocal(out=st2[:, 10:11], in_=st2[:, 9:10]),
                             reads=[b_st2], writes=[b_st2])
                        r_ = tm_tmp()
                        k.op("dve", lambda e, r_=r_, oc=oc, bo=bo: e.scalar_tensor_tensor(
                            out=tmpf[r_][:, 0:256], in0=ps[bo][:, oc], scalar=st2[:, 10:11],
                            in1=ongB[:, :], op0=ALU.mult, op1=ALU.mult),
                            reads=[psb[bo], b_st2, b_lc], writes=[b_tmpf[r_]])
                        k.op("dve", lambda e, r_=r_, vc=vc, j=j: e.tensor_tensor(
                            out=bout[:, vc], in0=tmpf[r_][:, 0:256], in1=r_s[:, j, vc], op=ALU.mult),
                            reads=[b_tmpf[r_], b_rs[j]], writes=[b_bout])
                for hp in range(2):
                    bk = next_bank()
                    for hq in range(2):
                        hh = hp * 2 + hq
                        oc = slice(hq * 256, (hq + 1) * 256)
                        vc = slice(hh * 256, (hh + 1) * 256)
                        k.op("pe", lambda e, hh=hh, oc=oc, vc=vc, bk=bk, j=j: e.matmul(
                            ps[bk][:, oc], lhsT=ksT[:, hh, j, :], rhs=v_gla[:, j, vc],
                            start=True, stop=True),
                            reads=[b_ksT[hh], b_vgla[j]], writes=[psb[bk]])
                        k.op("dve", lambda e, hh=hh, oc=oc, bk=bk, j=j: e.scalar_tensor_tensor(
                            out=Sst[:, hh, :], in0=Sst[:, hh, :], scalar=explast[:, hh, j:j + 1],
                            in1=ps[bk][:, oc], op0=ALU.mult, op1=ALU.add),
                            reads=[psb[bk], b_S[hh], b_E[hh]], writes=[b_S[hh]])
                        k.op("act", lambda e, hh=hh: e.copy(out=Sbf[:, hh, :], in_=Sst[:, hh, :]),
                             reads=[b_S[hh]], writes=[b_Sbf[hh]])
                bt = next_bank()
                pt = ps[bt].bitcast(BF16)
                for q8 in range(8):
                    k.op("pe", lambda e, q8=q8, pt=pt: e.transpose(
                        out=pt[:, q8 * 128:(q8 + 1) * 128], in_=bout[:, q8 * 128:(q8 + 1) * 128],
                        identity=ident[:]), reads=[b_bout, b_ident], writes=[psb[bt]])
                k.op("act", lambda e, pt=pt, jc=jc: e.copy(
                    out=catT[:, 8:16, jc], in_=pt[:, :].rearrange("p (q t) -> p q t", q=8)),
                    reads=[psb[bt]], writes=b_catT[8:16])
            for cq in range(8):
                s_ = wchunk(w_out, cq * 256, 256, key=("out", cq))
                for j in range(4):
                    bi = next_bank()
                    for cb in range(16):
                        k.op("pe", lambda e, s_=s_, j=j, cb=cb, bi=bi: e.matmul(
                            ps[bi][:, 0:256], lhsT=catT[:, cb, j * 128:(j + 1) * 128],
                            rhs=w1s[s_][:, cb, 0:256], start=(cb == 0), stop=(cb == 15)),
                            reads=[b_w1s[s_], b_catT[cb]], writes=[psb[bi]])
                    k.op("dve", lambda e, j=j, bi=bi, cq=cq: e.tensor_tensor(
                        out=hs[:, j, cq * 256:(cq + 1) * 256], in0=ps[bi][:, 0:256],
                        in1=hs[:, j, cq * 256:(cq + 1) * 256], op=ALU.add),
                        reads=[psb[bi], b_hs[j]], writes=[b_hs[j]])
            alias_fence(mix_bufs, b_actT)


        qT = carve(0, 8192, "p (h t) -> p h t", h=16)
        iqT = carve(8192, 4096, "p (b t) -> p b t", b=8)
        selT = carve(12288, 16384, "p (k t) -> p k t", k=32)
        vn = carve(28672, 2048, "p (j c) -> p j c", j=4)
        kTn = carve(30720, 2048, "p (g t) -> p g t", g=4)
        b_qT = [Buf() for _ in range(16)]
        b_iqT = [Buf() for _ in range(8)]
        b_selT = [Buf() for _ in range(4)]
        b_vn = [Buf() for _ in range(4)]
        b_kTn = [Buf() for _ in range(4)]
        odd_bufs = b_qT + b_iqT + b_selT + b_vn + b_kTn
        iw_t = sb("iw_t", [128, 4, 16], F32)
        b_iw = [Buf() for _ in range(4)]
        thr = sb("thr", [128, 16], F32)
        b_thr = Buf()
        halfc = sb("halfc", [128, 1], F32)
        negtri = sb("negtri", [128, 128], F32)
        rb = sb("rb", [32, 16], F32)
        OHs = sb("OHs", [32, 384], F32)
        cfarB = sb("cfarB", [128, 16], F32)
        ones128 = sb("ones128", [128, 128], BF16)
        fsb = sb("fsb", [16, 384], F32)
        selq = sb("selq", [128, 512], BF16)
        b_selq = Buf()
        b_oc = Buf()
        b_kvd = [Buf() for _ in range(NST)]
        b_fpad = Buf()
        has_odd = any(l % 2 == 1 for l in layers) and "mix" in parts
        if has_odd:
            k.op("pool", lambda e: e.memset(halfc[:], 0.5), writes=[b_oc])
            k.op("pool", lambda e: e.memset(ones128[:], 1.0), writes=[b_oc])
            k.op("pool", lambda e: e.memset(negtri[:], 0.0), writes=[b_oc])
            k.op("pool", lambda e: e.affine_select(
                out=negtri[:], in_=negtri[:], pattern=[[-1, 128]], compare_op=ALU.is_ge,
                fill=-1.0e30, base=0, channel_multiplier=1), reads=[b_oc], writes=[b_oc])
            k.dma("sp", rb[:], rel_bias, writes=[b_oc], chan="oc")
            k.dma("sp", OHs[:], oh_pad, writes=[b_oc], chan="oc")
            k.dma("sp", cfarB[:], rel_bias[31:32, :].partition_broadcast(128), writes=[b_oc], chan="oc")
            bi = next_bank()
            k.op("pe", lambda e, bi=bi: e.matmul(ps[bi][0:16, 0:384], lhsT=rb[:, :], rhs=OHs[:, :],
                                                 start=True, stop=True),
                 reads=[b_oc], writes=[psb[bi]])
            k.op("act", lambda e, bi=bi: e.copy(out=fsb[:], in_=ps[bi][0:16, 0:384]),
                 reads=[psb[bi]], writes=[b_oc])
            k.dma("sp", fpad_d, fsb[:], reads=[b_oc], writes=[b_fpad], chan="oc2")

        def odd_consts(i):
            alias_fence(even2_bufs + [b_lc], odd2_bufs)
            k.op("pool", lambda e: e.memset(cf[:], 0.0), writes=[b_ident])
            k.op("pool", lambda e: e.affine_select(
                out=cf[:], in_=cf[:], pattern=[[1, 128]], compare_op=ALU.not_equal,
                fill=1.0, base=-127, channel_multiplier=1), reads=[b_ident], writes=[b_ident])
            for hd in range(16):
                r_ = hd % 2
                src = bass.AP(tensor=fpad_d.tensor, offset=fpad_d[hd:hd + 1, :].offset,
                              ap=[[1, 128], [1, 256]])
                k.dma("sp", rtmp[r_][:, 0:256], src, reads=[b_fpad], writes=[b_rtmp[r_]],
                      chan=f"rt{r_}")
                bi = next_bank()
                k.op("pe", lambda e, r_=r_, bi=bi: e.matmul(
                    ps[bi][:, 0:256], lhsT=cf[:, :], rhs=rtmp[r_][:, 0:256], start=True, stop=True),
                    reads=[b_rtmp[r_], b_ident], writes=[psb[bi]])
                k.op("dve", lambda e, hd=hd, bi=bi: e.tensor_scalar(
                    out=BTn[:, hd, :], in0=ps[bi][:, 0:256], scalar1=cfarB[:, hd:hd + 1],
                    scalar2=None, op0=ALU.subtract),
                    reads=[psb[bi], b_oc], writes=[b_BTn])

        def odd_mixer(i, st):
            w_in = c_w_in[i]
            w_out = c_w_out[i]
            t0 = st * TS
            alias_fence(b_actT, odd_bufs)
            for c in range(8):
                def ev_q(fb, bi, c=c):
                    hd = c * 2 + fb
                    k.op("act", lambda e, hd=hd, bi=bi: e.activation(
                        out=qT[:, hd, :], in_=ps[bi][:, :], func=AF.Copy, scale=128.0 ** -0.5),
                        reads=[psb[bi]], writes=[b_qT[hd]])
                proj_fm(w_in, c * 256, 256, ev_q)
            for c in range(2):
                def ev_k(fb, bi, c=c):
                    g = c * 2 + fb
                    k.op("act", lambda e, g=g, bi=bi: e.copy(out=kTn[:, g, :], in_=ps[bi][:, :]),
                         reads=[psb[bi]], writes=[b_kTn[g]])
                    k.dma("sp", kT_d[g, :, t0:t0 + TS], kTn[:, g, :], reads=[b_kTn[g]],
                          writes=[b_kvd[st]], chan=f"kvw{g}")
                proj_fm(w_in, 2048 + c * 256, 256, ev_k)
            for c in range(4):
                def ev_iq(fb, bi, c=c):
                    blk = c * 2 + fb
                    k.op("act", lambda e, blk=blk, bi=bi: e.copy(out=iqT[:, blk, :], in_=ps[bi][:, :]),
                         reads=[psb[bi]], writes=[b_iqT[blk]])
                proj_fm(w_in, 3072 + c * 256, 256, ev_iq)
            s_ = w1_rr[0]
            w1_rr[0] = (s_ + 1) % NW1
            def ik_loads(s_=s_):
                for dup in range(2):
                    k.dma("pool", w1s[s_][:, :, dup * 64:(dup + 1) * 64],
                          w_in[:, 4096:4160].rearrange("(b p) c -> p b c", p=128),
                          writes=[b_w1s[s_]], chan=f"w1_{s_}")
            cached_load(w1s[s_], b_w1s[s_], s_, ("ik",), "w1", lambda v: v[:, :, 0:128], ik_loads)
            bi = next_bank()
            for db in range(NDB):
                k.op("pe", lambda e, s_=s_, db=db, bi=bi: e.matmul(
                    ps[bi][:, :], lhsT=w1s[s_][:, db, 0:128], rhs=hnT[:, db, :],
                    start=(db == 0), stop=(db == NDB - 1)),
                    reads=[b_w1s[s_]] + b_hnT, writes=[psb[bi]])
            k.op("act", lambda e, bi=bi: e.copy(out=ikT[:, t0:t0 + TS], in_=ps[bi][:, :]),
                 reads=[psb[bi]], writes=[b_ikT[st]])
            for c in range(2):
                def ev_v(j, bi, c=c):
                    k.op("act", lambda e, j=j, bi=bi, c=c: e.copy(
                        out=vn[:, j, c * 256:(c + 1) * 256], in_=ps[bi][:, 0:256]),
                        reads=[psb[bi]], writes=[b_vn[j]])
                    if c == 1:
                        k.dma("sp", v_d[t0 + j * 128:t0 + (j + 1) * 128, :], vn[:, j, :],
                              reads=[b_vn[j]], writes=[b_kvd[st]], chan=f"kvw{j}")
                proj_tm(w_in, 2560 + c * 256, ev_v)
            s_ = wchunk(w_in, 4160, 16)
            for j in range(4):
                bi = next_bank()
                for db in range(NDB):
                    k.op("pe", lambda e, s_=s_, j=j, db=db, bi=bi: e.matmul(
                        ps[bi][:, 0:16], lhsT=hnT[:, db, j * 128:(j + 1) * 128],
                        rhs=w1s[s_][:, db, 0:16], start=(db == 0), stop=(db == NDB - 1)),
                        reads=[b_w1s[s_], b_hnT[j]], writes=[psb[bi]])
                k.op("act", lambda e, j=j, bi=bi: e.copy(out=iw_t[:, j, :], in_=ps[bi][:, 0:16]),
                     reads=[psb[bi]], writes=[b_iw[j]])
            for j in range(4):
                qb = st * 4 + j
                nk = (qb + 1) * 128
                nch = (nk + 511) // 512
                for h in range(16):
                    blk, po = h // 2, (h % 2) * 64
                    for c in range(nch):
                        ncol = min(512, nk - c * 512)
                        bi = next_bank()
                        k.op("pe", lambda e, blk=blk, po=po, j=j, c=c, ncol=ncol, bi=bi: e.matmul(
                            ps[bi][:, 0:ncol], lhsT=iqT[po:po + 64, blk, j * 128:(j + 1) * 128],
                            rhs=ikT[po:po + 64, c * 512:c * 512 + ncol], start=True, stop=True),
                            reads=[b_iqT[blk], b_ikT[c]], writes=[psb[bi]])
                        r_ = (h * nch + c) % 2
                        k.op("act", lambda e, r_=r_, ncol=ncol, bi=bi: e.activation(
                            out=rtmp[r_][:, 0:ncol], in_=ps[bi][:, 0:ncol], func=AF.Relu),
                            reads=[psb[bi]], writes=[b_rtmp[r_]])
                        cs = slice(c * 512, c * 512 + ncol)
                        if h == 0:
                            k.op("dve", lambda e, r_=r_, ncol=ncol, cs=cs, j=j: e.tensor_scalar(
                                out=Iacc[:, cs], in0=rtmp[r_][:, 0:ncol], scalar1=iw_t[:, j, 0:1],
                                scalar2=None, op0=ALU.mult),
                                reads=[b_rtmp[r_], b_iw[j]], writes=[b_I])
                        else:
                            k.op("dve", lambda e, r_=r_, ncol=ncol, cs=cs, j=j, h=h: e.scalar_tensor_tensor(
                                out=Iacc[:, cs], in0=rtmp[r_][:, 0:ncol], scalar=iw_t[:, j, h:h + 1],
                                in1=Iacc[:, cs], op0=ALU.mult, op1=ALU.add),
                                reads=[b_rtmp[r_], b_iw[j], b_I], writes=[b_I])
                k.op("dve", lambda e, nk=nk: e.tensor_reduce(out=thr[:, 1:2], in_=Iacc[:, 0:nk],
                                                            axis=AX.X, op=ALU.max),
                     reads=[b_I], writes=[b_thr])
                k.op("dve", lambda e, nk=nk: e.tensor_reduce(out=thr[:, 0:1], in_=Iacc[:, 0:nk],
                                                            axis=AX.X, op=ALU.min),
                     reads=[b_I], writes=[b_thr])
                k.op("dve", lambda e, qb=qb: e.tensor_tensor(
                    out=Iacc[:, qb * 128:(qb + 1) * 128], in0=Iacc[:, qb * 128:(qb + 1) * 128],
                    in1=negtri[:, :], op=ALU.add), reads=[b_I, b_oc], writes=[b_I])
                if nk > 256:
                    for it in range(22):
                        k.op("dve", lambda e: e.scalar_tensor_tensor(
                            out=thr[:, 2:3], in0=thr[:, 0:1], scalar=thr[:, 1:2], in1=halfc[:, 0:1],
                            op0=ALU.add, op1=ALU.mult), reads=[b_thr, b_oc], writes=[b_thr])
                        n0 = min(nk, 2048)
                        k.op("dve", lambda e, n0=n0: e.tensor_scalar(
                            out=hn[:, 0:n0], in0=Iacc[:, 0:n0], scalar1=thr[:, 2:3], scalar2=None,
                            op0=ALU.is_ge, op1=ALU.add, accum_out=thr[:, 3:4]),
                            reads=[b_I, b_thr], writes=[b_hn, b_thr])
                        if nk > 2048:
                            k.op("dve", lambda e, nk=nk: e.tensor_scalar(
                                out=hn[:, 0:nk - 2048], in0=Iacc[:, 2048:nk], scalar1=thr[:, 2:3],
                                scalar2=None, op0=ALU.is_ge, op1=ALU.add, accum_out=thr[:, 4:5]),
                                reads=[b_I, b_thr], writes=[b_hn, b_thr])
                            k.op("dve", lambda e: e.tensor_tensor(
                                out=thr[:, 3:4], in0=thr[:, 3:4], in1=thr[:, 4:5], op=ALU.add),
                                reads=[b_thr], writes=[b_thr])
                        k.op("dve", lambda e: e.tensor_scalar(
                            out=thr[:, 5:6], in0=thr[:, 3:4], scalar1=255.5, scalar2=None,
                            op0=ALU.is_ge), reads=[b_thr], writes=[b_thr])
                        k.op("dve", lambda e: e.tensor_tensor(
                            out=thr[:, 6:7], in0=thr[:, 2:3], in1=thr[:, 0:1], op=ALU.subtract),
                            reads=[b_thr], writes=[b_thr])
                        k.op("dve", lambda e: e.tensor_tensor(
                            out=thr[:, 7:8], in0=thr[:, 1:2], in1=thr[:, 2:3], op=ALU.subtract),
                            reads=[b_thr], writes=[b_thr])
                        k.op("dve", lambda e: e.scalar_tensor_tensor(
                            out=thr[:, 0:1], in0=thr[:, 6:7], scalar=thr[:, 5:6], in1=thr[:, 0:1],
                            op0=ALU.mult, op1=ALU.add), reads=[b_thr], writes=[b_thr])
                        k.op("dve", lambda e: e.scalar_tensor_tensor(
                            out=thr[:, 1:2], in0=thr[:, 7:8], scalar=thr[:, 5:6], in1=thr[:, 2:3],
                            op0=ALU.mult, op1=ALU.add), reads=[b_thr], writes=[b_thr])
                for c in range(nch):
                    ncol = min(512, nk - c * 512)
                    nb = ncol // 128
                    k.op("dve", lambda e, c=c, ncol=ncol: e.tensor_scalar(
                        out=selq[:, 0:ncol], in0=Iacc[:, c * 512:c * 512 + ncol], scalar1=thr[:, 0:1],
                        scalar2=None, op0=ALU.is_ge), reads=[b_I, b_thr], writes=[b_selq])
                    bt = next_bank()
                    pt = ps[bt].bitcast(BF16)
                    for q in range(nb):
                        k.op("pe", lambda e, q=q, pt=pt: e.transpose(
                            out=pt[:, q * 128:(q + 1) * 128], in_=selq[:, q * 128:(q + 1) * 128],
                            identity=ident[:]), reads=[b_selq, b_ident], writes=[psb[bt]])
                    k.op("act", lambda e, c=c, nb=nb, pt=pt, j=j: e.copy(
                        out=selT[:, c * 4:c * 4 + nb, j * 128:(j + 1) * 128],
                        in_=pt[:, 0:nb * 128].rearrange("p (q t) -> p q t", q=nb)),
                        reads=[psb[bt]], writes=[b_selT[j]])
            for hd in range(16):
                g = hd // 4
                bo = reserve_bank()
                bl = reserve_bank()
                nkb = st * 4 + 4
                for c in range(st + 1):
                    s_ = w2_rr[0]
                    w2_rr[0] = (s_ + 1) % NW2
                    kslot = w2s[s_][:, 0:1, :].rearrange("p a c -> p (a c)")
                    vslot = w2s[s_][:, 1:2, :].rearrange("p a (q v) -> p (a q) v", q=4)
                    k.dma("sp", kslot, kT_d[g, :, c * 512:(c + 1) * 512], reads=[b_kvd[c]],
                          writes=[b_w2s[s_]], chan=f"kv{s_}")
                    k.dma("sp", vslot, v_d[c * 512:(c + 1) * 512, g * 128:(g + 1) * 128].rearrange(
                        "(q p) v -> p q v", p=128), reads=[b_kvd[c]], writes=[b_w2s[s_]], chan=f"kv{s_}")
                    for kq in range(4):
                        kb = c * 4 + kq
                        col0 = kq * 128 if c == st else 0
                        ncols = 512 - col0
                        near = kb >= 4 * st - 1
                        bi = next_bank()
                        k.op("pe", lambda e, kslot=kslot, kq=kq, hd=hd, col0=col0, ncols=ncols, bi=bi, near=near: e.matmul(
                            ps[bi][:, 0:ncols], lhsT=kslot[:, kq * 128:(kq + 1) * 128],
                            rhs=qT[:, hd, col0:512], start=True, stop=(not near)),
                            reads=[b_w2s[s_], b_qT[hd]], writes=[psb[bi]])
                        if near:
                            if c == st:
                                off, nbc = 0, min(256, ncols)
                            else:
                                off, nbc = 128, 128
                            k.op("pe", lambda e, hd=hd, off=off, nbc=nbc, bi=bi: e.matmul(
                                ps[bi][:, 0:nbc], lhsT=ident[:, :], rhs=BTn[:, hd, off:off + nbc],
                                start=False, stop=True),
                                reads=[b_BTn, b_ident], writes=[psb[bi]])
                        pr = kb % 2
                        k.op("act", lambda e, pr=pr, ncols=ncols, bi=bi, hd=hd: e.activation(
                            out=PTb[pr][:, 0:ncols], in_=ps[bi][:, 0:ncols], func=AF.Exp,
                            bias=cfarB[:, hd:hd + 1], scale=1.0),
                            reads=[psb[bi], b_oc], writes=[b_PT[pr]])
                        k.op("dve", lambda e, pr=pr, ncols=ncols, kb=kb, col0=col0: e.tensor_tensor(
                            out=PMb[pr][:, 0:ncols], in0=PTb[pr][:, 0:ncols], in1=selT[:, kb, col0:512],
                            op=ALU.mult), reads=[b_PT[pr]] + b_selT, writes=[b_PM[pr]])
                        k.op("pe", lambda e, vslot=vslot, kq=kq, pr=pr, col0=col0, ncols=ncols, kb=kb, bo=bo, nkb=nkb: e.matmul(
                            ps[bo][:, col0:512], lhsT=vslot[:, kq, :], rhs=PMb[pr][:, 0:ncols],
                            start=(kb == 0), stop=(kb == nkb - 1)),
                            reads=[b_w2s[s_], b_PM[pr]], writes=[psb[bo]])
                        k.op("pe", lambda e, pr=pr, col0=col0, ncols=ncols, kb=kb, bl=bl, nkb=nkb: e.matmul(
                            ps[bl][:, col0:512], lhsT=ones128[:, :], rhs=PMb[pr][:, 0:ncols],
                            start=(kb == 0), stop=(kb == nkb - 1)),
                            reads=[b_oc, b_PM[pr]], writes=[psb[bl]])
                r_ = hd % 2
                k.op("dve", lambda e, r_=r_, bl=bl: e.reciprocal(out=rtmp[r_][:, :], in_=ps[bl][:, :]),
                     reads=[psb[bl]], writes=[b_rtmp[r_]])
                k.op("dve", lambda e, r_=r_, bo=bo, hd=hd: e.tensor_tensor(
                    out=qT[:, hd, :], in0=ps[bo][:, :], in1=rtmp[r_][:, :], op=ALU.mult),
                    reads=[psb[bo], b_rtmp[r_]], writes=[b_qT[hd]])
                reserved.discard(bo)
                reserved.discard(bl)
            for cq in range(8):
                s_ = wchunk(w_out, cq * 256, 256, key=("out", cq))
                for j in range(4):
                    bi = next_bank()
                    for cb in range(16):
                        k.op("pe", lambda e, s_=s_, j=j, cb=cb, bi=bi: e.matmul(
                            ps[bi][:, 0:256], lhsT=qT[:, cb, j * 128:(j + 1) * 128],
                            rhs=w1s[s_][:, cb, 0:256], start=(cb == 0), stop=(cb == 15)),
                            reads=[b_w1s[s_], b_qT[cb]], writes=[psb[bi]])
                    k.op("dve", lambda e, j=j, bi=bi, cq=cq: e.tensor_tensor(
                        out=hs[:, j, cq * 256:(cq + 1) * 256], in0=ps[bi][:, 0:256],
                        in1=hs[:, j, cq * 256:(cq + 1) * 256], op=ALU.add),
                        reads=[psb[bi], b_hs[j]], writes=[b_hs[j]])
            alias_fence(odd_bufs, b_actT)

        first = True
        for li, l in enumerate(layers):
            last = (li == len(layers) - 1)
            if "mix" in parts and l % 2 == 0:
                alias_fence(odd2_bufs, even2_bufs + [b_lc])
                even_consts(l // 2)
            if "mix" in parts and l % 2 == 1:
                odd_consts(l // 2)
            for st in range(NST):
                ctx["l"], ctx["st"] = l, st
                src_h = x if first else hscr
                for j in range(TS // 128):
                    k.dma("sp", hs[:, j, :], src_h[st * TS + j * 128: st * TS + (j + 1) * 128, :],
                          reads=[b_hdram[st]] if not first else [], writes=[b_hs[j]], chan=f"hs{j}")
                if "mix" in parts:
                    load_gain(norm_mix_g[l:l + 1, :])
                    for j in range(TS // 128):
                        norm_tile(j)
                        norm_apply_T(j)
                    if l % 2 == 0:
                        even_mixer(l // 2)
                    else:
                        odd_mixer(l // 2, st)
                if "ffn" in parts:
                    load_gain(norm_ffn_g[l:l + 1, :])
                    for j in range(TS // 128):
                        norm_tile(j)
                        norm_apply_T(j)
                    ffn_supertile(l)
                if last and do_final:
                    load_gain(final_norm_g[0:1, :])
                    for j in range(TS // 128):
                        norm_tile(j)
                        k.op("dve", lambda e, j=j: e.scalar_tensor_tensor(
                            out=hs[:, j, :], in0=hs[:, j, :], scalar=stat[:, 2:3], in1=gB[:],
                            op0=ALU.mult, op1=ALU.mult),
                            reads=[b_hs[j], b_stat, b_gB], writes=[b_hs[j]])
                dst_h = out if last else hscr
                for j in range(TS // 128):
                    k.dma("sp", dst_h[st * TS + j * 128: st * TS + (j + 1) * 128, :], hs[:, j, :],
                          reads=[b_hs[j]], writes=[b_hdram[st]], chan=f"ho{j}")
            first = False
        k.wait_all("sp", b_hdram)

        with nc.Block() as block:
            @block.tensor
            def _(e):
                k.replay("pe", e)

            @block.scalar
            def _(e):
                k.replay("act", e)

            @block.vector
            def _(e):
                k.replay("dve", e)

            @block.gpsimd
            def _(e):
                k.replay("pool", e)

            @block.sync
            def _(e):
                k.replay("sp", e)
    return nc


def _bucket_table():
    d = np.arange(0, 257)
    dd = np.maximum(d, 1).astype(np.float32)
    large = 16 + (np.log(dd / np.float32(16)) / np.float32(math.log(128 / 16))
                  * np.float32(16)).astype(np.int32)
    large = np.minimum(large, 31)
    return np.where(d < 16, d, large)


def _oh_pad():
    bk = _bucket_table()
    oh = np.zeros((32, 384), np.float32)
    for m in range(127, 384):
        oh[bk[m - 127], m] = 1.0
    return oh


def make_in_map(inp, x):
    f = lambda a: np.ascontiguousarray(np.asarray(a, dtype=np.float32))
    return dict(
        x=f(x), norm_mix_g=f(inp["norm_mix_g"]), norm_ffn_g=f(inp["norm_ffn_g"]),
        final_norm_g=f(inp["final_norm_g"]).reshape(1, -1),
        ffn_w1=f(inp["ffn_w1"]), ffn_w2=f(inp["ffn_w2"]),
        ab_w_in=f(inp["ab_w_in"]), a_v_ln_g=f(inp["a_v_ln_g"]).reshape(2, 1, 1024),
        a_w_s=f(inp["a_w_s"]), a_b_s=f(inp["a_b_s"]).reshape(2, 1, 1024),
        b_gate_w2=f(inp["b_gate_w2"]), b_gate_b=f(inp["b_gate_b"]).reshape(2, 1, 512),
        b_out_norm_g=f(inp["b_out_norm_g"]).reshape(2, 1, 256), ab_w_out=f(inp["ab_w_out"]),
        c_w_in=f(inp["c_w_in"]), c_w_out=f(inp["c_w_out"]), rel_bias=f(inp["rel_bias"]),
        oh_pad=_oh_pad())


def kernel(**inputs):
    x = np.asarray(inputs["x"], dtype=np.float32)
    nc = build(dict(T=SEQ, layers=[0, 1, 2, 3]))
    in_maps = [make_in_map(inputs, x[c % BATCH]) for c in range(8)]
    res = run_bass_kernel_spmd(nc, in_maps, core_ids=list(range(8)))
    out = np.stack([np.asarray(res.results[b]["out"]) for b in range(BATCH)])
    return out.astype(np.float32)
```

```python
import math
from contextlib import ExitStack

import numpy as np
import concourse.bass as bass
import concourse.mybir as mybir
from concourse.bass_utils import run_bass_kernel_spmd

F32 = mybir.dt.float32
BF16 = mybir.dt.bfloat16
AF = mybir.ActivationFunctionType
ALU = mybir.AluOpType
AX = mybir.AxisListType

D = 2048
NDB = D // 128
DFF = 8192
NFB = DFF // 128
SEQ = 4096
BATCH = 4
DEPTH = 4
TS = 512
EPS = 1e-6
AB_IN = 5136
C_IN = 4176


class Buf:
    __slots__ = ("w", "r")

    def __init__(self):
        self.w = {}
        self.r = {}


def _merge(dst, src):
    for s, v in src.items():
        if dst.get(s, 0) < v:
            dst[s] = v


class KB:
    ENG = ("pe", "act", "dve", "pool", "sp")

    def __init__(self, nc, es):
        self.nc = nc
        self.es = es
        self.q = {e: [] for e in self.ENG}
        self.semh = {}
        self.semc = {}
        for e in ("pe", "act", "dve", "pool"):
            self.newsem(e)

    def newsem(self, name):
        if name not in self.semh:
            self.semh[name] = self.es.enter_context(self.nc.semaphore("s_" + name))
            self.semc[name] = 0
        return name

    def op(self, eng, fn, reads=(), writes=(), sem=None, inc=1):
        deps = {}
        for b in reads:
            _merge(deps, b.w)
        for b in writes:
            _merge(deps, b.w)
            _merge(deps, b.r)
        if sem is None:
            sem = eng
        if eng == "pe":
            deps.pop("pe", None)
        self.semc[sem] += inc
        val = self.semc[sem]
        self.q[eng].append((fn, deps, sem, inc))
        for b in reads:
            if b.r.get(sem, 0) < val:
                b.r[sem] = val
        for b in writes:
            b.w = {sem: val}
            b.r = {}
        return (sem, val)

    def dma(self, eng, out, in_, reads=(), writes=(), chan="d0"):
        self.newsem(chan)
        return self.op(eng, lambda e: e.dma_start(out=out, in_=in_), reads, writes,
                       sem=chan, inc=16)

    def wait_all(self, eng, bufs):
        deps = {}
        for b in bufs:
            _merge(deps, b.w)
            _merge(deps, b.r)
        self.q[eng].append((None, deps, None, 0))

    def replay(self, eng, e):
        waited = {}
        for fn, deps, sem, inc in self.q[eng]:
            for s, v in deps.items():
                if waited.get(s, 0) < v:
                    e.wait_ge(self.semh[s], v)
                    waited[s] = v
            if fn is not None:
                ins = fn(e)
                ins.then_inc(self.semh[sem], inc)


def build(cfg):
    T = cfg["T"]
    layers = cfg["layers"]
    parts = cfg.get("parts", ("mix", "ffn"))
    do_final = cfg.get("final", True)
    NST = T // TS
    nc = bass.Bass("TRN2", target_bir_lowering=False)

    def din(name, shape):
        return nc.dram_tensor(name, list(shape), F32, kind="ExternalInput").ap()

    x = din("x", [T, D])
    norm_mix_g = din("norm_mix_g", [DEPTH, D])
    norm_ffn_g = din("norm_ffn_g", [DEPTH, D])
    final_norm_g = din("final_norm_g", [1, D])
    ffn_w1 = din("ffn_w1", [DEPTH, D, DFF])
    ffn_w2 = din("ffn_w2", [DEPTH, DFF, D])
    ab_w_in = din("ab_w_in", [2, D, AB_IN])
    a_v_ln_g = din("a_v_ln_g", [2, 1, 1024])
    a_w_s = din("a_w_s", [2, 8, 128, 128])
    a_b_s = din("a_b_s", [2, 1, 1024])
    b_gate_w2 = din("b_gate_w2", [2, 16, 512])
    b_gate_b = din("b_gate_b", [2, 1, 512])
    b_out_norm_g = din("b_out_norm_g", [2, 1, 256])
    ab_w_out = din("ab_w_out", [2, D, D])
    c_w_in = din("c_w_in", [2, D, C_IN])
    c_w_out = din("c_w_out", [2, D, D])
    rel_bias = din("rel_bias", [32, 16])
    oh_pad = din("oh_pad", [32, 384])
    kT_d = nc.dram_tensor("kT_d", [4, 128, T], BF16, kind="Internal").ap()
    v_d = nc.dram_tensor("v_d", [T, 512], BF16, kind="Internal").ap()
    fpad_d = nc.dram_tensor("fpad_d", [16, 384], F32, kind="Internal").ap()
    out = nc.dram_tensor("out", [T, D], F32, kind="ExternalOutput").ap()
    hscr = nc.dram_tensor("hscr", [T, D], F32, kind="Internal").ap()

    NCHK = 136
    wscr = [nc.dram_tensor(f"wscr{i}", [NCHK, 128, 4096], BF16, kind="Internal").ap()
            for i in range(DEPTH)]
    wreg = {}
    ctx = {"l": 0, "st": 0}

    es = ExitStack()
    with es:
        k = KB(nc, es)

        def cached_load(slot_t, slot_buf, slot_id, key, kind, sub, cast_loads):
            l, st = ctx["l"], ctx["st"]
            if (l, key) not in wreg:
                wreg[(l, key)] = (sum(1 for (ll, _) in wreg if ll == l), Buf())
            idx, wb = wreg[(l, key)]
            assert idx < NCHK
            if kind == "w1":
                scr = wscr[l][idx].rearrange("p (b c) -> p b c", b=16)
            else:
                scr = wscr[l][idx][:, 0:2048].rearrange("p (b c) -> p b c", b=4)
            if st == 0:
                cast_loads()
                k.dma("sp", sub(scr), sub(slot_t), reads=[slot_buf], writes=[wb],
                      chan=f"wo_{kind}{slot_id}")
            else:
                k.dma("sp", sub(slot_t), sub(scr), reads=[wb], writes=[slot_buf],
                      chan=f"wh_{kind}{slot_id}")

        def sb(name, shape, dt):
            return es.enter_context(nc.sbuf_tensor(name, list(shape), dt))

        ps = [es.enter_context(nc.psum_tensor(f"ps{i}", [128, 512], F32)) for i in range(8)]
        psb = [Buf() for _ in range(8)]
        ps_rr = [0]

        reserved = set()

        def next_bank():
            while True:
                i = ps_rr[0]
                ps_rr[0] = (i + 1) % 8
                if i not in reserved:
                    return i

        def reserve_bank():
            i = next_bank()
            reserved.add(i)
            return i

        ident = sb("ident", [128, 128], BF16)
        identf = sb("identf", [128, 128], F32)
        b_ident = Buf()
        hs = sb("hs", [128, TS // 128, D], F32)
        b_hs = [Buf() for _ in range(TS // 128)]
        gB = sb("gB", [128, D], F32)
        b_gB = Buf()
        hn = sb("hn", [128, D], BF16)
        b_hn = Buf()
        junk = sb("junk", [128, 256], BF16)
        b_junk = Buf()
        stat = sb("stat", [128, 8], F32)
        b_stat = Buf()
        hnT = sb("hnT", [128, NDB, TS], BF16)
        b_hnT = [Buf() for _ in range(TS // 128)]
        actT = sb("actT", [128, NFB, TS], BF16)
        b_actT = [Buf() for _ in range(NFB)]
        arena = actT[:, :, :].rearrange("p a b -> p (a b)")

        def carve(off, n, pat=None, **kw):
            v = arena[:, off:off + n]
            return v.rearrange(pat, **kw) if pat else v
        a_uT = carve(0, 4096, "p (g t) -> p g t", g=8)
        v_sgu = carve(4096, 4096, "p (j c) -> p j c", j=4)
        v_gla = carve(8192, 4096, "p (j c) -> p j c", j=4)
        r_s = carve(12288, 4096, "p (j c) -> p j c", j=4)
        catT = carve(16384, 8192, "p (b t) -> p b t", b=16)
        qe = carve(24576, 2048, "p (h t) -> p h t", h=4)
        ke = carve(26624, 2048, "p (h t) -> p h t", h=4)
        ks = carve(28672, 2048, "p (h t) -> p h t", h=4)
        ksT = carve(30720, 2048, "p (h j d) -> p h j d", h=4, j=4)
        b_auT = [Buf() for _ in range(8)]
        b_vsgu = [Buf() for _ in range(4)]
        b_vgla = [Buf() for _ in range(4)]
        b_rs = [Buf() for _ in range(4)]
        b_catT = [Buf() for _ in range(16)]
        b_qe = [Buf() for _ in range(4)]
        b_ke = [Buf() for _ in range(4)]
        b_ks = [Buf() for _ in range(4)]
        b_ksT = [Buf() for _ in range(4)]
        mix_bufs = b_auT + b_vsgu + b_vgla + b_rs + b_catT + b_qe + b_ke + b_ks + b_ksT
        arena2 = sb("arena2", [128, 18432], BF16)

        def carve2(off_b, nbytes, dt, pat=None, **kw):
            v = arena2[:, off_b // 2:(off_b + nbytes) // 2]
            if dt == F32:
                v = v.bitcast(F32)
            return v.rearrange(pat, **kw) if pat else v
        E1 = carve2(0, 4096, BF16, "p (h t) -> p h t", h=4)
        E2 = carve2(4096, 4096, BF16, "p (h t) -> p h t", h=4)
        b_E = [Buf() for _ in range(4)]
        explast = sb("explast", [128, 4, 4], F32)
        spt = [carve2(8192 + i * 2048, 2048, F32) for i in range(2)]
        b_spt = [Buf() for _ in range(2)]
        tmpf = [carve2(12288 + i * 2048, 2048, F32) for i in range(2)]
        b_tmpf = [Buf() for _ in range(2)]
        tmpf_rr = [0]
        scTm = carve2(16384, 1024, BF16, "p (h t) -> p h t", h=4)
        b_scTm = Buf()
        Sst = carve2(17408, 4096, F32, "p (h v) -> p h v", h=4)
        Sbf = carve2(21504, 2048, BF16, "p (h v) -> p h v", h=4)
        b_S = [Buf() for _ in range(4)]
        b_Sbf = [Buf() for _ in range(4)]
        lnB = carve2(23552, 4096, F32)
        ongB = carve2(27648, 1024, F32)
        WT = carve2(28672, 2048, BF16, "p (g t) -> p g t", g=8)
        bout = carve2(30720, 2048, BF16)
        b_bout = Buf()
        bs_row = sb("bs_row", [1, 1024], BF16)
        W2ext = sb("W2ext", [32, 512], BF16)
        even2_bufs = b_E + b_spt + b_tmpf + [b_scTm] + b_S + b_Sbf + [b_bout]
        Iacc = carve2(0, 16384, F32)
        b_I = Buf()
        ikT = carve2(16384, 8192, BF16)
        b_ikT = [Buf() for _ in range(8)]
        BTn = carve2(24576, 8192, BF16, "p (h t) -> p h t", h=16)
        b_BTn = Buf()
        PTb = [carve2(32768 + i * 1024, 1024, BF16) for i in range(2)]
        PMb = [carve2(34816 + i * 1024, 1024, BF16) for i in range(2)]
        b_PT = [Buf() for _ in range(2)]
        b_PM = [Buf() for _ in range(2)]
        odd2_bufs = [b_I] + b_ikT + [b_BTn] + b_PT + b_PM
        b_lc = Buf()
        glr_ext = sb("glr_ext", [32, TS], BF16)
        b_glr = Buf()
        ones_row = sb("ones_row", [1, 128], BF16)
        onec = sb("onec", [128, 1], F32)
        triNeg = sb("triNeg", [128, 128], F32)
        maskT = sb("maskT", [128, 4, 128], BF16)
        cf = sb("cf", [128, 128], F32)
        st2 = sb("st2", [128, 16], F32)
        b_st2 = Buf()

        def alias_fence(src, dst):
            tok = {}
            for b in src:
                _merge(tok, b.w)
                _merge(tok, b.r)
            for b in dst:
                _merge(b.r, tok)
        rtmp = [sb(f"rtmp{i}", [128, TS], F32) for i in range(2)]
        b_rtmp = [Buf() for _ in range(2)]
        NW1 = 2
        W1C = 256
        w1s = [sb(f"w1s{i}", [128, NDB, W1C], BF16) for i in range(NW1)]
        b_w1s = [Buf() for _ in range(NW1)]
        NW2 = 3
        W2R = 4
        w2s = [sb(f"w2s{i}", [128, W2R, 512], BF16) for i in range(NW2)]
        b_w2s = [Buf() for _ in range(NW2)]
        w1_rr = [0]
        w2_rr = [0]
        b_hdram = [Buf() for _ in range(NST)]

        epsc = sb("epsc", [128, 1], F32)
        k.op("pool", lambda e: e.memset(epsc[:], EPS), writes=[b_ident])
        k.op("pool", lambda e: e.memset(identf[:], 0.0), writes=[b_ident])
        k.op("pool", lambda e: e.affine_select(
            out=identf[:], in_=identf[:], pattern=[[-1, 128]], compare_op=ALU.not_equal,
            fill=1.0, base=0, channel_multiplier=1), reads=[b_ident], writes=[b_ident])
        k.op("dve", lambda e: e.tensor_copy(out=ident[:], in_=identf[:]),
             reads=[b_ident], writes=[b_ident])

        k.op("pool", lambda e: e.memset(onec[:], 1.0), writes=[b_ident])
        k.op("pool", lambda e: e.memset(ones_row[:], 1.0), writes=[b_ident])
        k.op("pool", lambda e: e.memset(glr_ext[:], 1.0), writes=[b_glr])
        k.op("pool", lambda e: e.memset(triNeg[:], -1.0 / 16.0), writes=[b_ident])
        k.op("pool", lambda e: e.affine_select(
            out=triNeg[:], in_=triNeg[:], pattern=[[1, 128]], compare_op=ALU.is_ge,
            fill=0.0, base=0, channel_multiplier=-1), reads=[b_ident], writes=[b_ident])
        k.op("pool", lambda e: e.memset(cf[:], 1.0), writes=[b_ident])
        k.op("pool", lambda e: e.affine_select(
            out=cf[:], in_=cf[:], pattern=[[1, 128]], compare_op=ALU.is_ge,
            fill=0.0, base=0, channel_multiplier=-1), reads=[b_ident], writes=[b_ident])
        for hh in range(4):
            k.op("dve", lambda e, hh=hh: e.tensor_copy(out=maskT[:, hh, :], in_=cf[:]),
                 reads=[b_ident], writes=[b_ident])
        def load_gain(g_ap):
            k.dma("sp", gB[:], g_ap.partition_broadcast(128), writes=[b_gB], chan="gB")

        def norm_tile(j, dst_T=True):
            k.op("act", lambda e: e.activation(out=hn[:], in_=hs[:, j, :], func=AF.Square,
                                               accum_out=stat[:, 0:1]),
                 reads=[b_hs[j]], writes=[b_hn, b_stat])
            k.op("act", lambda e: e.activation(out=stat[:, 1:2], in_=stat[:, 0:1], func=AF.Sqrt,
                                               bias=epsc[:, 0:1], scale=1.0 / D),
                 reads=[b_stat, b_ident], writes=[b_stat])
            k.op("dve", lambda e: e.reciprocal(out=stat[:, 2:3], in_=stat[:, 1:2]),
                 reads=[b_stat], writes=[b_stat])

        def norm_apply_T(j):
            k.op("dve", lambda e: e.scalar_tensor_tensor(
                out=hn[:], in0=hs[:, j, :], scalar=stat[:, 2:3], in1=gB[:],
                op0=ALU.mult, op1=ALU.mult),
                reads=[b_hs[j], b_stat, b_gB], writes=[b_hn])
            for half in range(2):
                bi = next_bank()
                pt = ps[bi].bitcast(BF16)
                for q in range(8):
                    db = half * 8 + q
                    k.op("pe", lambda e, db=db, q=q, pt=pt: e.transpose(
                        out=pt[:, q * 128:(q + 1) * 128], in_=hn[:, db * 128:(db + 1) * 128],
                        identity=ident[:]),
                        reads=[b_hn, b_ident], writes=[psb[bi]])
                eng = "act" if half == 0 else "dve"
                src = pt[:, :].rearrange("p (q t) -> p q t", q=8)
                dst = hnT[:, half * 8:(half + 1) * 8, j * 128:(j + 1) * 128]
                if eng == "act":
                    k.op("act", lambda e, src=src, dst=dst: e.copy(out=dst, in_=src),
                         reads=[psb[bi]], writes=[b_hnT[j]])
                else:
                    k.op("dve", lambda e, src=src, dst=dst: e.tensor_copy(out=dst, in_=src),
                         reads=[psb[bi]], writes=[b_hnT[j]])

        def ffn_supertile(l):
            NTT = TS // 128
            for c in range(DFF // W1C):
                s = w1_rr[0]
                w1_rr[0] = (s + 1) % NW1
                src = ffn_w1[l, :, c * W1C:(c + 1) * W1C].rearrange("(b p) c -> p b c", p=128)
                cached_load(w1s[s], b_w1s[s], s, ("w1", c), "w1", lambda v: v[:, :, :],
                            lambda s=s, src=src: k.dma("pool", w1s[s][:], src, writes=[b_w1s[s]],
                                                       chan=f"w1_{s}"))
                for fb in range(W1C // 128):
                    ffb = c * (W1C // 128) + fb
                    bi = next_bank()
                    for db in range(NDB):
                        k.op("pe", lambda e, s=s, fb=fb, db=db, bi=bi: e.matmul(
                            ps[bi][:, :], lhsT=w1s[s][:, db, fb * 128:(fb + 1) * 128],
                            rhs=hnT[:, db, :], start=(db == 0), stop=(db == NDB - 1)),
                            reads=[b_w1s[s]] + b_hnT, writes=[psb[bi]])
                    r = ffb % 2
                    k.op("act", lambda e, bi=bi, r=r: e.activation(
                        out=rtmp[r][:], in_=ps[bi][:, :], func=AF.Relu),
                        reads=[psb[bi]], writes=[b_rtmp[r]])
                    k.op("dve", lambda e, r=r, ffb=ffb: e.tensor_tensor(
                        out=actT[:, ffb, :], in0=rtmp[r][:], in1=rtmp[r][:], op=ALU.mult),
                        reads=[b_rtmp[r]], writes=[b_actT[ffb]])
            for dq in range(D // 512):
                banks = [next_bank() for _ in range(NTT)]
                for c in range(NFB // W2R):
                    s = w2_rr[0]
                    w2_rr[0] = (s + 1) % NW2
                    src = ffn_w2[l, c * W2R * 128:(c + 1) * W2R * 128,
                                 dq * 512:(dq + 1) * 512].rearrange("(b p) c -> p b c", p=128)
                    cached_load(w2s[s], b_w2s[s], s, ("w2", dq, c), "w2", lambda v: v[:, :, :],
                                lambda s=s, src=src: k.dma("pool", w2s[s][:], src, writes=[b_w2s[s]],
                                                           chan=f"w2_{s}"))
                    for fr in range(W2R):
                        ffb = c * W2R + fr
                        for j in range(NTT):
                            bi = banks[j]
                            k.op("pe", lambda e, s=s, fr=fr, ffb=ffb, j=j, bi=bi: e.matmul(
                                ps[bi][:, :], lhsT=actT[:, ffb, j * 128:(j + 1) * 128],
                                rhs=w2s[s][:, fr, :], start=(ffb == 0), stop=(ffb == NFB - 1)),
                                reads=[b_w2s[s], b_actT[ffb]], writes=[psb[bi]])
                for j in range(NTT):
                    bi = banks[j]
                    k.op("dve", lambda e, j=j, bi=bi, dq=dq: e.tensor_tensor(
                        out=hs[:, j, dq * 512:(dq + 1) * 512], in0=ps[bi][:, :],
                        in1=hs[:, j, dq * 512:(dq + 1) * 512], op=ALU.add),
                        reads=[psb[bi], b_hs[j]], writes=[b_hs[j]])


        lnsc = sb("lnsc", [128, 1], F32)
        k.op("pool", lambda e: e.memset(lnsc[:], -0.5 * math.log(128.0)), writes=[b_ident])

        def tm_tmp():
            r = tmpf_rr[0]
            tmpf_rr[0] = 1 - r
            return r

        def wchunk(w2d, c0, ncols, key=None):
            s_ = w1_rr[0]
            w1_rr[0] = (s_ + 1) % NW1
            if key is None:
                key = ("in", c0)
            cached_load(w1s[s_], b_w1s[s_], s_, key, "w1", lambda v: v[:, :, 0:ncols],
                        lambda: k.dma("pool", w1s[s_][:, :, 0:ncols],
                                      w2d[:, c0:c0 + ncols].rearrange("(b p) c -> p b c", p=128),
                                      writes=[b_w1s[s_]], chan=f"w1_{s_}"))
            return s_

        def proj_fm(w2d, c0, ncols, evac):
            s_ = wchunk(w2d, c0, ncols)
            nb = (ncols + 127) // 128
            for fb in range(nb):
                m = min(128, ncols - fb * 128)
                bi = next_bank()
                for db in range(NDB):
                    k.op("pe", lambda e, s_=s_, fb=fb, db=db, bi=bi, m=m: e.matmul(
                        ps[bi][0:m, :], lhsT=w1s[s_][:, db, fb * 128:fb * 128 + m],
                        rhs=hnT[:, db, :], start=(db == 0), stop=(db == NDB - 1)),
                        reads=[b_w1s[s_]] + b_hnT, writes=[psb[bi]])
                evac(fb, bi)

        def proj_tm(w2d, c0, evac):
            s_ = wchunk(w2d, c0, 256)
            for j in range(4):
                bi = next_bank()
                for db in range(NDB):
                    k.op("pe", lambda e, s_=s_, j=j, db=db, bi=bi: e.matmul(
                        ps[bi][:, 0:256], lhsT=hnT[:, db, j * 128:(j + 1) * 128],
                        rhs=w1s[s_][:, db, 0:256], start=(db == 0), stop=(db == NDB - 1)),
                        reads=[b_w1s[s_], b_hnT[j]], writes=[psb[bi]])
                evac(j, bi)

        def even_consts(i):
            k.dma("pool", W2ext[0:16, :], b_gate_w2[i], writes=[b_lc], chan="lc")
            k.dma("pool", W2ext[16:17, :], b_gate_b[i], writes=[b_lc], chan="lc")
            k.dma("sp", lnB[:], a_v_ln_g[i].partition_broadcast(128), writes=[b_lc], chan="lcs")
            k.dma("sp", ongB[:], b_out_norm_g[i].partition_broadcast(128), writes=[b_lc], chan="lcs")
            k.dma("pool", bs_row[:], a_b_s[i], writes=[b_lc], chan="lc")
            for half in range(2):
                k.dma("sp", rtmp[half][:].rearrange("p (g s) -> p g s", g=4),
                      a_w_s[i, half * 4:(half + 1) * 4].rearrange("g t s -> t g s"),
                      writes=[b_rtmp[half]], chan=f"rt{half}")
                bi = next_bank()
                for gq in range(4):
                    k.op("pe", lambda e, half=half, gq=gq, bi=bi: e.transpose(
                        out=ps[bi][:, gq * 128:(gq + 1) * 128],
                        in_=rtmp[half][:, gq * 128:(gq + 1) * 128], identity=identf[:]),
                        reads=[b_rtmp[half], b_ident], writes=[psb[bi]])
                r_ = tm_tmp()
                k.op("act", lambda e, r_=r_, bi=bi: e.copy(out=tmpf[r_][:], in_=ps[bi][:, :]),
                     reads=[psb[bi]], writes=[b_tmpf[r_]])
                v3 = tmpf[r_][:].rearrange("p (g t) -> p g t", g=4)
                k.op("pool", lambda e, v3=v3: e.affine_select(
                    out=v3, in_=v3, pattern=[[0, 4], [1, 128]], compare_op=ALU.is_ge,
                    fill=0.0, base=0, channel_multiplier=-1),
                    reads=[b_tmpf[r_]], writes=[b_tmpf[r_]])
                k.op("dve", lambda e, v3=v3, half=half: e.tensor_copy(
                    out=WT[:, half * 4:(half + 1) * 4, :], in_=v3),
                    reads=[b_tmpf[r_]], writes=[b_lc])
            k.op("pool", lambda e: e.memset(Sst[:], 0.0), writes=b_S)
            k.op("pool", lambda e: e.memset(Sbf[:], 0.0), writes=b_Sbf)

        def even_mixer(i):
            w_in = ab_w_in[i]
            w_out = ab_w_out[i]
            alias_fence(b_actT, mix_bufs)
            def ev_glr(fb, bi):
                k.op("act", lambda e, bi=bi: e.copy(out=glr_ext[0:16, :], in_=ps[bi][0:16, :]),
                     reads=[psb[bi]], writes=[b_glr])
            proj_fm(w_in, 5120, 16, ev_glr)
            cumb = [reserve_bank() for _ in range(4)]
            for j in range(4):
                bg = next_bank()
                k.op("pe", lambda e, j=j, bg=bg: e.matmul(
                    ps[bg][:, :], lhsT=glr_ext[0:17, j * 128:(j + 1) * 128], rhs=W2ext[0:17, :],
                    start=True, stop=True), reads=[b_glr, b_lc], writes=[psb[bg]])
                sj = j % 2
                k.op("act", lambda e, sj=sj, bg=bg: e.activation(
                    out=spt[sj][:], in_=ps[bg][:, :], func=AF.Exp, scale=-1.0),
                    reads=[psb[bg]], writes=[b_spt[sj]])
                k.op("act", lambda e, sj=sj: e.activation(
                    out=spt[sj][:], in_=spt[sj][:], func=AF.Ln, bias=onec[:, 0:1], scale=1.0),
                    reads=[b_spt[sj], b_ident], writes=[b_spt[sj]])
                for hh in range(4):
                    k.op("pe", lambda e, sj=sj, hh=hh, j=j: e.matmul(
                        ps[cumb[hh]][:, j * 128:(j + 1) * 128],
                        lhsT=spt[sj][:, hh * 128:(hh + 1) * 128], rhs=triNeg[:, :],
                        start=True, stop=True),
                        reads=[b_spt[sj], b_ident], writes=[psb[cumb[hh]]])
            for hh in range(4):
                cb = cumb[hh]
                k.op("act", lambda e, hh=hh, cb=cb: e.activation(
                    out=E1[:, hh, :], in_=ps[cb][:, :], func=AF.Exp, bias=lnsc[:, 0:1], scale=1.0),
                    reads=[psb[cb], b_ident], writes=[b_E[hh]])
                k.op("act", lambda e, hh=hh, cb=cb: e.activation(
                    out=E2[:, hh, :], in_=ps[cb][:, :], func=AF.Exp, scale=-1.0),
                    reads=[psb[cb]], writes=[b_E[hh]])
                k.op("act", lambda e, hh=hh, cb=cb: e.activation(
                    out=explast[:, hh, :], in_=ps[cb][:, 127:512:128], func=AF.Exp),
                    reads=[psb[cb]], writes=[b_E[hh]])
                reserved.discard(cb)
            for c in range(2):
                def ev_q(fb, bi, c=c):
                    hh = c * 2 + fb
                    k.op("dve", lambda e, hh=hh, bi=bi: e.tensor_tensor(
                        out=qe[:, hh, :], in0=ps[bi][:, :], in1=E1[:, hh, :], op=ALU.mult),
                        reads=[psb[bi], b_E[hh]], writes=[b_qe[hh]])
                proj_fm(w_in, 2048 + c * 256, 256, ev_q)
            for c in range(2):
                def ev_k(fb, bi, c=c):
                    hh = c * 2 + fb
                    k.op("dve", lambda e, hh=hh, bi=bi: e.tensor_tensor(
                        out=ke[:, hh, :], in0=ps[bi][:, :], in1=E2[:, hh, :], op=ALU.mult),
                        reads=[psb[bi], b_E[hh]], writes=[b_ke[hh]])
                    for j in range(4):
                        k.op("dve", lambda e, hh=hh, bi=bi, j=j: e.scalar_tensor_tensor(
                            out=ks[:, hh, j * 128:(j + 1) * 128], in0=ps[bi][:, j * 128:(j + 1) * 128],
                            scalar=explast[:, hh, j:j + 1], in1=E2[:, hh, j * 128:(j + 1) * 128],
                            op0=ALU.mult, op1=ALU.mult),
                            reads=[psb[bi], b_E[hh]], writes=[b_ks[hh]])
                    bt = next_bank()
                    pt = ps[bt].bitcast(BF16)
                    for j in range(4):
                        k.op("pe", lambda e, hh=hh, j=j, pt=pt: e.transpose(
                            out=pt[:, j * 128:(j + 1) * 128], in_=ks[:, hh, j * 128:(j + 1) * 128],
                            identity=ident[:]), reads=[b_ks[hh], b_ident], writes=[psb[bt]])
                    k.op("act", lambda e, hh=hh, pt=pt: e.copy(
                        out=ksT[:, hh, :, :], in_=pt[:, 0:512].rearrange("p (j d) -> p j d", j=4)),
                        reads=[psb[bt]], writes=[b_ksT[hh]])
                proj_fm(w_in, 2560 + c * 256, 256, ev_k)
            for c in range(4):
                def ev_u(fb, bi, c=c):
                    g = c * 2 + fb
                    k.op("act", lambda e, g=g, bi=bi: e.activation(
                        out=a_uT[:, g, :], in_=ps[bi][:, :], func=AF.Gelu),
                        reads=[psb[bi]], writes=[b_auT[g]])
                proj_fm(w_in, c * 256, 256, ev_u)
            for c in range(4):
                def ev_v(j, bi, c=c):
                    for gg in range(2):
                        g = c * 2 + gg
                        r_ = tm_tmp()
                        k.op("act", lambda e, r_=r_, bi=bi, gg=gg: e.activation(
                            out=tmpf[r_][:, 0:128], in_=ps[bi][:, gg * 128:(gg + 1) * 128],
                            func=AF.Gelu, accum_out=st2[:, 0:1]),
                            reads=[psb[bi]], writes=[b_tmpf[r_], b_st2])
                        k.op("act", lambda e, r_=r_: e.activation(
                            out=junk[:, 0:128], in_=tmpf[r_][:, 0:128], func=AF.Square,
                            accum_out=st2[:, 1:2]),
                            reads=[b_tmpf[r_]], writes=[b_junk, b_st2])
                        k.op("dve", lambda e: e.tensor_scalar(
                            out=st2[:, 2:3], in0=st2[:, 0:1], scalar1=1.0 / 128, scalar2=None,
                            op0=ALU.mult), reads=[b_st2], writes=[b_st2])
                        k.op("dve", lambda e: e.tensor_tensor(
                            out=st2[:, 3:4], in0=st2[:, 2:3], in1=st2[:, 2:3], op=ALU.mult),
                            reads=[b_st2], writes=[b_st2])
                        k.op("dve", lambda e: e.scalar_tensor_tensor(
                            out=st2[:, 4:5], in0=st2[:, 1:2], scalar=1.0 / 128, in1=st2[:, 3:4],
                            op0=ALU.mult, op1=ALU.subtract), reads=[b_st2], writes=[b_st2])
                        k.op("act", lambda e: e.activation(
                            out=st2[:, 5:6], in_=st2[:, 4:5], func=AF.Sqrt, bias=epsc[:, 0:1],
                            scale=1.0), reads=[b_st2, b_ident], writes=[b_st2])
                        k.op("dve", lambda e: e.reciprocal(out=st2[:, 6:7], in_=st2[:, 5:6]),
                             reads=[b_st2], writes=[b_st2])
                        k.op("dve", lambda e, r_=r_: e.tensor_scalar(
                            out=tmpf[r_][:, 0:128], in0=tmpf[r_][:, 0:128], scalar1=st2[:, 2:3],
                            scalar2=st2[:, 6:7], op0=ALU.subtract, op1=ALU.mult),
                            reads=[b_tmpf[r_], b_st2], writes=[b_tmpf[r_]])
                        k.op("dve", lambda e, r_=r_, g=g, j=j: e.tensor_tensor(
                            out=v_sgu[:, j, g * 128:(g + 1) * 128], in0=tmpf[r_][:, 0:128],
                            in1=lnB[:, g * 128:(g + 1) * 128], op=ALU.mult),
                            reads=[b_tmpf[r_], b_lc], writes=[b_vsgu[j]])
                proj_tm(w_in, 1024 + c * 256, ev_v)
            for c in range(4):
                def ev_vg(j, bi, c=c):
                    k.op("act", lambda e, j=j, bi=bi, c=c: e.copy(
                        out=v_gla[:, j, c * 256:(c + 1) * 256], in_=ps[bi][:, 0:256]),
                        reads=[psb[bi]], writes=[b_vgla[j]])
                proj_tm(w_in, 3072 + c * 256, ev_vg)
            for c in range(4):
                def ev_r(j, bi, c=c):
                    k.op("act", lambda e, j=j, bi=bi, c=c: e.activation(
                        out=r_s[:, j, c * 256:(c + 1) * 256], in_=ps[bi][:, 0:256], func=AF.Silu),
                        reads=[psb[bi]], writes=[b_rs[j]])
                proj_tm(w_in, 4096 + c * 256, ev_r)
            for g in range(8):
                bi = next_bank()
                for j in range(4):
                    k.op("pe", lambda e, g=g, j=j, bi=bi: e.matmul(
                        ps[bi][:, j * 128:(j + 1) * 128], lhsT=v_sgu[:, j, g * 128:(g + 1) * 128],
                        rhs=WT[:, g, :], start=True, stop=False),
                        reads=[b_vsgu[j], b_lc], writes=[psb[bi]])
                    k.op("pe", lambda e, g=g, j=j, bi=bi: e.matmul(
                        ps[bi][:, j * 128:(j + 1) * 128], lhsT=ones_row[0:1, :],
                        rhs=bs_row[0:1, g * 128:(g + 1) * 128], start=False, stop=True),
                        reads=[b_ident, b_lc], writes=[psb[bi]])
                k.op("dve", lambda e, g=g, bi=bi: e.tensor_tensor(
                    out=catT[:, g, :], in0=ps[bi][:, :], in1=a_uT[:, g, :], op=ALU.mult),
                    reads=[psb[bi], b_auT[g]], writes=[b_catT[g]])
            for j in range(4):
                jc = slice(j * 128, (j + 1) * 128)
                bs_ = next_bank()
                for hh in range(4):
                    k.op("pe", lambda e, hh=hh, jc=jc, bs_=bs_: e.matmul(
                        ps[bs_][:, hh * 128:(hh + 1) * 128], lhsT=ke[:, hh, jc], rhs=qe[:, hh, jc],
                        start=True, stop=True), reads=[b_ke[hh], b_qe[hh]], writes=[psb[bs_]])
                k.op("dve", lambda e, bs_=bs_: e.tensor_tensor(
                    out=scTm[:, :, :].rearrange("p h t -> p (h t)"), in0=ps[bs_][:, :],
                    in1=maskT[:, :, :].rearrange("p h t -> p (h t)"), op=ALU.mult),
                    reads=[psb[bs_], b_ident], writes=[b_scTm])
                for hp in range(2):
                    bo = next_bank()
                    for hq in range(2):
                        hh = hp * 2 + hq
                        oc = slice(hq * 256, (hq + 1) * 256)
                        vc = slice(hh * 256, (hh + 1) * 256)
                        k.op("pe", lambda e, hh=hh, oc=oc, vc=vc, bo=bo, j=j: e.matmul(
                            ps[bo][:, oc], lhsT=scTm[:, hh, :], rhs=v_gla[:, j, vc],
                            start=True, stop=False),
                            reads=[b_scTm, b_vgla[j]], writes=[psb[bo]])
                        k.op("pe", lambda e, hh=hh, oc=oc, bo=bo, jc=jc: e.matmul(
                            ps[bo][:, oc], lhsT=qe[:, hh, jc], rhs=Sbf[:, hh, :],
                            start=False, stop=True),
                            reads=[b_qe[hh], b_Sbf[hh]], writes=[psb[bo]])
                    for hq in range(2):
                        hh = hp * 2 + hq
                        oc = slice(hq * 256, (hq + 1) * 256)
                        vc = slice(hh * 256, (hh + 1) * 256)
                        k.op("act", lambda e, oc=oc, bo=bo: e.activation(
                            out=junk[:, 0:256], in_=ps[bo][:, oc], func=AF.Square,
                            accum_out=st2[:, 8:9]), reads=[psb[bo]], writes=[b_junk, b_st2])
                        k.op("act", lambda e: e.activation(
                            out=st2[:, 9:10], in_=st2[:, 8:9], func=AF.Sqrt, bias=epsc[:, 0:1],
                            scale=1.0 / 256), reads=[b_st2, b_ident], writes=[b_st2])
                        k.op("dve", lambda e: e.reciprocal(out=st2[:, 10:11], in_=st2[:, 9:10]),
                             reads=[b_st2], writes=[b_st2])
                        r_ = tm_tmp()
                        k.op("dve", lambda e, r_=r_, oc=oc, bo=bo: e.scalar_tensor_tensor(
                            out=tmpf[r_][:, 0:256], in0=ps[bo][:, oc], scalar=st2[:, 10:11],
                            in1=ongB[:, :], op0=ALU.mult, op1=ALU.mult),
                            reads=[psb[bo], b_st2, b_lc], writes=[b_tmpf[r_]])
                        k.op("dve", lambda e, r_=r_, vc=vc, j=j: e.tensor_tensor(
                            out=bout[:, vc], in0=tmpf[r_][:, 0:256], in1=r_s[:, j, vc], op=ALU.mult),
                            reads=[b_tmpf[r_], b_rs[j]], writes=[b_bout])
                for hp in range(2):
                    bk = next_bank()
                    for hq in range(2):
                        hh = hp * 2 + hq
                        oc = slice(hq * 256, (hq + 1) * 256)
                        vc = slice(hh * 256, (hh + 1) * 256)
                        k.op("pe", lambda e, hh=hh, oc=oc, vc=vc, bk=bk, j=j: e.matmul(
                            ps[bk][:, oc], lhsT=ksT[:, hh, j, :], rhs=v_gla[:, j, vc],
                            start=True, stop=True),
                            reads=[b_ksT[hh], b_vgla[j]], writes=[psb[bk]])
                        k.op("dve", lambda e, hh=hh, oc=oc, bk=bk, j=j: e.scalar_tensor_tensor(
                            out=Sst[:, hh, :], in0=Sst[:, hh, :], scalar=explast[:, hh, j:j + 1],
                            in1=ps[bk][:, oc], op0=ALU.mult, op1=ALU.add),
                            reads=[psb[bk], b_S[hh], b_E[hh]], writes=[b_S[hh]])
                        k.op("act", lambda e, hh=hh: e.copy(out=Sbf[:, hh, :], in_=Sst[:, hh, :]),
                             reads=[b_S[hh]], writes=[b_Sbf[hh]])
                bt = next_bank()
                pt = ps[bt].bitcast(BF16)
                for q8 in range(8):
                    k.op("pe", lambda e, q8=q8, pt=pt: e.transpose(
                        out=pt[:, q8 * 128:(q8 + 1) * 128], in_=bout[:, q8 * 128:(q8 + 1) * 128],
                        identity=ident[:]), reads=[b_bout, b_ident], writes=[psb[bt]])
                k.op("act", lambda e, pt=pt, jc=jc: e.copy(
                    out=catT[:, 8:16, jc], in_=pt[:, :].rearrange("p (q t) -> p q t", q=8)),
                    reads=[psb[bt]], writes=b_catT[8:16])
            for cq in range(8):
                s_ = wchunk(w_out, cq * 256, 256, key=("out", cq))
                for j in range(4):
                    bi = next_bank()
                    for cb in range(16):
                        k.op("pe", lambda e, s_=s_, j=j, cb=cb, bi=bi: e.matmul(
                            ps[bi][:, 0:256], lhsT=catT[:, cb, j * 128:(j + 1) * 128],
                            rhs=w1s[s_][:, cb, 0:256], start=(cb == 0), stop=(cb == 15)),
                            reads=[b_w1s[s_], b_catT[cb]], writes=[psb[bi]])
                    k.op("dve", lambda e, j=j, bi=bi, cq=cq: e.tensor_tensor(
                        out=hs[:, j, cq * 256:(cq + 1) * 256], in0=ps[bi][:, 0:256],
                        in1=hs[:, j, cq * 256:(cq + 1) * 256], op=ALU.add),
                        reads=[psb[bi], b_hs[j]], writes=[b_hs[j]])
            alias_fence(mix_bufs, b_actT)


        qT = carve(0, 8192, "p (h t) -> p h t", h=16)
        iqT = carve(8192, 4096, "p (b t) -> p b t", b=8)
        selT = carve(12288, 16384, "p (k t) -> p k t", k=32)
        vn = carve(28672, 2048, "p (j c) -> p j c", j=4)
        kTn = carve(30720, 2048, "p (g t) -> p g t", g=4)
        b_qT = [Buf() for _ in range(16)]
        b_iqT = [Buf() for _ in range(8)]
        b_selT = [Buf() for _ in range(4)]
        b_vn = [Buf() for _ in range(4)]
        b_kTn = [Buf() for _ in range(4)]
        odd_bufs = b_qT + b_iqT + b_selT + b_vn + b_kTn
        iw_t = sb("iw_t", [128, 4, 16], F32)
        b_iw = [Buf() for _ in range(4)]
        thr = sb("thr", [128, 16], F32)
        b_thr = Buf()
        halfc = sb("halfc", [128, 1], F32)
        negtri = sb("negtri", [128, 128], F32)
        rb = sb("rb", [32, 16], F32)
        OHs = sb("OHs", [32, 384], F32)
        cfarB = sb("cfarB", [128, 16], F32)
        ones128 = sb("ones128", [128, 128], BF16)
        fsb = sb("fsb", [16, 384], F32)
        selq = sb("selq", [128, 512], BF16)
        b_selq = Buf()
        b_oc = Buf()
        b_kvd = [Buf() for _ in range(NST)]
        b_fpad = Buf()
        has_odd = any(l % 2 == 1 for l in layers) and "mix" in parts
        if has_odd:
            k.op("pool", lambda e: e.memset(halfc[:], 0.5), writes=[b_oc])
            k.op("pool", lambda e: e.memset(ones128[:], 1.0), writes=[b_oc])
            k.op("pool", lambda e: e.memset(negtri[:], 0.0), writes=[b_oc])
            k.op("pool", lambda e: e.affine_select(
                out=negtri[:], in_=negtri[:], pattern=[[-1, 128]], compare_op=ALU.is_ge,
                fill=-1.0e30, base=0, channel_multiplier=1), reads=[b_oc], writes=[b_oc])
            k.dma("sp", rb[:], rel_bias, writes=[b_oc], chan="oc")
            k.dma("sp", OHs[:], oh_pad, writes=[b_oc], chan="oc")
            k.dma("sp", cfarB[:], rel_bias[31:32, :].partition_broadcast(128), writes=[b_oc], chan="oc")
            bi = next_bank()
            k.op("pe", lambda e, bi=bi: e.matmul(ps[bi][0:16, 0:384], lhsT=rb[:, :], rhs=OHs[:, :],
                                                 start=True, stop=True),
                 reads=[b_oc], writes=[psb[bi]])
            k.op("act", lambda e, bi=bi: e.copy(out=fsb[:], in_=ps[bi][0:16, 0:384]),
                 reads=[psb[bi]], writes=[b_oc])
            k.dma("sp", fpad_d, fsb[:], reads=[b_oc], writes=[b_fpad], chan="oc2")

        def odd_consts(i):
            alias_fence(even2_bufs + [b_lc], odd2_bufs)
            k.op("pool", lambda e: e.memset(cf[:], 0.0), writes=[b_ident])
            k.op("pool", lambda e: e.affine_select(
                out=cf[:], in_=cf[:], pattern=[[1, 128]], compare_op=ALU.not_equal,
                fill=1.0, base=-127, channel_multiplier=1), reads=[b_ident], writes=[b_ident])
            for hd in range(16):
                r_ = hd % 2
                src = bass.AP(tensor=fpad_d.tensor, offset=fpad_d[hd:hd + 1, :].offset,
                              ap=[[1, 128], [1, 256]])
                k.dma("sp", rtmp[r_][:, 0:256], src, reads=[b_fpad], writes=[b_rtmp[r_]],
                      chan=f"rt{r_}")
                bi = next_bank()
                k.op("pe", lambda e, r_=r_, bi=bi: e.matmul(
                    ps[bi][:, 0:256], lhsT=cf[:, :], rhs=rtmp[r_][:, 0:256], start=True, stop=True),
                    reads=[b_rtmp[r_], b_ident], writes=[psb[bi]])
                k.op("dve", lambda e, hd=hd, bi=bi: e.tensor_scalar(
                    out=BTn[:, hd, :], in0=ps[bi][:, 0:256], scalar1=cfarB[:, hd:hd + 1],
                    scalar2=None, op0=ALU.subtract),
                    reads=[psb[bi], b_oc], writes=[b_BTn])

        def odd_mixer(i, st):
            w_in = c_w_in[i]
            w_out = c_w_out[i]
            t0 = st * TS
            alias_fence(b_actT, odd_bufs)
            for c in range(8):
                def ev_q(fb, bi, c=c):
                    hd = c * 2 + fb
                    k.op("act", lambda e, hd=hd, bi=bi: e.activation(
                        out=qT[:, hd, :], in_=ps[bi][:, :], func=AF.Copy, scale=128.0 ** -0.5),
                        reads=[psb[bi]], writes=[b_qT[hd]])
                proj_fm(w_in, c * 256, 256, ev_q)
            for c in range(2):
                def ev_k(fb, bi, c=c):
                    g = c * 2 + fb
                    k.op("act", lambda e, g=g, bi=bi: e.copy(out=kTn[:, g, :], in_=ps[bi][:, :]),
                         reads=[psb[bi]], writes=[b_kTn[g]])
                    k.dma("sp", kT_d[g, :, t0:t0 + TS], kTn[:, g, :], reads=[b_kTn[g]],
                          writes=[b_kvd[st]], chan=f"kvw{g}")
                proj_fm(w_in, 2048 + c * 256, 256, ev_k)
            for c in range(4):
                def ev_iq(fb, bi, c=c):
                    blk = c * 2 + fb
                    k.op("act", lambda e, blk=blk, bi=bi: e.copy(out=iqT[:, blk, :], in_=ps[bi][:, :]),
                         reads=[psb[bi]], writes=[b_iqT[blk]])
                proj_fm(w_in, 3072 + c * 256, 256, ev_iq)
            s_ = w1_rr[0]
            w1_rr[0] = (s_ + 1) % NW1
            def ik_loads(s_=s_):
                for dup in range(2):
                    k.dma("pool", w1s[s_][:, :, dup * 64:(dup + 1) * 64],
                          w_in[:, 4096:4160].rearrange("(b p) c -> p b c", p=128),
                          writes=[b_w1s[s_]], chan=f"w1_{s_}")
            cached_load(w1s[s_], b_w1s[s_], s_, ("ik",), "w1", lambda v: v[:, :, 0:128], ik_loads)
            bi = next_bank()
            for db in range(NDB):
                k.op("pe", lambda e, s_=s_, db=db, bi=bi: e.matmul(
                    ps[bi][:, :], lhsT=w1s[s_][:, db, 0:128], rhs=hnT[:, db, :],
                    start=(db == 0), stop=(db == NDB - 1)),
                    reads=[b_w1s[s_]] + b_hnT, writes=[psb[bi]])
            k.op("act", lambda e, bi=bi: e.copy(out=ikT[:, t0:t0 + TS], in_=ps[bi][:, :]),
                 reads=[psb[bi]], writes=[b_ikT[st]])
            for c in range(2):
                def ev_v(j, bi, c=c):
                    k.op("act", lambda e, j=j, bi=bi, c=c: e.copy(
                        out=vn[:, j, c * 256:(c + 1) * 256], in_=ps[bi][:, 0:256]),
                        reads=[psb[bi]], writes=[b_vn[j]])
                    if c == 1:
                        k.dma("sp", v_d[t0 + j * 128:t0 + (j + 1) * 128, :], vn[:, j, :],
                              reads=[b_vn[j]], writes=[b_kvd[st]], chan=f"kvw{j}")
                proj_tm(w_in, 2560 + c * 256, ev_v)
            s_ = wchunk(w_in, 4160, 16)
            for j in range(4):
                bi = next_bank()
                for db in range(NDB):
                    k.op("pe", lambda e, s_=s_, j=j, db=db, bi=bi: e.matmul(
                        ps[bi][:, 0:16], lhsT=hnT[:, db, j * 128:(j + 1) * 128],
                        rhs=w1s[s_][:, db, 0:16], start=(db == 0), stop=(db == NDB - 1)),
                        reads=[b_w1s[s_], b_hnT[j]], writes=[psb[bi]])
                k.op("act", lambda e, j=j, bi=bi: e.copy(out=iw_t[:, j, :], in_=ps[bi][:, 0:16]),
                     reads=[psb[bi]], writes=[b_iw[j]])
            for j in range(4):
                qb = st * 4 + j
                nk = (qb + 1) * 128
                nch = (nk + 511) // 512
                for h in range(16):
                    blk, po = h // 2, (h % 2) * 64
                    for c in range(nch):
                        ncol = min(512, nk - c * 512)
                        bi = next_bank()
                        k.op("pe", lambda e, blk=blk, po=po, j=j, c=c, ncol=ncol, bi=bi: e.matmul(
                            ps[bi][:, 0:ncol], lhsT=iqT[po:po + 64, blk, j * 128:(j + 1) * 128],
                            rhs=ikT[po:po + 64, c * 512:c * 512 + ncol], start=True, stop=True),
                            reads=[b_iqT[blk], b_ikT[c]], writes=[psb[bi]])
                        r_ = (h * nch + c) % 2
                        k.op("act", lambda e, r_=r_, ncol=ncol, bi=bi: e.activation(
                            out=rtmp[r_][:, 0:ncol], in_=ps[bi][:, 0:ncol], func=AF.Relu),
                            reads=[psb[bi]], writes=[b_rtmp[r_]])
                        cs = slice(c * 512, c * 512 + ncol)
                        if h == 0:
                            k.op("dve", lambda e, r_=r_, ncol=ncol, cs=cs, j=j: e.tensor_scalar(
                                out=Iacc[:, cs], in0=rtmp[r_][:, 0:ncol], scalar1=iw_t[:, j, 0:1],
                                scalar2=None, op0=ALU.mult),
                                reads=[b_rtmp[r_], b_iw[j]], writes=[b_I])
                        else:
                            k.op("dve", lambda e, r_=r_, ncol=ncol, cs=cs, j=j, h=h: e.scalar_tensor_tensor(
                                out=Iacc[:, cs], in0=rtmp[r_][:, 0:ncol], scalar=iw_t[:, j, h:h + 1],
                                in1=Iacc[:, cs], op0=ALU.mult, op1=ALU.add),
                                reads=[b_rtmp[r_], b_iw[j], b_I], writes=[b_I])
                k.op("dve", lambda e, nk=nk: e.tensor_reduce(out=thr[:, 1:2], in_=Iacc[:, 0:nk],
                                                            axis=AX.X, op=ALU.max),
                     reads=[b_I], writes=[b_thr])
                k.op("dve", lambda e, nk=nk: e.tensor_reduce(out=thr[:, 0:1], in_=Iacc[:, 0:nk],
                                                            axis=AX.X, op=ALU.min),
                     reads=[b_I], writes=[b_thr])
                k.op("dve", lambda e, qb=qb: e.tensor_tensor(
                    out=Iacc[:, qb * 128:(qb + 1) * 128], in0=Iacc[:, qb * 128:(qb + 1) * 128],
                    in1=negtri[:, :], op=ALU.add), reads=[b_I, b_oc], writes=[b_I])
                if nk > 256:
                    k.op("dve", lambda e: e.tensor_tensor(
                        out=thr[:, 6:7], in0=thr[:, 1:2], in1=thr[:, 0:1], op=ALU.subtract),
                        reads=[b_thr], writes=[b_thr])
                    for it in range(20):
                        cit = 0.5 ** (it + 1)
                        k.op("dve", lambda e, cit=cit: e.scalar_tensor_tensor(
                            out=thr[:, 2:3], in0=thr[:, 6:7], scalar=cit, in1=thr[:, 0:1],
                            op0=ALU.mult, op1=ALU.add), reads=[b_thr], writes=[b_thr])
                        n0 = min(nk, 2048)
                        k.op("dve", lambda e, n0=n0: e.tensor_scalar(
                            out=hn[:, 0:n0], in0=Iacc[:, 0:n0], scalar1=thr[:, 2:3], scalar2=None,
                            op0=ALU.is_ge, op1=ALU.add, accum_out=thr[:, 3:4]),
                            reads=[b_I, b_thr], writes=[b_hn, b_thr])
                        if nk > 2048:
                            k.op("dve", lambda e, nk=nk: e.tensor_scalar(
                                out=hn[:, 0:nk - 2048], in0=Iacc[:, 2048:nk], scalar1=thr[:, 2:3],
                                scalar2=None, op0=ALU.is_ge, op1=ALU.add, accum_out=thr[:, 4:5]),
                                reads=[b_I, b_thr], writes=[b_hn, b_thr])
                            k.op("dve", lambda e: e.tensor_tensor(
                                out=thr[:, 3:4], in0=thr[:, 3:4], in1=thr[:, 4:5], op=ALU.add),
                                reads=[b_thr], writes=[b_thr])
                        k.op("dve", lambda e, cit=cit: e.tensor_scalar(
                            out=thr[:, 5:6], in0=thr[:, 3:4], scalar1=255.5, scalar2=cit,
                            op0=ALU.is_ge, op1=ALU.mult), reads=[b_thr], writes=[b_thr])
                        k.op("dve", lambda e: e.scalar_tensor_tensor(
                            out=thr[:, 0:1], in0=thr[:, 5:6], scalar=thr[:, 6:7], in1=thr[:, 0:1],
                            op0=ALU.mult, op1=ALU.add), reads=[b_thr], writes=[b_thr])
                for c in range(nch):
                    ncol = min(512, nk - c * 512)
                    nb = ncol // 128
                    k.op("dve", lambda e, c=c, ncol=ncol: e.tensor_scalar(
                        out=selq[:, 0:ncol], in0=Iacc[:, c * 512:c * 512 + ncol], scalar1=thr[:, 0:1],
                        scalar2=None, op0=ALU.is_ge), reads=[b_I, b_thr], writes=[b_selq])
                    bt = next_bank()
                    pt = ps[bt].bitcast(BF16)
                    for q in range(nb):
                        k.op("pe", lambda e, q=q, pt=pt: e.transpose(
                            out=pt[:, q * 128:(q + 1) * 128], in_=selq[:, q * 128:(q + 1) * 128],
                            identity=ident[:]), reads=[b_selq, b_ident], writes=[psb[bt]])
                    k.op("act", lambda e, c=c, nb=nb, pt=pt, j=j: e.copy(
                        out=selT[:, c * 4:c * 4 + nb, j * 128:(j + 1) * 128],
                        in_=pt[:, 0:nb * 128].rearrange("p (q t) -> p q t", q=nb)),
                        reads=[psb[bt]], writes=[b_selT[j]])
            steps = []
            for hd in range(16):
                for c in range(st + 1):
                    for kq in range(4):
                        steps.append((hd, c, kq))
            nkb = st * 4 + 4
            nst_ = len(steps)
            LA = 2
            slot_of = {}
            bank_of = {}
            obank = {}

            def emit_load(hd, c):
                g = hd // 4
                if (g, c, hd) in slot_of:
                    return
                s_ = w2_rr[0]
                w2_rr[0] = (s_ + 1) % NW2
                kslot = w2s[s_][:, 0:1, :].rearrange("p a c -> p (a c)")
                vslot = w2s[s_][:, 1:2, :].rearrange("p a (q v) -> p (a q) v", q=4)
                k.dma("sp", kslot, kT_d[g, :, c * 512:(c + 1) * 512], reads=[b_kvd[c]],
                      writes=[b_w2s[s_]], chan=f"kv{s_}")
                k.dma("sp", vslot, v_d[c * 512:(c + 1) * 512, g * 128:(g + 1) * 128].rearrange(
                    "(q p) v -> p q v", p=128), reads=[b_kvd[c]], writes=[b_w2s[s_]], chan=f"kv{s_}")
                slot_of[(g, c, hd)] = (s_, kslot, vslot)

            def geom(c, kq):
                kb = c * 4 + kq
                col0 = kq * 128 if c == st else 0
                return kb, col0, 512 - col0

            def emit_qk(i):
                hd, c, kq = steps[i]
                g = hd // 4
                emit_load(hd, c)
                if kq == 0:
                    if c < st:
                        emit_load(hd, c + 1)
                    elif hd < 15:
                        emit_load(hd + 1, 0)
                s_, kslot, vslot = slot_of[(g, c, hd)]
                kb, col0, ncols = geom(c, kq)
                near = kb >= 4 * st - 1
                bi = next_bank()
                bank_of[i] = bi
                k.op("pe", lambda e: e.matmul(
                    ps[bi][:, 0:ncols], lhsT=kslot[:, kq * 128:(kq + 1) * 128],
                    rhs=qT[:, hd, col0:512], start=True, stop=(not near)),
                    reads=[b_w2s[s_], b_qT[hd]], writes=[psb[bi]])
                if near:
                    if c == st:
                        off, nbc = 0, min(256, ncols)
                    else:
                        off, nbc = 128, 128
                    k.op("pe", lambda e: e.matmul(
                        ps[bi][:, 0:nbc], lhsT=ident[:, :], rhs=BTn[:, hd, off:off + nbc],
                        start=False, stop=True),
                        reads=[b_BTn, b_ident], writes=[psb[bi]])

            def emit_softmax(i):
                hd, c, kq = steps[i]
                kb, col0, ncols = geom(c, kq)
                bi = bank_of[i]
                pr = i % 2
                k.op("act", lambda e: e.activation(
                    out=PTb[pr][:, 0:ncols], in_=ps[bi][:, 0:ncols], func=AF.Exp,
                    bias=cfarB[:, hd:hd + 1], scale=1.0),
                    reads=[psb[bi], b_oc], writes=[b_PT[pr]])
                k.op("dve", lambda e: e.tensor_tensor(
                    out=PMb[pr][:, 0:ncols], in0=PTb[pr][:, 0:ncols], in1=selT[:, kb, col0:512],
                    op=ALU.mult), reads=[b_PT[pr]] + b_selT, writes=[b_PM[pr]])

            def emit_pv(i):
                hd, c, kq = steps[i]
                g = hd // 4
                kb, col0, ncols = geom(c, kq)
                s_, kslot, vslot = slot_of[(g, c, hd)]
                pr = i % 2
                if kb == 0:
                    obank[hd] = (reserve_bank(), reserve_bank())
                bo, bl = obank[hd]
                k.op("pe", lambda e: e.matmul(
                    ps[bo][:, col0:512], lhsT=vslot[:, kq, :], rhs=PMb[pr][:, 0:ncols],
                    start=(kb == 0), stop=(kb == nkb - 1)),
                    reads=[b_w2s[s_], b_PM[pr]], writes=[psb[bo]])
                k.op("pe", lambda e: e.matmul(
                    ps[bl][:, col0:512], lhsT=ones128[:, :], rhs=PMb[pr][:, 0:ncols],
                    start=(kb == 0), stop=(kb == nkb - 1)),
                    reads=[b_oc, b_PM[pr]], writes=[psb[bl]])
                if kb == nkb - 1:
                    r_ = hd % 2
                    k.op("dve", lambda e: e.reciprocal(out=rtmp[r_][:, :], in_=ps[bl][:, :]),
                         reads=[psb[bl]], writes=[b_rtmp[r_]])
                    k.op("dve", lambda e: e.tensor_tensor(
                        out=qT[:, hd, :], in0=ps[bo][:, :], in1=rtmp[r_][:, :], op=ALU.mult),
                        reads=[psb[bo], b_rtmp[r_]], writes=[b_qT[hd]])
                    reserved.discard(bo)
                    reserved.discard(bl)

            for i in range(min(LA, nst_)):
                emit_qk(i)
            for i in range(nst_):
                emit_softmax(i)
                if i + LA < nst_:
                    emit_qk(i + LA)
                emit_pv(i)
            for cq in range(8):
                s_ = wchunk(w_out, cq * 256, 256, key=("out", cq))
                for j in range(4):
                    bi = next_bank()
                    for cb in range(16):
                        k.op("pe", lambda e, s_=s_, j=j, cb=cb, bi=bi: e.matmul(
                            ps[bi][:, 0:256], lhsT=qT[:, cb, j * 128:(j + 1) * 128],
                            rhs=w1s[s_][:, cb, 0:256], start=(cb == 0), stop=(cb == 15)),
                            reads=[b_w1s[s_], b_qT[cb]], writes=[psb[bi]])
                    k.op("dve", lambda e, j=j, bi=bi, cq=cq: e.tensor_tensor(
                        out=hs[:, j, cq * 256:(cq + 1) * 256], in0=ps[bi][:, 0:256],
                        in1=hs[:, j, cq * 256:(cq + 1) * 256], op=ALU.add),
                        reads=[psb[bi], b_hs[j]], writes=[b_hs[j]])
            alias_fence(odd_bufs, b_actT)

        first = True
        for li, l in enumerate(layers):
            last = (li == len(layers) - 1)
            if "mix" in parts and l % 2 == 0:
                alias_fence(odd2_bufs, even2_bufs + [b_lc])
                even_consts(l // 2)
            if "mix" in parts and l % 2 == 1:
                odd_consts(l // 2)
            for st in range(NST):
                ctx["l"], ctx["st"] = l, st
                src_h = x if first else hscr
                for j in range(TS // 128):
                    k.dma("sp", hs[:, j, :], src_h[st * TS + j * 128: st * TS + (j + 1) * 128, :],
                          reads=[b_hdram[st]] if not first else [], writes=[b_hs[j]], chan=f"hs{j}")
                if "mix" in parts:
                    load_gain(norm_mix_g[l:l + 1, :])
                    for j in range(TS // 128):
                        norm_tile(j)
                        norm_apply_T(j)
                    if l % 2 == 0:
                        even_mixer(l // 2)
                    else:
                        odd_mixer(l // 2, st)
                if "ffn" in parts:
                    load_gain(norm_ffn_g[l:l + 1, :])
                    for j in range(TS // 128):
                        norm_tile(j)
                        norm_apply_T(j)
                    ffn_supertile(l)
                if last and do_final:
                    load_gain(final_norm_g[0:1, :])
                    for j in range(TS // 128):
                        norm_tile(j)
                        k.op("dve", lambda e, j=j: e.scalar_tensor_tensor(
                            out=hs[:, j, :], in0=hs[:, j, :], scalar=stat[:, 2:3], in1=gB[:],
                            op0=ALU.mult, op1=ALU.mult),
                            reads=[b_hs[j], b_stat, b_gB], writes=[b_hs[j]])
                dst_h = out if last else hscr
                for j in range(TS // 128):
                    k.dma("sp", dst_h[st * TS + j * 128: st * TS + (j + 1) * 128, :], hs[:, j, :],
                          reads=[b_hs[j]], writes=[b_hdram[st]], chan=f"ho{j}")
            first = False
        k.wait_all("sp", b_hdram)

        with nc.Block() as block:
            @block.tensor
            def _(e):
                k.replay("pe", e)

            @block.scalar
            def _(e):
                k.replay("act", e)

            @block.vector
            def _(e):
                k.replay("dve", e)

            @block.gpsimd
            def _(e):
                k.replay("pool", e)

            @block.sync
            def _(e):
                k.replay("sp", e)
    return nc


def _bucket_table():
    d = np.arange(0, 257)
    dd = np.maximum(d, 1).astype(np.float32)
    large = 16 + (np.log(dd / np.float32(16)) / np.float32(math.log(128 / 16))
                  * np.float32(16)).astype(np.int32)
    large = np.minimum(large, 31)
    return np.where(d < 16, d, large)


def _oh_pad():
    bk = _bucket_table()
    oh = np.zeros((32, 384), np.float32)
    for m in range(127, 384):
        oh[bk[m - 127], m] = 1.0
    return oh


def make_in_map(inp, x):
    f = lambda a: np.ascontiguousarray(np.asarray(a, dtype=np.float32))
    return dict(
        x=f(x), norm_mix_g=f(inp["norm_mix_g"]), norm_ffn_g=f(inp["norm_ffn_g"]),
        final_norm_g=f(inp["final_norm_g"]).reshape(1, -1),
        ffn_w1=f(inp["ffn_w1"]), ffn_w2=f(inp["ffn_w2"]),
        ab_w_in=f(inp["ab_w_in"]), a_v_ln_g=f(inp["a_v_ln_g"]).reshape(2, 1, 1024),
        a_w_s=f(inp["a_w_s"]), a_b_s=f(inp["a_b_s"]).reshape(2, 1, 1024),
        b_gate_w2=f(inp["b_gate_w2"]), b_gate_b=f(inp["b_gate_b"]).reshape(2, 1, 512),
        b_out_norm_g=f(inp["b_out_norm_g"]).reshape(2, 1, 256), ab_w_out=f(inp["ab_w_out"]),
        c_w_in=f(inp["c_w_in"]), c_w_out=f(inp["c_w_out"]), rel_bias=f(inp["rel_bias"]),
        oh_pad=_oh_pad())


def kernel(**inputs):
    x = np.asarray(inputs["x"], dtype=np.float32)
    nc = build(dict(T=SEQ, layers=[0, 1, 2, 3]))
    in_maps = [make_in_map(inputs, x[c % BATCH]) for c in range(8)]
    res = run_bass_kernel_spmd(nc, in_maps, core_ids=list(range(8)))
    out = np.stack([np.asarray(res.results[b]["out"]) for b in range(BATCH)])
    return out.astype(np.float32)
```

```python
import math
from contextlib import ExitStack

import numpy as np
import concourse.bass as bass
import concourse.mybir as mybir
from concourse.bass_utils import run_bass_kernel_spmd

F32 = mybir.dt.float32
BF16 = mybir.dt.bfloat16
AF = mybir.ActivationFunctionType
ALU = mybir.AluOpType
AX = mybir.AxisListType

D = 2048
NDB = D // 128
DFF = 8192
NFB = DFF // 128
SEQ = 4096
BATCH = 4
DEPTH = 4
TS = 512
EPS = 1e-6
AB_IN = 5136
C_IN = 4176


class Buf:
    __slots__ = ("w", "r")

    def __init__(self):
        self.w = {}
        self.r = {}


def _merge(dst, src):
    for s, v in src.items():
        if dst.get(s, 0) < v:
            dst[s] = v


class KB:
    ENG = ("pe", "act", "dve", "pool", "sp")

    def __init__(self, nc, es):
        self.nc = nc
        self.es = es
        self.q = {e: [] for e in self.ENG}
        self.semh = {}
        self.semc = {}
        for e in ("pe", "act", "dve", "pool"):
            self.newsem(e)

    def newsem(self, name):
        if name not in self.semh:
            self.semh[name] = self.es.enter_context(self.nc.semaphore("s_" + name))
            self.semc[name] = 0
        return name

    def op(self, eng, fn, reads=(), writes=(), sem=None, inc=1):
        deps = {}
        for b in reads:
            _merge(deps, b.w)
        for b in writes:
            _merge(deps, b.w)
            _merge(deps, b.r)
        if sem is None:
            sem = eng
        if eng == "pe":
            deps.pop("pe", None)
        self.semc[sem] += inc
        val = self.semc[sem]
        self.q[eng].append((fn, deps, sem, inc))
        for b in reads:
            if b.r.get(sem, 0) < val:
                b.r[sem] = val
        for b in writes:
            b.w = {sem: val}
            b.r = {}
        return (sem, val)

    def dma(self, eng, out, in_, reads=(), writes=(), chan="d0"):
        self.newsem(chan)
        return self.op(eng, lambda e: e.dma_start(out=out, in_=in_), reads, writes,
                       sem=chan, inc=16)

    def wait_all(self, eng, bufs):
        deps = {}
        for b in bufs:
            _merge(deps, b.w)
            _merge(deps, b.r)
        self.q[eng].append((None, deps, None, 0))

    def replay(self, eng, e):
        waited = {}
        for fn, deps, sem, inc in self.q[eng]:
            for s, v in deps.items():
                if waited.get(s, 0) < v:
                    e.wait_ge(self.semh[s], v)
                    waited[s] = v
            if fn is not None:
                ins = fn(e)
                ins.then_inc(self.semh[sem], inc)


def build(cfg):
    T = cfg["T"]
    layers = cfg["layers"]
    parts = cfg.get("parts", ("mix", "ffn"))
    do_final = cfg.get("final", True)
    NST = T // TS
    nc = bass.Bass("TRN2", target_bir_lowering=False)

    def din(name, shape):
        return nc.dram_tensor(name, list(shape), F32, kind="ExternalInput").ap()

    x = din("x", [T, D])
    norm_mix_g = din("norm_mix_g", [DEPTH, D])
    norm_ffn_g = din("norm_ffn_g", [DEPTH, D])
    final_norm_g = din("final_norm_g", [1, D])
    ffn_w1 = din("ffn_w1", [DEPTH, D, DFF])
    ffn_w2 = din("ffn_w2", [DEPTH, DFF, D])
    ab_w_in = din("ab_w_in", [2, D, AB_IN])
    a_v_ln_g = din("a_v_ln_g", [2, 1, 1024])
    a_w_s = din("a_w_s", [2, 8, 128, 128])
    a_b_s = din("a_b_s", [2, 1, 1024])
    b_gate_w2 = din("b_gate_w2", [2, 16, 512])
    b_gate_b = din("b_gate_b", [2, 1, 512])
    b_out_norm_g = din("b_out_norm_g", [2, 1, 256])
    ab_w_out = din("ab_w_out", [2, D, D])
    c_w_in = din("c_w_in", [2, D, C_IN])
    c_w_out = din("c_w_out", [2, D, D])
    rel_bias = din("rel_bias", [32, 16])
    oh_pad = din("oh_pad", [32, 384])
    kT_d = nc.dram_tensor("kT_d", [4, 128, T], BF16, kind="Internal").ap()
    v_d = nc.dram_tensor("v_d", [T, 512], BF16, kind="Internal").ap()
    fpad_d = nc.dram_tensor("fpad_d", [16, 384], F32, kind="Internal").ap()
    out = nc.dram_tensor("out", [T, D], F32, kind="ExternalOutput").ap()
    hscr = nc.dram_tensor("hscr", [T, D], F32, kind="Internal").ap()

    NCHK = 136
    wscr = [nc.dram_tensor(f"wscr{i}", [NCHK, 128, 4096], BF16, kind="Internal").ap()
            for i in range(DEPTH)]
    wreg = {}
    ctx = {"l": 0, "st": 0}

    es = ExitStack()
    with es:
        k = KB(nc, es)

        def cached_load(slot_t, slot_buf, slot_id, key, kind, sub, cast_loads):
            l, st = ctx["l"], ctx["st"]
            if (l, key) not in wreg:
                wreg[(l, key)] = (sum(1 for (ll, _) in wreg if ll == l), Buf())
            idx, wb = wreg[(l, key)]
            assert idx < NCHK
            if kind == "w1":
                scr = wscr[l][idx].rearrange("p (b c) -> p b c", b=16)
            else:
                scr = wscr[l][idx][:, 0:2048].rearrange("p (b c) -> p b c", b=4)
            if st == 0:
                cast_loads()
                k.dma("sp", sub(scr), sub(slot_t), reads=[slot_buf], writes=[wb],
                      chan=f"wo_{kind}{slot_id}")
            else:
                k.dma("sp", sub(slot_t), sub(scr), reads=[wb], writes=[slot_buf],
                      chan=f"wh_{kind}{slot_id}")

        def sb(name, shape, dt):
            return es.enter_context(nc.sbuf_tensor(name, list(shape), dt))

        ps = [es.enter_context(nc.psum_tensor(f"ps{i}", [128, 512], F32)) for i in range(8)]
        psb = [Buf() for _ in range(8)]
        ps_rr = [0]

        reserved = set()

        def next_bank():
            while True:
                i = ps_rr[0]
                ps_rr[0] = (i + 1) % 8
                if i not in reserved:
                    return i

        def reserve_bank():
            i = next_bank()
            reserved.add(i)
            return i

        ident = sb("ident", [128, 128], BF16)
        identf = sb("identf", [128, 128], F32)
        b_ident = Buf()
        hs = sb("hs", [128, TS // 128, D], F32)
        b_hs = [Buf() for _ in range(TS // 128)]
        gB = sb("gB", [128, D], F32)
        b_gB = Buf()
        hn = sb("hn", [128, D], BF16)
        b_hn = Buf()
        junk = sb("junk", [128, 256], BF16)
        b_junk = Buf()
        stat = sb("stat", [128, 8], F32)
        b_stat = Buf()
        hnT = sb("hnT", [128, NDB, TS], BF16)
        b_hnT = [Buf() for _ in range(TS // 128)]
        actT = sb("actT", [128, NFB, TS], BF16)
        b_actT = [Buf() for _ in range(NFB)]
        arena = actT[:, :, :].rearrange("p a b -> p (a b)")

        def carve(off, n, pat=None, **kw):
            v = arena[:, off:off + n]
            return v.rearrange(pat, **kw) if pat else v
        a_uT = carve(0, 4096, "p (g t) -> p g t", g=8)
        v_sgu = carve(4096, 4096, "p (j c) -> p j c", j=4)
        v_gla = carve(8192, 4096, "p (j c) -> p j c", j=4)
        r_s = carve(12288, 4096, "p (j c) -> p j c", j=4)
        catT = carve(16384, 8192, "p (b t) -> p b t", b=16)
        qe = carve(24576, 2048, "p (h t) -> p h t", h=4)
        ke = carve(26624, 2048, "p (h t) -> p h t", h=4)
        ks = carve(28672, 2048, "p (h t) -> p h t", h=4)
        ksT = carve(30720, 2048, "p (h j d) -> p h j d", h=4, j=4)
        b_auT = [Buf() for _ in range(8)]
        b_vsgu = [Buf() for _ in range(4)]
        b_vgla = [Buf() for _ in range(4)]
        b_rs = [Buf() for _ in range(4)]
        b_catT = [Buf() for _ in range(16)]
        b_qe = [Buf() for _ in range(4)]
        b_ke = [Buf() for _ in range(4)]
        b_ks = [Buf() for _ in range(4)]
        b_ksT = [Buf() for _ in range(4)]
        mix_bufs = b_auT + b_vsgu + b_vgla + b_rs + b_catT + b_qe + b_ke + b_ks + b_ksT
        arena2 = sb("arena2", [128, 18432], BF16)

        def carve2(off_b, nbytes, dt, pat=None, **kw):
            v = arena2[:, off_b // 2:(off_b + nbytes) // 2]
            if dt == F32:
                v = v.bitcast(F32)
            return v.rearrange(pat, **kw) if pat else v
        E1 = carve2(0, 4096, BF16, "p (h t) -> p h t", h=4)
        E2 = carve2(4096, 4096, BF16, "p (h t) -> p h t", h=4)
        b_E = [Buf() for _ in range(4)]
        explast = sb("explast", [128, 4, 4], F32)
        spt = [carve2(8192 + i * 2048, 2048, F32) for i in range(2)]
        b_spt = [Buf() for _ in range(2)]
        tmpf = [carve2(12288 + i * 2048, 2048, F32) for i in range(2)]
        b_tmpf = [Buf() for _ in range(2)]
        tmpf_rr = [0]
        scTm = carve2(16384, 1024, BF16, "p (h t) -> p h t", h=4)
        b_scTm = Buf()
        Sst = carve2(17408, 4096, F32, "p (h v) -> p h v", h=4)
        Sbf = carve2(21504, 2048, BF16, "p (h v) -> p h v", h=4)
        b_S = [Buf() for _ in range(4)]
        b_Sbf = [Buf() for _ in range(4)]
        lnB = carve2(23552, 4096, F32)
        ongB = carve2(27648, 1024, F32)
        WT = carve2(28672, 2048, BF16, "p (g t) -> p g t", g=8)
        bout = carve2(30720, 2048, BF16)
        b_bout = Buf()
        bs_row = sb("bs_row", [1, 1024], BF16)
        W2ext = sb("W2ext", [32, 512], BF16)
        even2_bufs = b_E + b_spt + b_tmpf + [b_scTm] + b_S + b_Sbf + [b_bout]
        Iacc = carve2(0, 16384, F32)
        b_I = Buf()
        ikT = carve2(16384, 8192, BF16)
        b_ikT = [Buf() for _ in range(8)]
        BTn = carve2(24576, 8192, BF16, "p (h t) -> p h t", h=16)
        b_BTn = Buf()
        PTb = [carve2(32768 + i * 1024, 1024, BF16) for i in range(2)]
        PMb = [carve2(34816 + i * 1024, 1024, BF16) for i in range(2)]
        b_PT = [Buf() for _ in range(2)]
        b_PM = [Buf() for _ in range(2)]
        odd2_bufs = [b_I] + b_ikT + [b_BTn] + b_PT + b_PM
        b_lc = Buf()
        glr_ext = sb("glr_ext", [32, TS], BF16)
        b_glr = Buf()
        ones_row = sb("ones_row", [1, 128], BF16)
        onec = sb("onec", [128, 1], F32)
        triNeg = sb("triNeg", [128, 128], F32)
        maskT = sb("maskT", [128, 4, 128], BF16)
        cf = sb("cf", [128, 128], F32)
        st2 = sb("st2", [128, 16], F32)
        b_st2 = Buf()

        def alias_fence(src, dst):
            tok = {}
            for b in src:
                _merge(tok, b.w)
                _merge(tok, b.r)
            for b in dst:
                _merge(b.r, tok)
        rtmp = [sb(f"rtmp{i}", [128, TS], F32) for i in range(2)]
        b_rtmp = [Buf() for _ in range(2)]
        NW1 = 2
        W1C = 256
        w1s = [sb(f"w1s{i}", [128, NDB, W1C], BF16) for i in range(NW1)]
        b_w1s = [Buf() for _ in range(NW1)]
        NW2 = 3
        W2R = 4
        w2s = [sb(f"w2s{i}", [128, W2R, 512], BF16) for i in range(NW2)]
        b_w2s = [Buf() for _ in range(NW2)]
        w1_rr = [0]
        w2_rr = [0]
        b_hdram = [Buf() for _ in range(NST)]

        epsc = sb("epsc", [128, 1], F32)
        k.op("pool", lambda e: e.memset(epsc[:], EPS), writes=[b_ident])
        k.op("pool", lambda e: e.memset(identf[:], 0.0), writes=[b_ident])
        k.op("pool", lambda e: e.affine_select(
            out=identf[:], in_=identf[:], pattern=[[-1, 128]], compare_op=ALU.not_equal,
            fill=1.0, base=0, channel_multiplier=1), reads=[b_ident], writes=[b_ident])
        k.op("dve", lambda e: e.tensor_copy(out=ident[:], in_=identf[:]),
             reads=[b_ident], writes=[b_ident])

        k.op("pool", lambda e: e.memset(onec[:], 1.0), writes=[b_ident])
        k.op("pool", lambda e: e.memset(ones_row[:], 1.0), writes=[b_ident])
        k.op("pool", lambda e: e.memset(glr_ext[:], 1.0), writes=[b_glr])
        k.op("pool", lambda e: e.memset(triNeg[:], -1.0 / 16.0), writes=[b_ident])
        k.op("pool", lambda e: e.affine_select(
            out=triNeg[:], in_=triNeg[:], pattern=[[1, 128]], compare_op=ALU.is_ge,
            fill=0.0, base=0, channel_multiplier=-1), reads=[b_ident], writes=[b_ident])
        k.op("pool", lambda e: e.memset(cf[:], 1.0), writes=[b_ident])
        k.op("pool", lambda e: e.affine_select(
            out=cf[:], in_=cf[:], pattern=[[1, 128]], compare_op=ALU.is_ge,
            fill=0.0, base=0, channel_multiplier=-1), reads=[b_ident], writes=[b_ident])
        for hh in range(4):
            k.op("dve", lambda e, hh=hh: e.tensor_copy(out=maskT[:, hh, :], in_=cf[:]),
                 reads=[b_ident], writes=[b_ident])
        def load_gain(g_ap):
            k.dma("sp", gB[:], g_ap.partition_broadcast(128), writes=[b_gB], chan="gB")

        def norm_tile(j, dst_T=True):
            k.op("act", lambda e: e.activation(out=hn[:], in_=hs[:, j, :], func=AF.Square,
                                               accum_out=stat[:, 0:1]),
                 reads=[b_hs[j]], writes=[b_hn, b_stat])
            k.op("act", lambda e: e.activation(out=stat[:, 1:2], in_=stat[:, 0:1], func=AF.Sqrt,
                                               bias=epsc[:, 0:1], scale=1.0 / D),
                 reads=[b_stat, b_ident], writes=[b_stat])
            k.op("dve", lambda e: e.reciprocal(out=stat[:, 2:3], in_=stat[:, 1:2]),
                 reads=[b_stat], writes=[b_stat])

        def norm_apply_T(j):
            k.op("dve", lambda e: e.scalar_tensor_tensor(
                out=hn[:], in0=hs[:, j, :], scalar=stat[:, 2:3], in1=gB[:],
                op0=ALU.mult, op1=ALU.mult),
                reads=[b_hs[j], b_stat, b_gB], writes=[b_hn])
            for half in range(2):
                bi = next_bank()
                pt = ps[bi].bitcast(BF16)
                for q in range(8):
                    db = half * 8 + q
                    k.op("pe", lambda e, db=db, q=q, pt=pt: e.transpose(
                        out=pt[:, q * 128:(q + 1) * 128], in_=hn[:, db * 128:(db + 1) * 128],
                        identity=ident[:]),
                        reads=[b_hn, b_ident], writes=[psb[bi]])
                eng = "act" if half == 0 else "dve"
                src = pt[:, :].rearrange("p (q t) -> p q t", q=8)
                dst = hnT[:, half * 8:(half + 1) * 8, j * 128:(j + 1) * 128]
                if eng == "act":
                    k.op("act", lambda e, src=src, dst=dst: e.copy(out=dst, in_=src),
                         reads=[psb[bi]], writes=[b_hnT[j]])
                else:
                    k.op("dve", lambda e, src=src, dst=dst: e.tensor_copy(out=dst, in_=src),
                         reads=[psb[bi]], writes=[b_hnT[j]])

        def ffn_supertile(l):
            NTT = TS // 128
            for c in range(DFF // W1C):
                s = w1_rr[0]
                w1_rr[0] = (s + 1) % NW1
                src = ffn_w1[l, :, c * W1C:(c + 1) * W1C].rearrange("(b p) c -> p b c", p=128)
                cached_load(w1s[s], b_w1s[s], s, ("w1", c), "w1", lambda v: v[:, :, :],
                            lambda s=s, src=src: k.dma("pool", w1s[s][:], src, writes=[b_w1s[s]],
                                                       chan=f"w1_{s}"))
                for fb in range(W1C // 128):
                    ffb = c * (W1C // 128) + fb
                    bi = next_bank()
                    for db in range(NDB):
                        k.op("pe", lambda e, s=s, fb=fb, db=db, bi=bi: e.matmul(
                            ps[bi][:, :], lhsT=w1s[s][:, db, fb * 128:(fb + 1) * 128],
                            rhs=hnT[:, db, :], start=(db == 0), stop=(db == NDB - 1)),
                            reads=[b_w1s[s]] + b_hnT, writes=[psb[bi]])
                    r = ffb % 2
                    k.op("act", lambda e, bi=bi, r=r: e.activation(
                        out=rtmp[r][:], in_=ps[bi][:, :], func=AF.Relu),
                        reads=[psb[bi]], writes=[b_rtmp[r]])
                    k.op("dve", lambda e, r=r, ffb=ffb: e.tensor_tensor(
                        out=actT[:, ffb, :], in0=rtmp[r][:], in1=rtmp[r][:], op=ALU.mult),
                        reads=[b_rtmp[r]], writes=[b_actT[ffb]])
            for dq in range(D // 512):
                banks = [next_bank() for _ in range(NTT)]
                for c in range(NFB // W2R):
                    s = w2_rr[0]
                    w2_rr[0] = (s + 1) % NW2
                    src = ffn_w2[l, c * W2R * 128:(c + 1) * W2R * 128,
                                 dq * 512:(dq + 1) * 512].rearrange("(b p) c -> p b c", p=128)
                    cached_load(w2s[s], b_w2s[s], s, ("w2", dq, c), "w2", lambda v: v[:, :, :],
                                lambda s=s, src=src: k.dma("pool", w2s[s][:], src, writes=[b_w2s[s]],
                                                           chan=f"w2_{s}"))
                    for fr in range(W2R):
                        ffb = c * W2R + fr
                        for j in range(NTT):
                            bi = banks[j]
                            k.op("pe", lambda e, s=s, fr=fr, ffb=ffb, j=j, bi=bi: e.matmul(
                                ps[bi][:, :], lhsT=actT[:, ffb, j * 128:(j + 1) * 128],
                                rhs=w2s[s][:, fr, :], start=(ffb == 0), stop=(ffb == NFB - 1)),
                                reads=[b_w2s[s], b_actT[ffb]], writes=[psb[bi]])
                for j in range(NTT):
                    bi = banks[j]
                    k.op("dve", lambda e, j=j, bi=bi, dq=dq: e.tensor_tensor(
                        out=hs[:, j, dq * 512:(dq + 1) * 512], in0=ps[bi][:, :],
                        in1=hs[:, j, dq * 512:(dq + 1) * 512], op=ALU.add),
                        reads=[psb[bi], b_hs[j]], writes=[b_hs[j]])


        lnsc = sb("lnsc", [128, 1], F32)
        k.op("pool", lambda e: e.memset(lnsc[:], -0.5 * math.log(128.0)), writes=[b_ident])

        def tm_tmp():
            r = tmpf_rr[0]
            tmpf_rr[0] = 1 - r
            return r

        def wchunk(w2d, c0, ncols, key=None):
            s_ = w1_rr[0]
            w1_rr[0] = (s_ + 1) % NW1
            if key is None:
                key = ("in", c0)
            cached_load(w1s[s_], b_w1s[s_], s_, key, "w1", lambda v: v[:, :, 0:ncols],
                        lambda: k.dma("pool", w1s[s_][:, :, 0:ncols],
                                      w2d[:, c0:c0 + ncols].rearrange("(b p) c -> p b c", p=128),
                                      writes=[b_w1s[s_]], chan=f"w1_{s_}"))
            return s_

        def proj_fm(w2d, c0, ncols, evac):
            s_ = wchunk(w2d, c0, ncols)
            nb = (ncols + 127) // 128
            for fb in range(nb):
                m = min(128, ncols - fb * 128)
                bi = next_bank()
                for db in range(NDB):
                    k.op("pe", lambda e, s_=s_, fb=fb, db=db, bi=bi, m=m: e.matmul(
                        ps[bi][0:m, :], lhsT=w1s[s_][:, db, fb * 128:fb * 128 + m],
                        rhs=hnT[:, db, :], start=(db == 0), stop=(db == NDB - 1)),
                        reads=[b_w1s[s_]] + b_hnT, writes=[psb[bi]])
                evac(fb, bi)

        def proj_tm(w2d, c0, evac):
            s_ = wchunk(w2d, c0, 256)
            for j in range(4):
                bi = next_bank()
                for db in range(NDB):
                    k.op("pe", lambda e, s_=s_, j=j, db=db, bi=bi: e.matmul(
                        ps[bi][:, 0:256], lhsT=hnT[:, db, j * 128:(j + 1) * 128],
                        rhs=w1s[s_][:, db, 0:256], start=(db == 0), stop=(db == NDB - 1)),
                        reads=[b_w1s[s_], b_hnT[j]], writes=[psb[bi]])
                evac(j, bi)

        def even_consts(i):
            k.dma("pool", W2ext[0:16, :], b_gate_w2[i], writes=[b_lc], chan="lc")
            k.dma("pool", W2ext[16:17, :], b_gate_b[i], writes=[b_lc], chan="lc")
            k.dma("sp", lnB[:], a_v_ln_g[i].partition_broadcast(128), writes=[b_lc], chan="lcs")
            k.dma("sp", ongB[:], b_out_norm_g[i].partition_broadcast(128), writes=[b_lc], chan="lcs")
            k.dma("pool", bs_row[:], a_b_s[i], writes=[b_lc], chan="lc")
            for half in range(2):
                k.dma("sp", rtmp[half][:].rearrange("p (g s) -> p g s", g=4),
                      a_w_s[i, half * 4:(half + 1) * 4].rearrange("g t s -> t g s"),
                      writes=[b_rtmp[half]], chan=f"rt{half}")
                bi = next_bank()
                for gq in range(4):
                    k.op("pe", lambda e, half=half, gq=gq, bi=bi: e.transpose(
                        out=ps[bi][:, gq * 128:(gq + 1) * 128],
                        in_=rtmp[half][:, gq * 128:(gq + 1) * 128], identity=identf[:]),
                        reads=[b_rtmp[half], b_ident], writes=[psb[bi]])
                r_ = tm_tmp()
                k.op("act", lambda e, r_=r_, bi=bi: e.copy(out=tmpf[r_][:], in_=ps[bi][:, :]),
                     reads=[psb[bi]], writes=[b_tmpf[r_]])
                v3 = tmpf[r_][:].rearrange("p (g t) -> p g t", g=4)
                k.op("pool", lambda e, v3=v3: e.affine_select(
                    out=v3, in_=v3, pattern=[[0, 4], [1, 128]], compare_op=ALU.is_ge,
                    fill=0.0, base=0, channel_multiplier=-1),
                    reads=[b_tmpf[r_]], writes=[b_tmpf[r_]])
                k.op("dve", lambda e, v3=v3, half=half: e.tensor_copy(
                    out=WT[:, half * 4:(half + 1) * 4, :], in_=v3),
                    reads=[b_tmpf[r_]], writes=[b_lc])
            k.op("pool", lambda e: e.memset(Sst[:], 0.0), writes=b_S)
            k.op("pool", lambda e: e.memset(Sbf[:], 0.0), writes=b_Sbf)

        def even_mixer(i):
            w_in = ab_w_in[i]
            w_out = ab_w_out[i]
            alias_fence(b_actT, mix_bufs)
            def ev_glr(fb, bi):
                k.op("act", lambda e, bi=bi: e.copy(out=glr_ext[0:16, :], in_=ps[bi][0:16, :]),
                     reads=[psb[bi]], writes=[b_glr])
            proj_fm(w_in, 5120, 16, ev_glr)
            cumb = [reserve_bank() for _ in range(4)]
            for j in range(4):
                bg = next_bank()
                k.op("pe", lambda e, j=j, bg=bg: e.matmul(
                    ps[bg][:, :], lhsT=glr_ext[0:17, j * 128:(j + 1) * 128], rhs=W2ext[0:17, :],
                    start=True, stop=True), reads=[b_glr, b_lc], writes=[psb[bg]])
                sj = j % 2
                k.op("act", lambda e, sj=sj, bg=bg: e.activation(
                    out=spt[sj][:], in_=ps[bg][:, :], func=AF.Exp, scale=-1.0),
                    reads=[psb[bg]], writes=[b_spt[sj]])
                k.op("act", lambda e, sj=sj: e.activation(
                    out=spt[sj][:], in_=spt[sj][:], func=AF.Ln, bias=onec[:, 0:1], scale=1.0),
                    reads=[b_spt[sj], b_ident], writes=[b_spt[sj]])
                for hh in range(4):
                    k.op("pe", lambda e, sj=sj, hh=hh, j=j: e.matmul(
                        ps[cumb[hh]][:, j * 128:(j + 1) * 128],
                        lhsT=spt[sj][:, hh * 128:(hh + 1) * 128], rhs=triNeg[:, :],
                        start=True, stop=True),
                        reads=[b_spt[sj], b_ident], writes=[psb[cumb[hh]]])
            for hh in range(4):
                cb = cumb[hh]
                k.op("act", lambda e, hh=hh, cb=cb: e.activation(
                    out=E1[:, hh, :], in_=ps[cb][:, :], func=AF.Exp, bias=lnsc[:, 0:1], scale=1.0),
                    reads=[psb[cb], b_ident], writes=[b_E[hh]])
                k.op("act", lambda e, hh=hh, cb=cb: e.activation(
                    out=E2[:, hh, :], in_=ps[cb][:, :], func=AF.Exp, scale=-1.0),
                    reads=[psb[cb]], writes=[b_E[hh]])
                k.op("act", lambda e, hh=hh, cb=cb: e.activation(
                    out=explast[:, hh, :], in_=ps[cb][:, 127:512:128], func=AF.Exp),
                    reads=[psb[cb]], writes=[b_E[hh]])
                reserved.discard(cb)
            for c in range(2):
                def ev_q(fb, bi, c=c):
                    hh = c * 2 + fb
                    k.op("dve", lambda e, hh=hh, bi=bi: e.tensor_tensor(
                        out=qe[:, hh, :], in0=ps[bi][:, :], in1=E1[:, hh, :], op=ALU.mult),
                        reads=[psb[bi], b_E[hh]], writes=[b_qe[hh]])
                proj_fm(w_in, 2048 + c * 256, 256, ev_q)
            for c in range(2):
                def ev_k(fb, bi, c=c):
                    hh = c * 2 + fb
                    k.op("dve", lambda e, hh=hh, bi=bi: e.tensor_tensor(
                        out=ke[:, hh, :], in0=ps[bi][:, :], in1=E2[:, hh, :], op=ALU.mult),
                        reads=[psb[bi], b_E[hh]], writes=[b_ke[hh]])
                    for j in range(4):
                        k.op("dve", lambda e, hh=hh, bi=bi, j=j: e.scalar_tensor_tensor(
                            out=ks[:, hh, j * 128:(j + 1) * 128], in0=ps[bi][:, j * 128:(j + 1) * 128],
                            scalar=explast[:, hh, j:j + 1], in1=E2[:, hh, j * 128:(j + 1) * 128],
                            op0=ALU.mult, op1=ALU.mult),
                            reads=[psb[bi], b_E[hh]], writes=[b_ks[hh]])
                    bt = next_bank()
                    pt = ps[bt].bitcast(BF16)
                    for j in range(4):
                        k.op("pe", lambda e, hh=hh, j=j, pt=pt: e.transpose(
                            out=pt[:, j * 128:(j + 1) * 128], in_=ks[:, hh, j * 128:(j + 1) * 128],
                            identity=ident[:]), reads=[b_ks[hh], b_ident], writes=[psb[bt]])
                    k.op("act", lambda e, hh=hh, pt=pt: e.copy(
                        out=ksT[:, hh, :, :], in_=pt[:, 0:512].rearrange("p (j d) -> p j d", j=4)),
                        reads=[psb[bt]], writes=[b_ksT[hh]])
                proj_fm(w_in, 2560 + c * 256, 256, ev_k)
            for c in range(4):
                def ev_u(fb, bi, c=c):
                    g = c * 2 + fb
                    k.op("act", lambda e, g=g, bi=bi: e.activation(
                        out=a_uT[:, g, :], in_=ps[bi][:, :], func=AF.Gelu),
                        reads=[psb[bi]], writes=[b_auT[g]])
                proj_fm(w_in, c * 256, 256, ev_u)
            for c in range(4):
                def ev_v(j, bi, c=c):
                    for gg in range(2):
                        g = c * 2 + gg
                        r_ = tm_tmp()
                        k.op("act", lambda e, r_=r_, bi=bi, gg=gg: e.activation(
                            out=tmpf[r_][:, 0:128], in_=ps[bi][:, gg * 128:(gg + 1) * 128],
                            func=AF.Gelu, accum_out=st2[:, 0:1]),
                            reads=[psb[bi]], writes=[b_tmpf[r_], b_st2])
                        k.op("act", lambda e, r_=r_: e.activation(
                            out=junk[:, 0:128], in_=tmpf[r_][:, 0:128], func=AF.Square,
                            accum_out=st2[:, 1:2]),
                            reads=[b_tmpf[r_]], writes=[b_junk, b_st2])
                        k.op("dve", lambda e: e.tensor_scalar(
                            out=st2[:, 2:3], in0=st2[:, 0:1], scalar1=1.0 / 128, scalar2=None,
                            op0=ALU.mult), reads=[b_st2], writes=[b_st2])
                        k.op("dve", lambda e: e.tensor_tensor(
                            out=st2[:, 3:4], in0=st2[:, 2:3], in1=st2[:, 2:3], op=ALU.mult),
                            reads=[b_st2], writes=[b_st2])
                        k.op("dve", lambda e: e.scalar_tensor_tensor(
                            out=st2[:, 4:5], in0=st2[:, 1:2], scalar=1.0 / 128, in1=st2[:, 3:4],
                            op0=ALU.mult, op1=ALU.subtract), reads=[b_st2], writes=[b_st2])
                        k.op("act", lambda e: e.activation(
                            out=st2[:, 5:6], in_=st2[:, 4:5], func=AF.Sqrt, bias=epsc[:, 0:1],
                            scale=1.0), reads=[b_st2, b_ident], writes=[b_st2])
                        k.op("dve", lambda e: e.reciprocal(out=st2[:, 6:7], in_=st2[:, 5:6]),
                             reads=[b_st2], writes=[b_st2])
                        k.op("dve", lambda e, r_=r_: e.tensor_scalar(
                            out=tmpf[r_][:, 0:128], in0=tmpf[r_][:, 0:128], scalar1=st2[:, 2:3],
                            scalar2=st2[:, 6:7], op0=ALU.subtract, op1=ALU.mult),
                            reads=[b_tmpf[r_], b_st2], writes=[b_tmpf[r_]])
                        k.op("dve", lambda e, r_=r_, g=g, j=j: e.tensor_tensor(
                            out=v_sgu[:, j, g * 128:(g + 1) * 128], in0=tmpf[r_][:, 0:128],
                            in1=lnB[:, g * 128:(g + 1) * 128], op=ALU.mult),
                            reads=[b_tmpf[r_], b_lc], writes=[b_vsgu[j]])
                proj_tm(w_in, 1024 + c * 256, ev_v)
            for c in range(4):
                def ev_vg(j, bi, c=c):
                    k.op("act", lambda e, j=j, bi=bi, c=c: e.copy(
                        out=v_gla[:, j, c * 256:(c + 1) * 256], in_=ps[bi][:, 0:256]),
                        reads=[psb[bi]], writes=[b_vgla[j]])
                proj_tm(w_in, 3072 + c * 256, ev_vg)
            for c in range(4):
                def ev_r(j, bi, c=c):
                    k.op("act", lambda e, j=j, bi=bi, c=c: e.activation(
                        out=r_s[:, j, c * 256:(c + 1) * 256], in_=ps[bi][:, 0:256], func=AF.Silu),
                        reads=[psb[bi]], writes=[b_rs[j]])
                proj_tm(w_in, 4096 + c * 256, ev_r)
            for g in range(8):
                bi = next_bank()
                for j in range(4):
                    k.op("pe", lambda e, g=g, j=j, bi=bi: e.matmul(
                        ps[bi][:, j * 128:(j + 1) * 128], lhsT=v_sgu[:, j, g * 128:(g + 1) * 128],
                        rhs=WT[:, g, :], start=True, stop=False),
                        reads=[b_vsgu[j], b_lc], writes=[psb[bi]])
                    k.op("pe", lambda e, g=g, j=j, bi=bi: e.matmul(
                        ps[bi][:, j * 128:(j + 1) * 128], lhsT=ones_row[0:1, :],
                        rhs=bs_row[0:1, g * 128:(g + 1) * 128], start=False, stop=True),
                        reads=[b_ident, b_lc], writes=[psb[bi]])
                k.op("dve", lambda e, g=g, bi=bi: e.tensor_tensor(
                    out=catT[:, g, :], in0=ps[bi][:, :], in1=a_uT[:, g, :], op=ALU.mult),
                    reads=[psb[bi], b_auT[g]], writes=[b_catT[g]])
            for j in range(4):
                jc = slice(j * 128, (j + 1) * 128)
                bs_ = next_bank()
                for hh in range(4):
                    k.op("pe", lambda e, hh=hh, jc=jc, bs_=bs_: e.matmul(
                        ps[bs_][:, hh * 128:(hh + 1) * 128], lhsT=ke[:, hh, jc], rhs=qe[:, hh, jc],
                        start=True, stop=True), reads=[b_ke[hh], b_qe[hh]], writes=[psb[bs_]])
                k.op("dve", lambda e, bs_=bs_: e.tensor_tensor(
                    out=scTm[:, :, :].rearrange("p h t -> p (h t)"), in0=ps[bs_][:, :],
                    in1=maskT[:, :, :].rearrange("p h t -> p (h t)"), op=ALU.mult),
                    reads=[psb[bs_], b_ident], writes=[b_scTm])
                for hp in range(2):
                    bo = next_bank()
                    for hq in range(2):
                        hh = hp * 2 + hq
                        oc = slice(hq * 256, (hq + 1) * 256)
                        vc = slice(hh * 256, (hh + 1) * 256)
                        k.op("pe", lambda e, hh=hh, oc=oc, vc=vc, bo=bo, j=j: e.matmul(
                            ps[bo][:, oc], lhsT=scTm[:, hh, :], rhs=v_gla[:, j, vc],
                            start=True, stop=False),
                            reads=[b_scTm, b_vgla[j]], writes=[psb[bo]])
                        k.op("pe", lambda e, hh=hh, oc=oc, bo=bo, jc=jc: e.matmul(
                            ps[bo][:, oc], lhsT=qe[:, hh, jc], rhs=Sbf[:, hh, :],
                            start=False, stop=True),
                            reads=[b_qe[hh], b_Sbf[hh]], writes=[psb[bo]])
                    for hq in range(2):
                        hh = hp * 2 + hq
                        oc = slice(hq * 256, (hq + 1) * 256)
                        vc = slice(hh * 256, (hh + 1) * 256)
                        k.op("act", lambda e, oc=oc, bo=bo: e.activation(
                            out=junk[:, 0:256], in_=ps[bo][:, oc], func=AF.Square,
                            accum_out=st2[:, 8:9]), reads=[psb[bo]], writes=[b_junk, b_st2])
                        k.op("act", lambda e: e.activation(
                            out=st2[:, 9:10], in_=st2[:, 8:9], func=AF.Sqrt, bias=epsc[:, 0:1],
                            scale=1.0 / 256), reads=[b_st2, b_ident], writes=[b_st2])
                        k.op("dve", lambda e: e.reciprocal(out=st2[:, 10:11], in_=st2[:, 9:10]),
                             reads=[b_st2], writes=[b_st2])
                        r_ = tm_tmp()
                        k.op("dve", lambda e, r_=r_, oc=oc, bo=bo: e.scalar_tensor_tensor(
                            out=tmpf[r_][:, 0:256], in0=ps[bo][:, oc], scalar=st2[:, 10:11],
                            in1=ongB[:, :], op0=ALU.mult, op1=ALU.mult),
                            reads=[psb[bo], b_st2, b_lc], writes=[b_tmpf[r_]])
                        k.op("dve", lambda e, r_=r_, vc=vc, j=j: e.tensor_tensor(
                            out=bout[:, vc], in0=tmpf[r_][:, 0:256], in1=r_s[:, j, vc], op=ALU.mult),
                            reads=[b_tmpf[r_], b_rs[j]], writes=[b_bout])
                for hp in range(2):
                    bk = next_bank()
                    for hq in range(2):
                        hh = hp * 2 + hq
                        oc = slice(hq * 256, (hq + 1) * 256)
                        vc = slice(hh * 256, (hh + 1) * 256)
                        k.op("pe", lambda e, hh=hh, oc=oc, vc=vc, bk=bk, j=j: e.matmul(
                            ps[bk][:, oc], lhsT=ksT[:, hh, j, :], rhs=v_gla[:, j, vc],
                            start=True, stop=True),
                            reads=[b_ksT[hh], b_vgla[j]], writes=[psb[bk]])
                        k.op("dve", lambda e, hh=hh, oc=oc, bk=bk, j=j: e.scalar_tensor_tensor(
                            out=Sst[:, hh, :], in0=Sst[:, hh, :], scalar=explast[:, hh, j:j + 1],
                            in1=ps[bk][:, oc], op0=ALU.mult, op1=ALU.add),
                            reads=[psb[bk], b_S[hh], b_E[hh]], writes=[b_S[hh]])
                        k.op("act", lambda e, hh=hh: e.copy(out=Sbf[:, hh, :], in_=Sst[:, hh, :]),
                             reads=[b_S[hh]], writes=[b_Sbf[hh]])
                bt = next_bank()
                pt = ps[bt].bitcast(BF16)
                for q8 in range(8):
                    k.op("pe", lambda e, q8=q8, pt=pt: e.transpose(
                        out=pt[:, q8 * 128:(q8 + 1) * 128], in_=bout[:, q8 * 128:(q8 + 1) * 128],
                        identity=ident[:]), reads=[b_bout, b_ident], writes=[psb[bt]])
                k.op("act", lambda e, pt=pt, jc=jc: e.copy(
                    out=catT[:, 8:16, jc], in_=pt[:, :].rearrange("p (q t) -> p q t", q=8)),
                    reads=[psb[bt]], writes=b_catT[8:16])
            for cq in range(8):
                s_ = wchunk(w_out, cq * 256, 256, key=("out", cq))
                for j in range(4):
                    bi = next_bank()
                    for cb in range(16):
                        k.op("pe", lambda e, s_=s_, j=j, cb=cb, bi=bi: e.matmul(
                            ps[bi][:, 0:256], lhsT=catT[:, cb, j * 128:(j + 1) * 128],
                            rhs=w1s[s_][:, cb, 0:256], start=(cb == 0), stop=(cb == 15)),
                            reads=[b_w1s[s_], b_catT[cb]], writes=[psb[bi]])
                    k.op("dve", lambda e, j=j, bi=bi, cq=cq: e.tensor_tensor(
                        out=hs[:, j, cq * 256:(cq + 1) * 256], in0=ps[bi][:, 0:256],
                        in1=hs[:, j, cq * 256:(cq + 1) * 256], op=ALU.add),
                        reads=[psb[bi], b_hs[j]], writes=[b_hs[j]])
            alias_fence(mix_bufs, b_actT)


        qT = carve(0, 8192, "p (h t) -> p h t", h=16)
        iqT = carve(8192, 4096, "p (b t) -> p b t", b=8)
        selT = carve(12288, 16384, "p (k t) -> p k t", k=32)
        vn = carve(28672, 2048, "p (j c) -> p j c", j=4)
        kTn = carve(30720, 2048, "p (g t) -> p g t", g=4)
        b_qT = [Buf() for _ in range(16)]
        b_iqT = [Buf() for _ in range(8)]
        b_selT = [Buf() for _ in range(4)]
        b_vn = [Buf() for _ in range(4)]
        b_kTn = [Buf() for _ in range(4)]
        odd_bufs = b_qT + b_iqT + b_selT + b_vn + b_kTn
        iw_t = sb("iw_t", [128, 4, 16], F32)
        b_iw = [Buf() for _ in range(4)]
        thr = sb("thr", [128, 16], F32)
        b_thr = Buf()
        halfc = sb("halfc", [128, 1], F32)
        negtri = sb("negtri", [128, 128], F32)
        rb = sb("rb", [32, 16], F32)
        OHs = sb("OHs", [32, 384], F32)
        cfarB = sb("cfarB", [128, 16], F32)
        ones128 = sb("ones128", [128, 128], BF16)
        fsb = sb("fsb", [16, 384], F32)
        selq = sb("selq", [128, 512], BF16)
        b_selq = Buf()
        b_oc = Buf()
        b_kvd = [Buf() for _ in range(NST)]
        b_fpad = Buf()
        has_odd = any(l % 2 == 1 for l in layers) and "mix" in parts
        if has_odd:
            k.op("pool", lambda e: e.memset(halfc[:], 0.5), writes=[b_oc])
            k.op("pool", lambda e: e.memset(ones128[:], 1.0), writes=[b_oc])
            k.op("pool", lambda e: e.memset(negtri[:], 0.0), writes=[b_oc])
            k.op("pool", lambda e: e.affine_select(
                out=negtri[:], in_=negtri[:], pattern=[[-1, 128]], compare_op=ALU.is_ge,
                fill=-1.0e30, base=0, channel_multiplier=1), reads=[b_oc], writes=[b_oc])
            k.dma("sp", rb[:], rel_bias, writes=[b_oc], chan="oc")
            k.dma("sp", OHs[:], oh_pad, writes=[b_oc], chan="oc")
            k.dma("sp", cfarB[:], rel_bias[31:32, :].partition_broadcast(128), writes=[b_oc], chan="oc")
            bi = next_bank()
            k.op("pe", lambda e, bi=bi: e.matmul(ps[bi][0:16, 0:384], lhsT=rb[:, :], rhs=OHs[:, :],
                                                 start=True, stop=True),
                 reads=[b_oc], writes=[psb[bi]])
            k.op("act", lambda e, bi=bi: e.copy(out=fsb[:], in_=ps[bi][0:16, 0:384]),
                 reads=[psb[bi]], writes=[b_oc])
            k.dma("sp", fpad_d, fsb[:], reads=[b_oc], writes=[b_fpad], chan="oc2")

        def odd_consts(i):
            alias_fence(even2_bufs + [b_lc], odd2_bufs)
            k.op("pool", lambda e: e.memset(cf[:], 0.0), writes=[b_ident])
            k.op("pool", lambda e: e.affine_select(
                out=cf[:], in_=cf[:], pattern=[[1, 128]], compare_op=ALU.not_equal,
                fill=1.0, base=-127, channel_multiplier=1), reads=[b_ident], writes=[b_ident])
            for hd in range(16):
                r_ = hd % 2
                src = bass.AP(tensor=fpad_d.tensor, offset=fpad_d[hd:hd + 1, :].offset,
                              ap=[[1, 128], [1, 256]])
                k.dma("sp", rtmp[r_][:, 0:256], src, reads=[b_fpad], writes=[b_rtmp[r_]],
                      chan=f"rt{r_}")
                bi = next_bank()
                k.op("pe", lambda e, r_=r_, bi=bi: e.matmul(
                    ps[bi][:, 0:256], lhsT=cf[:, :], rhs=rtmp[r_][:, 0:256], start=True, stop=True),
                    reads=[b_rtmp[r_], b_ident], writes=[psb[bi]])
                k.op("dve", lambda e, hd=hd, bi=bi: e.tensor_scalar(
                    out=BTn[:, hd, :], in0=ps[bi][:, 0:256], scalar1=cfarB[:, hd:hd + 1],
                    scalar2=None, op0=ALU.subtract),
                    reads=[psb[bi], b_oc], writes=[b_BTn])

        def odd_mixer(i, st):
            w_in = c_w_in[i]
            w_out = c_w_out[i]
            t0 = st * TS
            alias_fence(b_actT, odd_bufs)
            for c in range(8):
                def ev_q(fb, bi, c=c):
                    hd = c * 2 + fb
                    k.op("act", lambda e, hd=hd, bi=bi: e.activation(
                        out=qT[:, hd, :], in_=ps[bi][:, :], func=AF.Copy, scale=128.0 ** -0.5),
                        reads=[psb[bi]], writes=[b_qT[hd]])
                proj_fm(w_in, c * 256, 256, ev_q)
            for c in range(2):
                def ev_k(fb, bi, c=c):
                    g = c * 2 + fb
                    k.op("act", lambda e, g=g, bi=bi: e.copy(out=kTn[:, g, :], in_=ps[bi][:, :]),
                         reads=[psb[bi]], writes=[b_kTn[g]])
                    k.dma("sp", kT_d[g, :, t0:t0 + TS], kTn[:, g, :], reads=[b_kTn[g]],
                          writes=[b_kvd[st]], chan=f"kvw{g}")
                proj_fm(w_in, 2048 + c * 256, 256, ev_k)
            for c in range(4):
                def ev_iq(fb, bi, c=c):
                    blk = c * 2 + fb
                    k.op("act", lambda e, blk=blk, bi=bi: e.copy(out=iqT[:, blk, :], in_=ps[bi][:, :]),
                         reads=[psb[bi]], writes=[b_iqT[blk]])
                proj_fm(w_in, 3072 + c * 256, 256, ev_iq)
            s_ = w1_rr[0]
            w1_rr[0] = (s_ + 1) % NW1
            def ik_loads(s_=s_):
                for dup in range(2):
                    k.dma("pool", w1s[s_][:, :, dup * 64:(dup + 1) * 64],
                          w_in[:, 4096:4160].rearrange("(b p) c -> p b c", p=128),
                          writes=[b_w1s[s_]], chan=f"w1_{s_}")
            cached_load(w1s[s_], b_w1s[s_], s_, ("ik",), "w1", lambda v: v[:, :, 0:128], ik_loads)
            bi = next_bank()
            for db in range(NDB):
                k.op("pe", lambda e, s_=s_, db=db, bi=bi: e.matmul(
                    ps[bi][:, :], lhsT=w1s[s_][:, db, 0:128], rhs=hnT[:, db, :],
                    start=(db == 0), stop=(db == NDB - 1)),
                    reads=[b_w1s[s_]] + b_hnT, writes=[psb[bi]])
            k.op("act", lambda e, bi=bi: e.copy(out=ikT[:, t0:t0 + TS], in_=ps[bi][:, :]),
                 reads=[psb[bi]], writes=[b_ikT[st]])
            for c in range(2):
                def ev_v(j, bi, c=c):
                    k.op("act", lambda e, j=j, bi=bi, c=c: e.copy(
                        out=vn[:, j, c * 256:(c + 1) * 256], in_=ps[bi][:, 0:256]),
                        reads=[psb[bi]], writes=[b_vn[j]])
                    if c == 1:
                        k.dma("sp", v_d[t0 + j * 128:t0 + (j + 1) * 128, :], vn[:, j, :],
                              reads=[b_vn[j]], writes=[b_kvd[st]], chan=f"kvw{j}")
                proj_tm(w_in, 2560 + c * 256, ev_v)
            s_ = wchunk(w_in, 4160, 16)
            for j in range(4):
                bi = next_bank()
                for db in range(NDB):
                    k.op("pe", lambda e, s_=s_, j=j, db=db, bi=bi: e.matmul(
                        ps[bi][:, 0:16], lhsT=hnT[:, db, j * 128:(j + 1) * 128],
                        rhs=w1s[s_][:, db, 0:16], start=(db == 0), stop=(db == NDB - 1)),
                        reads=[b_w1s[s_], b_hnT[j]], writes=[psb[bi]])
                k.op("act", lambda e, j=j, bi=bi: e.copy(out=iw_t[:, j, :], in_=ps[bi][:, 0:16]),
                     reads=[psb[bi]], writes=[b_iw[j]])
            for j in range(4):
                qb = st * 4 + j
                nk = (qb + 1) * 128
                nch = (nk + 511) // 512
                for h in range(16):
                    blk, po = h // 2, (h % 2) * 64
                    for c in range(nch):
                        ncol = min(512, nk - c * 512)
                        bi = next_bank()
                        k.op("pe", lambda e, blk=blk, po=po, j=j, c=c, ncol=ncol, bi=bi: e.matmul(
                            ps[bi][:, 0:ncol], lhsT=iqT[po:po + 64, blk, j * 128:(j + 1) * 128],
                            rhs=ikT[po:po + 64, c * 512:c * 512 + ncol], start=True, stop=True),
                            reads=[b_iqT[blk], b_ikT[c]], writes=[psb[bi]])
                        r_ = (h * nch + c) % 2
                        k.op("act", lambda e, r_=r_, ncol=ncol, bi=bi: e.activation(
                            out=rtmp[r_][:, 0:ncol], in_=ps[bi][:, 0:ncol], func=AF.Relu),
                            reads=[psb[bi]], writes=[b_rtmp[r_]])
                        cs = slice(c * 512, c * 512 + ncol)
                        if h == 0:
                            k.op("dve", lambda e, r_=r_, ncol=ncol, cs=cs, j=j: e.tensor_scalar(
                                out=Iacc[:, cs], in0=rtmp[r_][:, 0:ncol], scalar1=iw_t[:, j, 0:1],
                                scalar2=None, op0=ALU.mult),
                                reads=[b_rtmp[r_], b_iw[j]], writes=[b_I])
                        else:
                            k.op("dve", lambda e, r_=r_, ncol=ncol, cs=cs, j=j, h=h: e.scalar_tensor_tensor(
                                out=Iacc[:, cs], in0=rtmp[r_][:, 0:ncol], scalar=iw_t[:, j, h:h + 1],
                                in1=Iacc[:, cs], op0=ALU.mult, op1=ALU.add),
                                reads=[b_rtmp[r_], b_iw[j], b_I], writes=[b_I])
                k.op("dve", lambda e, nk=nk: e.tensor_reduce(out=thr[:, 1:2], in_=Iacc[:, 0:nk],
                                                            axis=AX.X, op=ALU.max),
                     reads=[b_I], writes=[b_thr])
                k.op("dve", lambda e, nk=nk: e.tensor_reduce(out=thr[:, 0:1], in_=Iacc[:, 0:nk],
                                                            axis=AX.X, op=ALU.min),
                     reads=[b_I], writes=[b_thr])
                k.op("dve", lambda e, qb=qb: e.tensor_tensor(
                    out=Iacc[:, qb * 128:(qb + 1) * 128], in0=Iacc[:, qb * 128:(qb + 1) * 128],
                    in1=negtri[:, :], op=ALU.add), reads=[b_I, b_oc], writes=[b_I])
                if nk > 256:
                    k.op("dve", lambda e: e.tensor_tensor(
                        out=thr[:, 6:7], in0=thr[:, 1:2], in1=thr[:, 0:1], op=ALU.subtract),
                        reads=[b_thr], writes=[b_thr])
                    for it in range(20):
                        cit = 0.5 ** (it + 1)
                        k.op("dve", lambda e, cit=cit: e.scalar_tensor_tensor(
                            out=thr[:, 2:3], in0=thr[:, 6:7], scalar=cit, in1=thr[:, 0:1],
                            op0=ALU.mult, op1=ALU.add), reads=[b_thr], writes=[b_thr])
                        n0 = min(nk, 2048)
                        k.op("dve", lambda e, n0=n0: e.tensor_scalar(
                            out=hn[:, 0:n0], in0=Iacc[:, 0:n0], scalar1=thr[:, 2:3], scalar2=None,
                            op0=ALU.is_ge, op1=ALU.add, accum_out=thr[:, 3:4]),
                            reads=[b_I, b_thr], writes=[b_hn, b_thr])
                        if nk > 2048:
                            k.op("dve", lambda e, nk=nk: e.tensor_scalar(
                                out=hn[:, 0:nk - 2048], in0=Iacc[:, 2048:nk], scalar1=thr[:, 2:3],
                                scalar2=None, op0=ALU.is_ge, op1=ALU.add, accum_out=thr[:, 4:5]),
                                reads=[b_I, b_thr], writes=[b_hn, b_thr])
                            k.op("dve", lambda e: e.tensor_tensor(
                                out=thr[:, 3:4], in0=thr[:, 3:4], in1=thr[:, 4:5], op=ALU.add),
                                reads=[b_thr], writes=[b_thr])
                        k.op("dve", lambda e, cit=cit: e.tensor_scalar(
                            out=thr[:, 5:6], in0=thr[:, 3:4], scalar1=255.5, scalar2=cit,
                            op0=ALU.is_ge, op1=ALU.mult), reads=[b_thr], writes=[b_thr])
                        k.op("dve", lambda e: e.scalar_tensor_tensor(
                            out=thr[:, 0:1], in0=thr[:, 5:6], scalar=thr[:, 6:7], in1=thr[:, 0:1],
                            op0=ALU.mult, op1=ALU.add), reads=[b_thr], writes=[b_thr])
                for c in range(nch):
                    ncol = min(512, nk - c * 512)
                    nb = ncol // 128
                    k.op("dve", lambda e, c=c, ncol=ncol: e.tensor_scalar(
                        out=selq[:, 0:ncol], in0=Iacc[:, c * 512:c * 512 + ncol], scalar1=thr[:, 0:1],
                        scalar2=None, op0=ALU.is_ge), reads=[b_I, b_thr], writes=[b_selq])
                    bt = next_bank()
                    pt = ps[bt].bitcast(BF16)
                    for q in range(nb):
                        k.op("pe", lambda e, q=q, pt=pt: e.transpose(
                            out=pt[:, q * 128:(q + 1) * 128], in_=selq[:, q * 128:(q + 1) * 128],
                            identity=ident[:]), reads=[b_selq, b_ident], writes=[psb[bt]])
                    k.op("act", lambda e, c=c, nb=nb, pt=pt, j=j: e.copy(
                        out=selT[:, c * 4:c * 4 + nb, j * 128:(j + 1) * 128],
                        in_=pt[:, 0:nb * 128].rearrange("p (q t) -> p q t", q=nb)),
                        reads=[psb[bt]], writes=[b_selT[j]])
            steps = []
            for hd in range(16):
                for c in range(st + 1):
                    for kq in range(4):
                        steps.append((hd, c, kq))
            nkb = st * 4 + 4
            nst_ = len(steps)
            LA = 2
            slot_of = {}
            bank_of = {}
            obank = {}

            def emit_load(hd, c):
                g = hd // 4
                if (g, c, hd) in slot_of:
                    return
                s_ = w2_rr[0]
                w2_rr[0] = (s_ + 1) % NW2
                kslot = w2s[s_][:, 0:1, :].rearrange("p a c -> p (a c)")
                vslot = w2s[s_][:, 1:2, :].rearrange("p a (q v) -> p (a q) v", q=4)
                k.dma("sp", kslot, kT_d[g, :, c * 512:(c + 1) * 512], reads=[b_kvd[c]],
                      writes=[b_w2s[s_]], chan=f"kv{s_}")
                k.dma("sp", vslot, v_d[c * 512:(c + 1) * 512, g * 128:(g + 1) * 128].rearrange(
                    "(q p) v -> p q v", p=128), reads=[b_kvd[c]], writes=[b_w2s[s_]], chan=f"kv{s_}")
                slot_of[(g, c, hd)] = (s_, kslot, vslot)

            def geom(c, kq):
                kb = c * 4 + kq
                col0 = kq * 128 if c == st else 0
                return kb, col0, 512 - col0

            def emit_qk(i):
                hd, c, kq = steps[i]
                g = hd // 4
                emit_load(hd, c)
                if kq == 0:
                    if c < st:
                        emit_load(hd, c + 1)
                    elif hd < 15:
                        emit_load(hd + 1, 0)
                s_, kslot, vslot = slot_of[(g, c, hd)]
                kb, col0, ncols = geom(c, kq)
                near = kb >= 4 * st - 1
                bi = next_bank()
                bank_of[i] = bi
                k.op("pe", lambda e: e.matmul(
                    ps[bi][:, 0:ncols], lhsT=kslot[:, kq * 128:(kq + 1) * 128],
                    rhs=qT[:, hd, col0:512], start=True, stop=(not near)),
                    reads=[b_w2s[s_], b_qT[hd]], writes=[psb[bi]])
                if near:
                    if c == st:
                        off, nbc = 0, min(256, ncols)
                    else:
                        off, nbc = 128, 128
                    k.op("pe", lambda e: e.matmul(
                        ps[bi][:, 0:nbc], lhsT=ident[:, :], rhs=BTn[:, hd, off:off + nbc],
                        start=False, stop=True),
                        reads=[b_BTn, b_ident], writes=[psb[bi]])

            def emit_softmax(i):
                hd, c, kq = steps[i]
                kb, col0, ncols = geom(c, kq)
                bi = bank_of[i]
                pr = i % 2
                k.op("act", lambda e: e.activation(
                    out=PTb[pr][:, 0:ncols], in_=ps[bi][:, 0:ncols], func=AF.Exp,
                    bias=cfarB[:, hd:hd + 1], scale=1.0),
                    reads=[psb[bi], b_oc], writes=[b_PT[pr]])
                k.op("dve", lambda e: e.tensor_tensor(
                    out=PMb[pr][:, 0:ncols], in0=PTb[pr][:, 0:ncols], in1=selT[:, kb, col0:512],
                    op=ALU.mult), reads=[b_PT[pr]] + b_selT, writes=[b_PM[pr]])

            def emit_pv(i):
                hd, c, kq = steps[i]
                g = hd // 4
                kb, col0, ncols = geom(c, kq)
                s_, kslot, vslot = slot_of[(g, c, hd)]
                pr = i % 2
                if kb == 0:
                    obank[hd] = (reserve_bank(), reserve_bank())
                bo, bl = obank[hd]
                k.op("pe", lambda e: e.matmul(
                    ps[bo][:, col0:512], lhsT=vslot[:, kq, :], rhs=PMb[pr][:, 0:ncols],
                    start=(kb == 0), stop=(kb == nkb - 1)),
                    reads=[b_w2s[s_], b_PM[pr]], writes=[psb[bo]])
                k.op("pe", lambda e: e.matmul(
                    ps[bl][:, col0:512], lhsT=ones128[:, :], rhs=PMb[pr][:, 0:ncols],
                    start=(kb == 0), stop=(kb == nkb - 1)),
                    reads=[b_oc, b_PM[pr]], writes=[psb[bl]])
                if kb == nkb - 1:
                    r_ = hd % 2
                    k.op("dve", lambda e: e.reciprocal(out=rtmp[r_][:, :], in_=ps[bl][:, :]),
                         reads=[psb[bl]], writes=[b_rtmp[r_]])
                    k.op("dve", lambda e: e.tensor_tensor(
                        out=qT[:, hd, :], in0=ps[bo][:, :], in1=rtmp[r_][:, :], op=ALU.mult),
                        reads=[psb[bo], b_rtmp[r_]], writes=[b_qT[hd]])
                    reserved.discard(bo)
                    reserved.discard(bl)

            for i in range(min(LA, nst_)):
                emit_qk(i)
            for i in range(nst_):
                emit_softmax(i)
                if i + LA < nst_:
                    emit_qk(i + LA)
                emit_pv(i)
            for cq in range(8):
                s_ = wchunk(w_out, cq * 256, 256, key=("out", cq))
                for j in range(4):
                    bi = next_bank()
                    for cb in range(16):
                        k.op("pe", lambda e, s_=s_, j=j, cb=cb, bi=bi: e.matmul(
                            ps[bi][:, 0:256], lhsT=qT[:, cb, j * 128:(j + 1) * 128],
                            rhs=w1s[s_][:, cb, 0:256], start=(cb == 0), stop=(cb == 15)),
                            reads=[b_w1s[s_], b_qT[cb]], writes=[psb[bi]])
                    k.op("dve", lambda e, j=j, bi=bi, cq=cq: e.tensor_tensor(
                        out=hs[:, j, cq * 256:(cq + 1) * 256], in0=ps[bi][:, 0:256],
                        in1=hs[:, j, cq * 256:(cq + 1) * 256], op=ALU.add),
                        reads=[psb[bi], b_hs[j]], writes=[b_hs[j]])
            alias_fence(odd_bufs, b_actT)

        first = True
        for li, l in enumerate(layers):
            last = (li == len(layers) - 1)
            if "mix" in parts and l % 2 == 0:
                alias_fence(odd2_bufs, even2_bufs + [b_lc])
                even_consts(l // 2)
            if "mix" in parts and l % 2 == 1:
                odd_consts(l // 2)
            for st in range(NST):
                ctx["l"], ctx["st"] = l, st
                src_h = x if first else hscr
                for j in range(TS // 128):
                    k.dma("sp", hs[:, j, :], src_h[st * TS + j * 128: st * TS + (j + 1) * 128, :],
                          reads=[b_hdram[st]] if not first else [], writes=[b_hs[j]], chan=f"hs{j}")
                if "mix" in parts:
                    load_gain(norm_mix_g[l:l + 1, :])
                    for j in range(TS // 128):
                        norm_tile(j)
                        norm_apply_T(j)
                    if l % 2 == 0:
                        even_mixer(l // 2)
                    else:
                        odd_mixer(l // 2, st)
                if "ffn" in parts:
                    load_gain(norm_ffn_g[l:l + 1, :])
                    for j in range(TS // 128):
                        norm_tile(j)
                        norm_apply_T(j)
                    ffn_supertile(l)
                if last and do_final:
                    load_gain(final_norm_g[0:1, :])
                    for j in range(TS // 128):
                        norm_tile(j)
                        k.op("dve", lambda e, j=j: e.scalar_tensor_tensor(
                            out=hs[:, j, :], in0=hs[:, j, :], scalar=stat[:, 2:3], in1=gB[:],
                            op0=ALU.mult, op1=ALU.mult),
                            reads=[b_hs[j], b_stat, b_gB], writes=[b_hs[j]])
                dst_h = out if last else hscr
                for j in range(TS // 128):
                    k.dma("sp", dst_h[st * TS + j * 128: st * TS + (j + 1) * 128, :], hs[:, j, :],
                          reads=[b_hs[j]], writes=[b_hdram[st]], chan=f"ho{j}")
            first = False
        k.wait_all("sp", b_hdram)

        with nc.Block() as block:
            @block.tensor
            def _(e):
                k.replay("pe", e)

            @block.scalar
            def _(e):
                k.replay("act", e)

            @block.vector
            def _(e):
                k.replay("dve", e)

            @block.gpsimd
            def _(e):
                k.replay("pool", e)

            @block.sync
            def _(e):
                k.replay("sp", e)
    return nc


def _bucket_table():
    d = np.arange(0, 257)
    dd = np.maximum(d, 1).astype(np.float32)
    large = 16 + (np.log(dd / np.float32(16)) / np.float32(math.log(128 / 16))
                  * np.float32(16)).astype(np.int32)
    large = np.minimum(large, 31)
    return np.where(d < 16, d, large)


def _oh_pad():
    bk = _bucket_table()
    oh = np.zeros((32, 384), np.float32)
    for m in range(127, 384):
        oh[bk[m - 127], m] = 1.0
    return oh


def make_in_map(inp, x):
    f = lambda a: np.ascontiguousarray(np.asarray(a, dtype=np.float32))
    return dict(
        x=f(x), norm_mix_g=f(inp["norm_mix_g"]), norm_ffn_g=f(inp["norm_ffn_g"]),
        final_norm_g=f(inp["final_norm_g"]).reshape(1, -1),
        ffn_w1=f(inp["ffn_w1"]), ffn_w2=f(inp["ffn_w2"]),
        ab_w_in=f(inp["ab_w_in"]), a_v_ln_g=f(inp["a_v_ln_g"]).reshape(2, 1, 1024),
        a_w_s=f(inp["a_w_s"]), a_b_s=f(inp["a_b_s"]).reshape(2, 1, 1024),
        b_gate_w2=f(inp["b_gate_w2"]), b_gate_b=f(inp["b_gate_b"]).reshape(2, 1, 512),
        b_out_norm_g=f(inp["b_out_norm_g"]).reshape(2, 1, 256), ab_w_out=f(inp["ab_w_out"]),
        c_w_in=f(inp["c_w_in"]), c_w_out=f(inp["c_w_out"]), rel_bias=f(inp["rel_bias"]),
        oh_pad=_oh_pad())


ACTIVE = (0, 1, 4, 5)


def kernel(**inputs):
    x = np.asarray(inputs["x"], dtype=np.float32)
    nc = build(dict(T=SEQ, layers=[0, 1, 2, 3]))
    real = [make_in_map(inputs, x[b]) for b in range(BATCH)]
    zero = {kk: (v if kk == "oh_pad" else np.zeros_like(v)) for kk, v in real[0].items()}
    in_maps = [zero] * 8
    in_maps = list(in_maps)
    for b, c in enumerate(ACTIVE):
        in_maps[c] = real[b]
    res = run_bass_kernel_spmd(nc, in_maps, core_ids=list(range(8)))
    out = np.stack([np.asarray(res.results[c]["out"]) for c in ACTIVE])
    return out.astype(np.float32)
```

```python
import math
from contextlib import ExitStack

import numpy as np
import concourse.bass as bass
import concourse.mybir as mybir
from concourse.bass_utils import run_bass_kernel_spmd

F32 = mybir.dt.float32
BF16 = mybir.dt.bfloat16
AF = mybir.ActivationFunctionType
ALU = mybir.AluOpType
AX = mybir.AxisListType

D = 2048
NDB = D // 128
DFF = 8192
NFB = DFF // 128
SEQ = 4096
BATCH = 4
DEPTH = 4
TS = 512
EPS = 1e-6
AB_IN = 5136
C_IN = 4176


class Buf:
    __slots__ = ("w", "r")

    def __init__(self):
        self.w = {}
        self.r = {}


def _merge(dst, src):
    for s, v in src.items():
        if dst.get(s, 0) < v:
            dst[s] = v


class KB:
    ENG = ("pe", "act", "dve", "pool", "sp")

    def __init__(self, nc, es):
        self.nc = nc
        self.es = es
        self.q = {e: [] for e in self.ENG}
        self.semh = {}
        self.semc = {}
        for e in ("pe", "act", "dve", "pool"):
            self.newsem(e)

    def newsem(self, name):
        if name not in self.semh:
            self.semh[name] = self.es.enter_context(self.nc.semaphore("s_" + name))
            self.semc[name] = 0
        return name

    def op(self, eng, fn, reads=(), writes=(), sem=None, inc=1):
        deps = {}
        for b in reads:
            _merge(deps, b.w)
        for b in writes:
            _merge(deps, b.w)
            _merge(deps, b.r)
        if sem is None:
            sem = eng
        if eng == "pe":
            deps.pop("pe", None)
        self.semc[sem] += inc
        val = self.semc[sem]
        self.q[eng].append((fn, deps, sem, inc))
        for b in reads:
            if b.r.get(sem, 0) < val:
                b.r[sem] = val
        for b in writes:
            b.w = {sem: val}
            b.r = {}
        return (sem, val)

    def dma(self, eng, out, in_, reads=(), writes=(), chan="d0"):
        self.newsem(chan)
        return self.op(eng, lambda e: e.dma_start(out=out, in_=in_), reads, writes,
                       sem=chan, inc=16)

    def wait_all(self, eng, bufs):
        deps = {}
        for b in bufs:
            _merge(deps, b.w)
            _merge(deps, b.r)
        self.q[eng].append((None, deps, None, 0))

    def replay(self, eng, e):
        waited = {}
        for fn, deps, sem, inc in self.q[eng]:
            for s, v in deps.items():
                if waited.get(s, 0) < v:
                    e.wait_ge(self.semh[s], v)
                    waited[s] = v
            if fn is not None:
                ins = fn(e)
                ins.then_inc(self.semh[sem], inc)


def build(cfg):
    T = cfg["T"]
    layers = cfg["layers"]
    parts = cfg.get("parts", ("mix", "ffn"))
    do_final = cfg.get("final", True)
    NST = T // TS
    nc = bass.Bass("TRN2", target_bir_lowering=False)

    def din(name, shape):
        return nc.dram_tensor(name, list(shape), F32, kind="ExternalInput").ap()

    x = din("x", [T, D])
    norm_mix_g = din("norm_mix_g", [DEPTH, D])
    norm_ffn_g = din("norm_ffn_g", [DEPTH, D])
    final_norm_g = din("final_norm_g", [1, D])
    ffn_w1 = din("ffn_w1", [DEPTH, D, DFF])
    ffn_w2 = din("ffn_w2", [DEPTH, DFF, D])
    ab_w_in = din("ab_w_in", [2, D, AB_IN])
    a_v_ln_g = din("a_v_ln_g", [2, 1, 1024])
    a_w_s = din("a_w_s", [2, 8, 128, 128])
    a_b_s = din("a_b_s", [2, 1, 1024])
    b_gate_w2 = din("b_gate_w2", [2, 16, 512])
    b_gate_b = din("b_gate_b", [2, 1, 512])
    b_out_norm_g = din("b_out_norm_g", [2, 1, 256])
    ab_w_out = din("ab_w_out", [2, D, D])
    c_w_in = din("c_w_in", [2, D, C_IN])
    c_w_out = din("c_w_out", [2, D, D])
    rel_bias = din("rel_bias", [32, 16])
    oh_pad = din("oh_pad", [32, 384])
    kT_d = nc.dram_tensor("kT_d", [4, 128, T], BF16, kind="Internal").ap()
    v_d = nc.dram_tensor("v_d", [T, 512], BF16, kind="Internal").ap()
    fpad_d = nc.dram_tensor("fpad_d", [16, 384], F32, kind="Internal").ap()
    out = nc.dram_tensor("out", [T, D], F32, kind="ExternalOutput").ap()
    hscr = nc.dram_tensor("hscr", [T, D], F32, kind="Internal").ap()

    NCHK = 136
    wscr = [nc.dram_tensor(f"wscr{i}", [NCHK, 128, 4096], BF16, kind="Internal").ap()
            for i in range(DEPTH)]
    wreg = {}
    ctx = {"l": 0, "st": 0}

    es = ExitStack()
    with es:
        k = KB(nc, es)

        def cached_load(slot_t, slot_buf, slot_id, key, kind, sub, cast_loads):
            l, st = ctx["l"], ctx["st"]
            if (l, key) not in wreg:
                wreg[(l, key)] = (sum(1 for (ll, _) in wreg if ll == l), Buf())
            idx, wb = wreg[(l, key)]
            assert idx < NCHK
            if kind == "w1":
                scr = wscr[l][idx].rearrange("p (b c) -> p b c", b=16)
            else:
                scr = wscr[l][idx][:, 0:2048].rearrange("p (b c) -> p b c", b=4)
            if st == 0:
                cast_loads()
                k.dma("sp", sub(scr), sub(slot_t), reads=[slot_buf], writes=[wb],
                      chan=f"wo_{kind}{slot_id}")
            else:
                k.dma("sp", sub(slot_t), sub(scr), reads=[wb], writes=[slot_buf],
                      chan=f"wh_{kind}{slot_id}")

        def sb(name, shape, dt):
            return es.enter_context(nc.sbuf_tensor(name, list(shape), dt))

        ps = [es.enter_context(nc.psum_tensor(f"ps{i}", [128, 512], F32)) for i in range(8)]
        psb = [Buf() for _ in range(8)]
        ps_rr = [0]

        reserved = set()

        def next_bank():
            while True:
                i = ps_rr[0]
                ps_rr[0] = (i + 1) % 8
                if i not in reserved:
                    return i

        def reserve_bank():
            i = next_bank()
            reserved.add(i)
            return i

        ident = sb("ident", [128, 128], BF16)
        identf = sb("identf", [128, 128], F32)
        b_ident = Buf()
        hs = sb("hs", [128, TS // 128, D], F32)
        b_hs = [Buf() for _ in range(TS // 128)]
        gB = sb("gB", [128, D], F32)
        b_gB = Buf()
        hn = sb("hn", [128, D], BF16)
        b_hn = Buf()
        junk = sb("junk", [128, 256], BF16)
        b_junk = Buf()
        stat = sb("stat", [128, 8], F32)
        b_stat = Buf()
        hnT = sb("hnT", [128, NDB, TS], BF16)
        b_hnT = [Buf() for _ in range(TS // 128)]
        actT = sb("actT", [128, NFB, TS], BF16)
        b_actT = [Buf() for _ in range(NFB)]
        arena = actT[:, :, :].rearrange("p a b -> p (a b)")

        def carve(off, n, pat=None, **kw):
            v = arena[:, off:off + n]
            return v.rearrange(pat, **kw) if pat else v
        a_uT = carve(0, 4096, "p (g t) -> p g t", g=8)
        v_sgu = carve(4096, 4096, "p (j c) -> p j c", j=4)
        v_gla = carve(8192, 4096, "p (j c) -> p j c", j=4)
        r_s = carve(12288, 4096, "p (j c) -> p j c", j=4)
        catT = carve(16384, 8192, "p (b t) -> p b t", b=16)
        qe = carve(24576, 2048, "p (h t) -> p h t", h=4)
        ke = carve(26624, 2048, "p (h t) -> p h t", h=4)
        ks = carve(28672, 2048, "p (h t) -> p h t", h=4)
        ksT = carve(30720, 2048, "p (h j d) -> p h j d", h=4, j=4)
        b_auT = [Buf() for _ in range(8)]
        b_vsgu = [Buf() for _ in range(4)]
        b_vgla = [Buf() for _ in range(4)]
        b_rs = [Buf() for _ in range(4)]
        b_catT = [Buf() for _ in range(16)]
        b_qe = [Buf() for _ in range(4)]
        b_ke = [Buf() for _ in range(4)]
        b_ks = [Buf() for _ in range(4)]
        b_ksT = [Buf() for _ in range(4)]
        mix_bufs = b_auT + b_vsgu + b_vgla + b_rs + b_catT + b_qe + b_ke + b_ks + b_ksT
        arena2 = sb("arena2", [128, 18432], BF16)

        def carve2(off_b, nbytes, dt, pat=None, **kw):
            v = arena2[:, off_b // 2:(off_b + nbytes) // 2]
            if dt == F32:
                v = v.bitcast(F32)
            return v.rearrange(pat, **kw) if pat else v
        E1 = carve2(0, 4096, BF16, "p (h t) -> p h t", h=4)
        E2 = carve2(4096, 4096, BF16, "p (h t) -> p h t", h=4)
        b_E = [Buf() for _ in range(4)]
        explast = sb("explast", [128, 4, 4], F32)
        spt = [carve2(8192 + i * 2048, 2048, F32) for i in range(2)]
        b_spt = [Buf() for _ in range(2)]
        tmpf = [carve2(12288 + i * 2048, 2048, F32) for i in range(2)]
        b_tmpf = [Buf() for _ in range(2)]
        tmpf_rr = [0]
        scTm = carve2(16384, 1024, BF16, "p (h t) -> p h t", h=4)
        b_scTm = Buf()
        Sst = carve2(17408, 4096, F32, "p (h v) -> p h v", h=4)
        Sbf = carve2(21504, 2048, BF16, "p (h v) -> p h v", h=4)
        b_S = [Buf() for _ in range(4)]
        b_Sbf = [Buf() for _ in range(4)]
        lnB = carve2(23552, 4096, F32)
        ongB = carve2(27648, 1024, F32)
        WT = carve2(28672, 2048, BF16, "p (g t) -> p g t", g=8)
        bout = carve2(30720, 2048, BF16)
        b_bout = Buf()
        bs_row = sb("bs_row", [1, 1024], BF16)
        W2ext = sb("W2ext", [32, 512], BF16)
        even2_bufs = b_E + b_spt + b_tmpf + [b_scTm] + b_S + b_Sbf + [b_bout]
        Iacc = carve2(0, 16384, F32)
        b_I = Buf()
        ikT = carve2(16384, 8192, BF16)
        b_ikT = [Buf() for _ in range(8)]
        BTn = carve2(24576, 8192, BF16, "p (h t) -> p h t", h=16)
        b_BTn = Buf()
        PTb = [carve2(32768 + i * 1024, 1024, BF16) for i in range(2)]
        PMb = [carve2(34816 + i * 1024, 1024, BF16) for i in range(2)]
        b_PT = [Buf() for _ in range(2)]
        b_PM = [Buf() for _ in range(2)]
        odd2_bufs = [b_I] + b_ikT + [b_BTn] + b_PT + b_PM
        b_lc = Buf()
        glr_ext = sb("glr_ext", [32, TS], BF16)
        b_glr = Buf()
        ones_row = sb("ones_row", [1, 128], BF16)
        onec = sb("onec", [128, 1], F32)
        triNeg = sb("triNeg", [128, 128], F32)
        maskT = sb("maskT", [128, 4, 128], BF16)
        cf = sb("cf", [128, 128], F32)
        st2 = sb("st2", [128, 16], F32)
        b_st2 = Buf()
        st3 = sb("st3", [128, 64], F32)
        b_st3 = Buf()

        def alias_fence(src, dst):
            tok = {}
            for b in src:
                _merge(tok, b.w)
                _merge(tok, b.r)
            for b in dst:
                _merge(b.r, tok)
        rtmp = [sb(f"rtmp{i}", [128, TS], F32) for i in range(2)]
        b_rtmp = [Buf() for _ in range(2)]
        NW1 = 2
        W1C = 256
        w1s = [sb(f"w1s{i}", [128, NDB, W1C], BF16) for i in range(NW1)]
        b_w1s = [Buf() for _ in range(NW1)]
        NW2 = 3
        W2R = 4
        w2s = [sb(f"w2s{i}", [128, W2R, 512], BF16) for i in range(NW2)]
        b_w2s = [Buf() for _ in range(NW2)]
        w1_rr = [0]
        w2_rr = [0]
        b_hdram = [Buf() for _ in range(NST)]

        epsc = sb("epsc", [128, 1], F32)
        k.op("pool", lambda e: e.memset(epsc[:], EPS), writes=[b_ident])
        k.op("pool", lambda e: e.memset(identf[:], 0.0), writes=[b_ident])
        k.op("pool", lambda e: e.affine_select(
            out=identf[:], in_=identf[:], pattern=[[-1, 128]], compare_op=ALU.not_equal,
            fill=1.0, base=0, channel_multiplier=1), reads=[b_ident], writes=[b_ident])
        k.op("dve", lambda e: e.tensor_copy(out=ident[:], in_=identf[:]),
             reads=[b_ident], writes=[b_ident])

        k.op("pool", lambda e: e.memset(onec[:], 1.0), writes=[b_ident])
        k.op("pool", lambda e: e.memset(ones_row[:], 1.0), writes=[b_ident])
        k.op("pool", lambda e: e.memset(glr_ext[:], 1.0), writes=[b_glr])
        k.op("pool", lambda e: e.memset(triNeg[:], -1.0 / 16.0), writes=[b_ident])
        k.op("pool", lambda e: e.affine_select(
            out=triNeg[:], in_=triNeg[:], pattern=[[1, 128]], compare_op=ALU.is_ge,
            fill=0.0, base=0, channel_multiplier=-1), reads=[b_ident], writes=[b_ident])
        k.op("pool", lambda e: e.memset(cf[:], 1.0), writes=[b_ident])
        k.op("pool", lambda e: e.affine_select(
            out=cf[:], in_=cf[:], pattern=[[1, 128]], compare_op=ALU.is_ge,
            fill=0.0, base=0, channel_multiplier=-1), reads=[b_ident], writes=[b_ident])
        for hh in range(4):
            k.op("dve", lambda e, hh=hh: e.tensor_copy(out=maskT[:, hh, :], in_=cf[:]),
                 reads=[b_ident], writes=[b_ident])
        def load_gain(g_ap):
            k.dma("sp", gB[:], g_ap.partition_broadcast(128), writes=[b_gB], chan="gB")

        def norm_tile(j, dst_T=True):
            k.op("act", lambda e: e.activation(out=hn[:], in_=hs[:, j, :], func=AF.Square,
                                               accum_out=stat[:, 0:1]),
                 reads=[b_hs[j]], writes=[b_hn, b_stat])
            k.op("act", lambda e: e.activation(out=stat[:, 1:2], in_=stat[:, 0:1], func=AF.Ln,
                                               bias=epsc[:, 0:1], scale=1.0 / D),
                 reads=[b_stat, b_ident], writes=[b_stat])
            k.op("act", lambda e: e.activation(out=stat[:, 2:3], in_=stat[:, 1:2], func=AF.Exp,
                                               scale=-0.5),
                 reads=[b_stat], writes=[b_stat])

        def norm_apply_T(j):
            k.op("dve", lambda e: e.scalar_tensor_tensor(
                out=hn[:], in0=hs[:, j, :], scalar=stat[:, 2:3], in1=gB[:],
                op0=ALU.mult, op1=ALU.mult),
                reads=[b_hs[j], b_stat, b_gB], writes=[b_hn])
            for half in range(2):
                bi = next_bank()
                pt = ps[bi].bitcast(BF16)
                for q in range(8):
                    db = half * 8 + q
                    k.op("pe", lambda e, db=db, q=q, pt=pt: e.transpose(
                        out=pt[:, q * 128:(q + 1) * 128], in_=hn[:, db * 128:(db + 1) * 128],
                        identity=ident[:]),
                        reads=[b_hn, b_ident], writes=[psb[bi]])
                eng = "act" if half == 0 else "dve"
                src = pt[:, :].rearrange("p (q t) -> p q t", q=8)
                dst = hnT[:, half * 8:(half + 1) * 8, j * 128:(j + 1) * 128]
                if eng == "act":
                    k.op("act", lambda e, src=src, dst=dst: e.copy(out=dst, in_=src),
                         reads=[psb[bi]], writes=[b_hnT[j]])
                else:
                    k.op("dve", lambda e, src=src, dst=dst: e.tensor_copy(out=dst, in_=src),
                         reads=[psb[bi]], writes=[b_hnT[j]])

        def ffn_supertile(l):
            NTT = TS // 128
            for c in range(DFF // W1C):
                s = w1_rr[0]
                w1_rr[0] = (s + 1) % NW1
                src = ffn_w1[l, :, c * W1C:(c + 1) * W1C].rearrange("(b p) c -> p b c", p=128)
                cached_load(w1s[s], b_w1s[s], s, ("w1", c), "w1", lambda v: v[:, :, :],
                            lambda s=s, src=src: k.dma("pool", w1s[s][:], src, writes=[b_w1s[s]],
                                                       chan=f"w1_{s}"))
                for fb in range(W1C // 128):
                    ffb = c * (W1C // 128) + fb
                    bi = next_bank()
                    for db in range(NDB):
                        k.op("pe", lambda e, s=s, fb=fb, db=db, bi=bi: e.matmul(
                            ps[bi][:, :], lhsT=w1s[s][:, db, fb * 128:(fb + 1) * 128],
                            rhs=hnT[:, db, :], start=(db == 0), stop=(db == NDB - 1)),
                            reads=[b_w1s[s]] + b_hnT, writes=[psb[bi]])
                    r = ffb % 2
                    k.op("act", lambda e, bi=bi, r=r: e.activation(
                        out=rtmp[r][:], in_=ps[bi][:, :], func=AF.Relu),
                        reads=[psb[bi]], writes=[b_rtmp[r]])
                    k.op("dve", lambda e, r=r, ffb=ffb: e.tensor_tensor(
                        out=actT[:, ffb, :], in0=rtmp[r][:], in1=rtmp[r][:], op=ALU.mult),
                        reads=[b_rtmp[r]], writes=[b_actT[ffb]])
            for dq in range(D // 512):
                banks = [next_bank() for _ in range(NTT)]
                for c in range(NFB // W2R):
                    s = w2_rr[0]
                    w2_rr[0] = (s + 1) % NW2
                    src = ffn_w2[l, c * W2R * 128:(c + 1) * W2R * 128,
                                 dq * 512:(dq + 1) * 512].rearrange("(b p) c -> p b c", p=128)
                    cached_load(w2s[s], b_w2s[s], s, ("w2", dq, c), "w2", lambda v: v[:, :, :],
                                lambda s=s, src=src: k.dma("pool", w2s[s][:], src, writes=[b_w2s[s]],
                                                           chan=f"w2_{s}"))
                    for fr in range(W2R):
                        ffb = c * W2R + fr
                        for j in range(NTT):
                            bi = banks[j]
                            k.op("pe", lambda e, s=s, fr=fr, ffb=ffb, j=j, bi=bi: e.matmul(
                                ps[bi][:, :], lhsT=actT[:, ffb, j * 128:(j + 1) * 128],
                                rhs=w2s[s][:, fr, :], start=(ffb == 0), stop=(ffb == NFB - 1)),
                                reads=[b_w2s[s], b_actT[ffb]], writes=[psb[bi]])
                for j in range(NTT):
                    bi = banks[j]
                    k.op("dve", lambda e, j=j, bi=bi, dq=dq: e.tensor_tensor(
                        out=hs[:, j, dq * 512:(dq + 1) * 512], in0=ps[bi][:, :],
                        in1=hs[:, j, dq * 512:(dq + 1) * 512], op=ALU.add),
                        reads=[psb[bi], b_hs[j]], writes=[b_hs[j]])


        lnsc = sb("lnsc", [128, 1], F32)
        k.op("pool", lambda e: e.memset(lnsc[:], -0.5 * math.log(128.0)), writes=[b_ident])

        def tm_tmp():
            r = tmpf_rr[0]
            tmpf_rr[0] = 1 - r
            return r

        def wchunk(w2d, c0, ncols, key=None):
            s_ = w1_rr[0]
            w1_rr[0] = (s_ + 1) % NW1
            if key is None:
                key = ("in", c0)
            cached_load(w1s[s_], b_w1s[s_], s_, key, "w1", lambda v: v[:, :, 0:ncols],
                        lambda: k.dma("pool", w1s[s_][:, :, 0:ncols],
                                      w2d[:, c0:c0 + ncols].rearrange("(b p) c -> p b c", p=128),
                                      writes=[b_w1s[s_]], chan=f"w1_{s_}"))
            return s_

        def proj_fm_items(w2d, c0, ncols, evac):
            box = {}
            items = []
            nb = (ncols + 127) // 128
            for fb in range(nb):
                def item(fb=fb):
                    if fb == 0:
                        box["s"] = wchunk(w2d, c0, ncols)
                    s_ = box["s"]
                    m = min(128, ncols - fb * 128)
                    bi = next_bank()
                    for db in range(NDB):
                        k.op("pe", lambda e, db=db: e.matmul(
                            ps[bi][0:m, :], lhsT=w1s[s_][:, db, fb * 128:fb * 128 + m],
                            rhs=hnT[:, db, :], start=(db == 0), stop=(db == NDB - 1)),
                            reads=[b_w1s[s_]] + b_hnT, writes=[psb[bi]])
                    evac(fb, bi)
                items.append(item)
            return items

        def proj_fm(w2d, c0, ncols, evac):
            for it in proj_fm_items(w2d, c0, ncols, evac):
                it()

        def proj_tm_items(w2d, c0, evac):
            box = {}
            items = []
            for j in range(4):
                def item(j=j):
                    if j == 0:
                        box["s"] = wchunk(w2d, c0, 256)
                    s_ = box["s"]
                    bi = next_bank()
                    for db in range(NDB):
                        k.op("pe", lambda e, db=db: e.matmul(
                            ps[bi][:, 0:256], lhsT=hnT[:, db, j * 128:(j + 1) * 128],
                            rhs=w1s[s_][:, db, 0:256], start=(db == 0), stop=(db == NDB - 1)),
                            reads=[b_w1s[s_], b_hnT[j]], writes=[psb[bi]])
                    evac(j, bi)
                items.append(item)
            return items

        def proj_tm(w2d, c0, evac):
            for it in proj_tm_items(w2d, c0, evac):
                it()

        def even_consts(i):
            k.dma("pool", W2ext[0:16, :], b_gate_w2[i], writes=[b_lc], chan="lc")
            k.dma("pool", W2ext[16:17, :], b_gate_b[i], writes=[b_lc], chan="lc")
            k.dma("sp", lnB[:], a_v_ln_g[i].partition_broadcast(128), writes=[b_lc], chan="lcs")
            k.dma("sp", ongB[:], b_out_norm_g[i].partition_broadcast(128), writes=[b_lc], chan="lcs")
            k.dma("pool", bs_row[:], a_b_s[i], writes=[b_lc], chan="lc")
            for half in range(2):
                k.dma("sp", rtmp[half][:].rearrange("p (g s) -> p g s", g=4),
                      a_w_s[i, half * 4:(half + 1) * 4].rearrange("g t s -> t g s"),
                      writes=[b_rtmp[half]], chan=f"rt{half}")
                bi = next_bank()
                for gq in range(4):
                    k.op("pe", lambda e, half=half, gq=gq, bi=bi: e.transpose(
                        out=ps[bi][:, gq * 128:(gq + 1) * 128],
                        in_=rtmp[half][:, gq * 128:(gq + 1) * 128], identity=identf[:]),
                        reads=[b_rtmp[half], b_ident], writes=[psb[bi]])
                r_ = tm_tmp()
                k.op("act", lambda e, r_=r_, bi=bi: e.copy(out=tmpf[r_][:], in_=ps[bi][:, :]),
                     reads=[psb[bi]], writes=[b_tmpf[r_]])
                v3 = tmpf[r_][:].rearrange("p (g t) -> p g t", g=4)
                k.op("pool", lambda e, v3=v3: e.affine_select(
                    out=v3, in_=v3, pattern=[[0, 4], [1, 128]], compare_op=ALU.is_ge,
                    fill=0.0, base=0, channel_multiplier=-1),
                    reads=[b_tmpf[r_]], writes=[b_tmpf[r_]])
                k.op("dve", lambda e, v3=v3, half=half: e.tensor_copy(
                    out=WT[:, half * 4:(half + 1) * 4, :], in_=v3),
                    reads=[b_tmpf[r_]], writes=[b_lc])
            k.op("pool", lambda e: e.memset(Sst[:], 0.0), writes=b_S)
            k.op("pool", lambda e: e.memset(Sbf[:], 0.0), writes=b_Sbf)

        def even_mixer(i):
            w_in = ab_w_in[i]
            w_out = ab_w_out[i]
            alias_fence(b_actT, mix_bufs)
            def ev_glr(fb, bi):
                k.op("act", lambda e, bi=bi: e.copy(out=glr_ext[0:16, :], in_=ps[bi][0:16, :]),
                     reads=[psb[bi]], writes=[b_glr])
            proj_fm(w_in, 5120, 16, ev_glr)
            cumb = [reserve_bank() for _ in range(4)]
            for j in range(4):
                bg = next_bank()
                k.op("pe", lambda e, j=j, bg=bg: e.matmul(
                    ps[bg][:, :], lhsT=glr_ext[0:17, j * 128:(j + 1) * 128], rhs=W2ext[0:17, :],
                    start=True, stop=True), reads=[b_glr, b_lc], writes=[psb[bg]])
                sj = j % 2
                k.op("act", lambda e, sj=sj, bg=bg: e.activation(
                    out=spt[sj][:], in_=ps[bg][:, :], func=AF.Exp, scale=-1.0),
                    reads=[psb[bg]], writes=[b_spt[sj]])
                k.op("act", lambda e, sj=sj: e.activation(
                    out=spt[sj][:], in_=spt[sj][:], func=AF.Ln, bias=onec[:, 0:1], scale=1.0),
                    reads=[b_spt[sj], b_ident], writes=[b_spt[sj]])
                for hh in range(4):
                    k.op("pe", lambda e, sj=sj, hh=hh, j=j: e.matmul(
                        ps[cumb[hh]][:, j * 128:(j + 1) * 128],
                        lhsT=spt[sj][:, hh * 128:(hh + 1) * 128], rhs=triNeg[:, :],
                        start=True, stop=True),
                        reads=[b_spt[sj], b_ident], writes=[psb[cumb[hh]]])
            for hh in range(4):
                cb = cumb[hh]
                k.op("act", lambda e, hh=hh, cb=cb: e.activation(
                    out=E1[:, hh, :], in_=ps[cb][:, :], func=AF.Exp, bias=lnsc[:, 0:1], scale=1.0),
                    reads=[psb[cb], b_ident], writes=[b_E[hh]])
                k.op("act", lambda e, hh=hh, cb=cb: e.activation(
                    out=E2[:, hh, :], in_=ps[cb][:, :], func=AF.Exp, scale=-1.0),
                    reads=[psb[cb]], writes=[b_E[hh]])
                k.op("act", lambda e, hh=hh, cb=cb: e.activation(
                    out=explast[:, hh, :], in_=ps[cb][:, 127:512:128], func=AF.Exp),
                    reads=[psb[cb]], writes=[b_E[hh]])
                reserved.discard(cb)
            for c in range(2):
                def ev_q(fb, bi, c=c):
                    hh = c * 2 + fb
                    k.op("dve", lambda e, hh=hh, bi=bi: e.tensor_tensor(
                        out=qe[:, hh, :], in0=ps[bi][:, :], in1=E1[:, hh, :], op=ALU.mult),
                        reads=[psb[bi], b_E[hh]], writes=[b_qe[hh]])
                proj_fm(w_in, 2048 + c * 256, 256, ev_q)
            for c in range(2):
                def ev_k(fb, bi, c=c):
                    hh = c * 2 + fb
                    k.op("dve", lambda e, hh=hh, bi=bi: e.tensor_tensor(
                        out=ke[:, hh, :], in0=ps[bi][:, :], in1=E2[:, hh, :], op=ALU.mult),
                        reads=[psb[bi], b_E[hh]], writes=[b_ke[hh]])
                    for j in range(4):
                        k.op("dve", lambda e, hh=hh, bi=bi, j=j: e.scalar_tensor_tensor(
                            out=ks[:, hh, j * 128:(j + 1) * 128], in0=ps[bi][:, j * 128:(j + 1) * 128],
                            scalar=explast[:, hh, j:j + 1], in1=E2[:, hh, j * 128:(j + 1) * 128],
                            op0=ALU.mult, op1=ALU.mult),
                            reads=[psb[bi], b_E[hh]], writes=[b_ks[hh]])
                    bt = next_bank()
                    pt = ps[bt].bitcast(BF16)
                    for j in range(4):
                        k.op("pe", lambda e, hh=hh, j=j, pt=pt: e.transpose(
                            out=pt[:, j * 128:(j + 1) * 128], in_=ks[:, hh, j * 128:(j + 1) * 128],
                            identity=ident[:]), reads=[b_ks[hh], b_ident], writes=[psb[bt]])
                    k.op("act", lambda e, hh=hh, pt=pt: e.copy(
                        out=ksT[:, hh, :, :], in_=pt[:, 0:512].rearrange("p (j d) -> p j d", j=4)),
                        reads=[psb[bt]], writes=[b_ksT[hh]])
                proj_fm(w_in, 2560 + c * 256, 256, ev_k)
            for c in range(4):
                def ev_u(fb, bi, c=c):
                    g = c * 2 + fb
                    k.op("act", lambda e, g=g, bi=bi: e.activation(
                        out=a_uT[:, g, :], in_=ps[bi][:, :], func=AF.Gelu),
                        reads=[psb[bi]], writes=[b_auT[g]])
                proj_fm(w_in, c * 256, 256, ev_u)
            for c in range(4):
                def ln_finish(c=c):
                    for h2 in range(2):
                        k.op("dve", lambda e, h2=h2: e.tensor_tensor(
                            out=spt[h2][:], in0=tmpf[h2][:], in1=tmpf[h2][:], op=ALU.mult),
                            reads=[b_tmpf[h2]], writes=[b_spt[h2]])
                        k.op("dve", lambda e, h2=h2: e.tensor_reduce(
                            out=st3[:, h2 * 4:(h2 + 1) * 4],
                            in_=tmpf[h2][:].rearrange("p (a c) -> p a c", a=4), axis=AX.X, op=ALU.add),
                            reads=[b_tmpf[h2]], writes=[b_st3])
                        k.op("dve", lambda e, h2=h2: e.tensor_reduce(
                            out=st3[:, 8 + h2 * 4:8 + (h2 + 1) * 4],
                            in_=spt[h2][:].rearrange("p (a c) -> p a c", a=4), axis=AX.X, op=ALU.add),
                            reads=[b_spt[h2]], writes=[b_st3])
                    k.op("dve", lambda e: e.tensor_scalar(
                        out=st3[:, 16:24], in0=st3[:, 0:8], scalar1=1.0 / 128, scalar2=None,
                        op0=ALU.mult), reads=[b_st3], writes=[b_st3])
                    k.op("dve", lambda e: e.tensor_tensor(
                        out=st3[:, 24:32], in0=st3[:, 16:24], in1=st3[:, 16:24], op=ALU.mult),
                        reads=[b_st3], writes=[b_st3])
                    k.op("dve", lambda e: e.scalar_tensor_tensor(
                        out=st3[:, 32:40], in0=st3[:, 8:16], scalar=1.0 / 128, in1=st3[:, 24:32],
                        op0=ALU.mult, op1=ALU.subtract), reads=[b_st3], writes=[b_st3])
                    k.op("act", lambda e: e.activation(
                        out=st3[:, 40:48], in_=st3[:, 32:40], func=AF.Ln, bias=epsc[:, 0:1],
                        scale=1.0), reads=[b_st3, b_ident], writes=[b_st3])
                    k.op("act", lambda e: e.activation(
                        out=st3[:, 48:56], in_=st3[:, 40:48], func=AF.Exp, scale=-0.5),
                        reads=[b_st3], writes=[b_st3])
                    for q in range(8):
                        j, gl = q // 2, q % 2
                        g = c * 2 + gl
                        src = tmpf[j // 2][:, (j % 2) * 256 + gl * 128:(j % 2) * 256 + (gl + 1) * 128]
                        k.op("dve", lambda e, src=src, q=q: e.tensor_scalar(
                            out=src, in0=src, scalar1=st3[:, 16 + q:17 + q], scalar2=st3[:, 48 + q:49 + q],
                            op0=ALU.subtract, op1=ALU.mult),
                            reads=[b_tmpf[j // 2], b_st3], writes=[b_tmpf[j // 2]])
                        k.op("dve", lambda e, src=src, g=g, j=j: e.tensor_tensor(
                            out=v_sgu[:, j, g * 128:(g + 1) * 128], in0=src,
                            in1=lnB[:, g * 128:(g + 1) * 128], op=ALU.mult),
                            reads=[b_tmpf[j // 2], b_lc], writes=[b_vsgu[j]])

                def ev_v(j, bi, c=c, ln_finish=ln_finish):
                    k.op("act", lambda e: e.activation(
                        out=tmpf[j // 2][:, (j % 2) * 256:(j % 2 + 1) * 256], in_=ps[bi][:, 0:256],
                        func=AF.Gelu), reads=[psb[bi]], writes=[b_tmpf[j // 2]])
                    if j == 3:
                        ln_finish()
                proj_tm(w_in, 1024 + c * 256, ev_v)
            for c in range(4):
                def ev_vg(j, bi, c=c):
                    k.op("act", lambda e, j=j, bi=bi, c=c: e.copy(
                        out=v_gla[:, j, c * 256:(c + 1) * 256], in_=ps[bi][:, 0:256]),
                        reads=[psb[bi]], writes=[b_vgla[j]])
                proj_tm(w_in, 3072 + c * 256, ev_vg)
            for c in range(4):
                def ev_r(j, bi, c=c):
                    k.op("act", lambda e, j=j, bi=bi, c=c: e.activation(
                        out=r_s[:, j, c * 256:(c + 1) * 256], in_=ps[bi][:, 0:256], func=AF.Silu),
                        reads=[psb[bi]], writes=[b_rs[j]])
                proj_tm(w_in, 4096 + c * 256, ev_r)
            for g in range(8):
                bi = next_bank()
                for j in range(4):
                    k.op("pe", lambda e, g=g, j=j, bi=bi: e.matmul(
                        ps[bi][:, j * 128:(j + 1) * 128], lhsT=v_sgu[:, j, g * 128:(g + 1) * 128],
                        rhs=WT[:, g, :], start=True, stop=False),
                        reads=[b_vsgu[j], b_lc], writes=[psb[bi]])
                    k.op("pe", lambda e, g=g, j=j, bi=bi: e.matmul(
                        ps[bi][:, j * 128:(j + 1) * 128], lhsT=ones_row[0:1, :],
                        rhs=bs_row[0:1, g * 128:(g + 1) * 128], start=False, stop=True),
                        reads=[b_ident, b_lc], writes=[psb[bi]])
                k.op("dve", lambda e, g=g, bi=bi: e.tensor_tensor(
                    out=catT[:, g, :], in0=ps[bi][:, :], in1=a_uT[:, g, :], op=ALU.mult),
                    reads=[psb[bi], b_auT[g]], writes=[b_catT[g]])
            for j in range(4):
                jc = slice(j * 128, (j + 1) * 128)
                bs_ = next_bank()
                for hh in range(4):
                    k.op("pe", lambda e, hh=hh, jc=jc, bs_=bs_: e.matmul(
                        ps[bs_][:, hh * 128:(hh + 1) * 128], lhsT=ke[:, hh, jc], rhs=qe[:, hh, jc],
                        start=True, stop=True), reads=[b_ke[hh], b_qe[hh]], writes=[psb[bs_]])
                k.op("dve", lambda e, bs_=bs_: e.tensor_tensor(
                    out=scTm[:, :, :].rearrange("p h t -> p (h t)"), in0=ps[bs_][:, :],
                    in1=maskT[:, :, :].rearrange("p h t -> p (h t)"), op=ALU.mult),
                    reads=[psb[bs_], b_ident], writes=[b_scTm])
                for hp in range(2):
                    bo = next_bank()
                    for hq in range(2):
                        hh = hp * 2 + hq
                        oc = slice(hq * 256, (hq + 1) * 256)
                        vc = slice(hh * 256, (hh + 1) * 256)
                        k.op("pe", lambda e, hh=hh, oc=oc, vc=vc, bo=bo, j=j: e.matmul(
                            ps[bo][:, oc], lhsT=scTm[:, hh, :], rhs=v_gla[:, j, vc],
                            start=True, stop=False),
                            reads=[b_scTm, b_vgla[j]], writes=[psb[bo]])
                        k.op("pe", lambda e, hh=hh, oc=oc, bo=bo, jc=jc: e.matmul(
                            ps[bo][:, oc], lhsT=qe[:, hh, jc], rhs=Sbf[:, hh, :],
                            start=False, stop=True),
                            reads=[b_qe[hh], b_Sbf[hh]], writes=[psb[bo]])
                    for hq in range(2):
                        hh = hp * 2 + hq
                        oc = slice(hq * 256, (hq + 1) * 256)
                        vc = slice(hh * 256, (hh + 1) * 256)
                        k.op("act", lambda e, oc=oc, bo=bo: e.activation(
                            out=junk[:, 0:256], in_=ps[bo][:, oc], func=AF.Square,
                            accum_out=st2[:, 8:9]), reads=[psb[bo]], writes=[b_junk, b_st2])
                        k.op("act", lambda e: e.activation(
                            out=st2[:, 9:10], in_=st2[:, 8:9], func=AF.Ln, bias=epsc[:, 0:1],
                            scale=1.0 / 256), reads=[b_st2, b_ident], writes=[b_st2])
                        k.op("act", lambda e: e.activation(
                            out=st2[:, 10:11], in_=st2[:, 9:10], func=AF.Exp, scale=-0.5),
                            reads=[b_st2], writes=[b_st2])
                        r_ = tm_tmp()
                        k.op("dve", lambda e, r_=r_, oc=oc, bo=bo: e.scalar_tensor_tensor(
                            out=tmpf[r_][:, 0:256], in0=ps[bo][:, oc], scalar=st2[:, 10:11],
                            in1=ongB[:, :], op0=ALU.mult, op1=ALU.mult),
                            reads=[psb[bo], b_st2, b_lc], writes=[b_tmpf[r_]])
                        k.op("dve", lambda e, r_=r_, vc=vc, j=j: e.tensor_tensor(
                            out=bout[:, vc], in0=tmpf[r_][:, 0:256], in1=r_s[:, j, vc], op=ALU.mult),
                            reads=[b_tmpf[r_], b_rs[j]], writes=[b_bout])
                for hp in range(2):
                    bk = next_bank()
                    for hq in range(2):
                        hh = hp * 2 + hq
                        oc = slice(hq * 256, (hq + 1) * 256)
                        vc = slice(hh * 256, (hh + 1) * 256)
                        k.op("pe", lambda e, hh=hh, oc=oc, vc=vc, bk=bk, j=j: e.matmul(
                            ps[bk][:, oc], lhsT=ksT[:, hh, j, :], rhs=v_gla[:, j, vc],
                            start=True, stop=True),
                            reads=[b_ksT[hh], b_vgla[j]], writes=[psb[bk]])
                        k.op("dve", lambda e, hh=hh, oc=oc, bk=bk, j=j: e.scalar_tensor_tensor(
                            out=Sst[:, hh, :], in0=Sst[:, hh, :], scalar=explast[:, hh, j:j + 1],
                            in1=ps[bk][:, oc], op0=ALU.mult, op1=ALU.add),
                            reads=[psb[bk], b_S[hh], b_E[hh]], writes=[b_S[hh]])
                        k.op("act", lambda e, hh=hh: e.copy(out=Sbf[:, hh, :], in_=Sst[:, hh, :]),
                             reads=[b_S[hh]], writes=[b_Sbf[hh]])
                bt = next_bank()
                pt = ps[bt].bitcast(BF16)
                for q8 in range(8):
                    k.op("pe", lambda e, q8=q8, pt=pt: e.transpose(
                        out=pt[:, q8 * 128:(q8 + 1) * 128], in_=bout[:, q8 * 128:(q8 + 1) * 128],
                        identity=ident[:]), reads=[b_bout, b_ident], writes=[psb[bt]])
                k.op("act", lambda e, pt=pt, jc=jc: e.copy(
                    out=catT[:, 8:16, jc], in_=pt[:, :].rearrange("p (q t) -> p q t", q=8)),
                    reads=[psb[bt]], writes=b_catT[8:16])
            for cq in range(8):
                s_ = wchunk(w_out, cq * 256, 256, key=("out", cq))
                for j in range(4):
                    bi = next_bank()
                    for cb in range(16):
                        k.op("pe", lambda e, s_=s_, j=j, cb=cb, bi=bi: e.matmul(
                            ps[bi][:, 0:256], lhsT=catT[:, cb, j * 128:(j + 1) * 128],
                            rhs=w1s[s_][:, cb, 0:256], start=(cb == 0), stop=(cb == 15)),
                            reads=[b_w1s[s_], b_catT[cb]], writes=[psb[bi]])
                    k.op("dve", lambda e, j=j, bi=bi, cq=cq: e.tensor_tensor(
                        out=hs[:, j, cq * 256:(cq + 1) * 256], in0=ps[bi][:, 0:256],
                        in1=hs[:, j, cq * 256:(cq + 1) * 256], op=ALU.add),
                        reads=[psb[bi], b_hs[j]], writes=[b_hs[j]])
            alias_fence(mix_bufs, b_actT)


        qT = carve(0, 8192, "p (h t) -> p h t", h=16)
        iqT = carve(8192, 4096, "p (b t) -> p b t", b=8)
        selT = carve(12288, 16384, "p (k t) -> p k t", k=32)
        vn = carve(28672, 2048, "p (j c) -> p j c", j=4)
        kTn = carve(30720, 2048, "p (g t) -> p g t", g=4)
        b_qT = [Buf() for _ in range(16)]
        b_iqT = [Buf() for _ in range(8)]
        b_selT = [Buf() for _ in range(4)]
        b_vn = [Buf() for _ in range(4)]
        b_kTn = [Buf() for _ in range(4)]
        odd_bufs = b_qT + b_iqT + b_selT + b_vn + b_kTn
        iw_t = sb("iw_t", [128, 4, 16], F32)
        b_iw = [Buf() for _ in range(4)]
        thr = sb("thr", [128, 16], F32)
        b_thr = Buf()
        halfc = sb("halfc", [128, 1], F32)
        negtri = sb("negtri", [128, 128], F32)
        rb = sb("rb", [32, 16], F32)
        OHs = sb("OHs", [32, 384], F32)
        cfarB = sb("cfarB", [128, 16], F32)
        ones128 = sb("ones128", [128, 128], BF16)
        fsb = sb("fsb", [16, 384], F32)
        selq = sb("selq", [128, 512], BF16)
        b_selq = Buf()
        b_oc = Buf()
        b_kvd = [Buf() for _ in range(NST)]
        b_fpad = Buf()
        has_odd = any(l % 2 == 1 for l in layers) and "mix" in parts
        if has_odd:
            k.op("pool", lambda e: e.memset(halfc[:], 0.5), writes=[b_oc])
            k.op("pool", lambda e: e.memset(ones128[:], 1.0), writes=[b_oc])
            k.op("pool", lambda e: e.memset(negtri[:], 0.0), writes=[b_oc])
            k.op("pool", lambda e: e.affine_select(
                out=negtri[:], in_=negtri[:], pattern=[[-1, 128]], compare_op=ALU.is_ge,
                fill=-1.0e30, base=0, channel_multiplier=1), reads=[b_oc], writes=[b_oc])
            k.dma("sp", rb[:], rel_bias, writes=[b_oc], chan="oc")
            k.dma("sp", OHs[:], oh_pad, writes=[b_oc], chan="oc")
            k.dma("sp", cfarB[:], rel_bias[31:32, :].partition_broadcast(128), writes=[b_oc], chan="oc")
            bi = next_bank()
            k.op("pe", lambda e, bi=bi: e.matmul(ps[bi][0:16, 0:384], lhsT=rb[:, :], rhs=OHs[:, :],
                                                 start=True, stop=True),
                 reads=[b_oc], writes=[psb[bi]])
            k.op("act", lambda e, bi=bi: e.copy(out=fsb[:], in_=ps[bi][0:16, 0:384]),
                 reads=[psb[bi]], writes=[b_oc])
            k.dma("sp", fpad_d, fsb[:], reads=[b_oc], writes=[b_fpad], chan="oc2")

        def odd_consts(i):
            alias_fence(even2_bufs + [b_lc], odd2_bufs)
            k.op("pool", lambda e: e.memset(cf[:], 0.0), writes=[b_ident])
            k.op("pool", lambda e: e.affine_select(
                out=cf[:], in_=cf[:], pattern=[[1, 128]], compare_op=ALU.not_equal,
                fill=1.0, base=-127, channel_multiplier=1), reads=[b_ident], writes=[b_ident])
            for hd in range(16):
                r_ = hd % 2
                src = bass.AP(tensor=fpad_d.tensor, offset=fpad_d[hd:hd + 1, :].offset,
                              ap=[[1, 128], [1, 256]])
                k.dma("sp", rtmp[r_][:, 0:256], src, reads=[b_fpad], writes=[b_rtmp[r_]],
                      chan=f"rt{r_}")
                bi = next_bank()
                k.op("pe", lambda e, r_=r_, bi=bi: e.matmul(
                    ps[bi][:, 0:256], lhsT=cf[:, :], rhs=rtmp[r_][:, 0:256], start=True, stop=True),
                    reads=[b_rtmp[r_], b_ident], writes=[psb[bi]])
                k.op("dve", lambda e, hd=hd, bi=bi: e.tensor_scalar(
                    out=BTn[:, hd, :], in0=ps[bi][:, 0:256], scalar1=cfarB[:, hd:hd + 1],
                    scalar2=None, op0=ALU.subtract),
                    reads=[psb[bi], b_oc], writes=[b_BTn])

        def odd_mixer(i, st):
            w_in = c_w_in[i]
            w_out = c_w_out[i]
            t0 = st * TS
            alias_fence(b_actT, odd_bufs)
            work = []
            for c in range(8):
                def ev_q(fb, bi, c=c):
                    hd = c * 2 + fb
                    k.op("act", lambda e, hd=hd, bi=bi: e.activation(
                        out=qT[:, hd, :], in_=ps[bi][:, :], func=AF.Copy, scale=128.0 ** -0.5),
                        reads=[psb[bi]], writes=[b_qT[hd]])
                work += proj_fm_items(w_in, c * 256, 256, ev_q)
            for c in range(2):
                def ev_k(fb, bi, c=c):
                    g = c * 2 + fb
                    k.op("act", lambda e, g=g, bi=bi: e.copy(out=kTn[:, g, :], in_=ps[bi][:, :]),
                         reads=[psb[bi]], writes=[b_kTn[g]])
                    k.dma("sp", kT_d[g, :, t0:t0 + TS], kTn[:, g, :], reads=[b_kTn[g]],
                          writes=[b_kvd[st]], chan=f"kvw{g}")
                work += proj_fm_items(w_in, 2048 + c * 256, 256, ev_k)
            for c in range(4):
                def ev_iq(fb, bi, c=c):
                    blk = c * 2 + fb
                    k.op("act", lambda e, blk=blk, bi=bi: e.copy(out=iqT[:, blk, :], in_=ps[bi][:, :]),
                         reads=[psb[bi]], writes=[b_iqT[blk]])
                proj_fm(w_in, 3072 + c * 256, 256, ev_iq)
            s_ = w1_rr[0]
            w1_rr[0] = (s_ + 1) % NW1
            def ik_loads(s_=s_):
                for dup in range(2):
                    k.dma("pool", w1s[s_][:, :, dup * 64:(dup + 1) * 64],
                          w_in[:, 4096:4160].rearrange("(b p) c -> p b c", p=128),
                          writes=[b_w1s[s_]], chan=f"w1_{s_}")
            cached_load(w1s[s_], b_w1s[s_], s_, ("ik",), "w1", lambda v: v[:, :, 0:128], ik_loads)
            bi = next_bank()
            for db in range(NDB):
                k.op("pe", lambda e, s_=s_, db=db, bi=bi: e.matmul(
                    ps[bi][:, :], lhsT=w1s[s_][:, db, 0:128], rhs=hnT[:, db, :],
                    start=(db == 0), stop=(db == NDB - 1)),
                    reads=[b_w1s[s_]] + b_hnT, writes=[psb[bi]])
            k.op("act", lambda e, bi=bi: e.copy(out=ikT[:, t0:t0 + TS], in_=ps[bi][:, :]),
                 reads=[psb[bi]], writes=[b_ikT[st]])
            for c in range(2):
                def ev_v(j, bi, c=c):
                    k.op("act", lambda e, j=j, bi=bi, c=c: e.copy(
                        out=vn[:, j, c * 256:(c + 1) * 256], in_=ps[bi][:, 0:256]),
                        reads=[psb[bi]], writes=[b_vn[j]])
                    if c == 1:
                        k.dma("sp", v_d[t0 + j * 128:t0 + (j + 1) * 128, :], vn[:, j, :],
                              reads=[b_vn[j]], writes=[b_kvd[st]], chan=f"kvw{j}")
                work += proj_tm_items(w_in, 2560 + c * 256, ev_v)
            s_ = wchunk(w_in, 4160, 16)
            for j in range(4):
                bi = next_bank()
                for db in range(NDB):
                    k.op("pe", lambda e, s_=s_, j=j, db=db, bi=bi: e.matmul(
                        ps[bi][:, 0:16], lhsT=hnT[:, db, j * 128:(j + 1) * 128],
                        rhs=w1s[s_][:, db, 0:16], start=(db == 0), stop=(db == NDB - 1)),
                        reads=[b_w1s[s_], b_hnT[j]], writes=[psb[bi]])
                k.op("act", lambda e, j=j, bi=bi: e.copy(out=iw_t[:, j, :], in_=ps[bi][:, 0:16]),
                     reads=[psb[bi]], writes=[b_iw[j]])
            for j in range(4):
                qb = st * 4 + j
                nk = (qb + 1) * 128
                nch = (nk + 511) // 512
                for h in range(16):
                    blk, po = h // 2, (h % 2) * 64
                    for c in range(nch):
                        ncol = min(512, nk - c * 512)
                        bi = next_bank()
                        k.op("pe", lambda e, blk=blk, po=po, j=j, c=c, ncol=ncol, bi=bi: e.matmul(
                            ps[bi][:, 0:ncol], lhsT=iqT[po:po + 64, blk, j * 128:(j + 1) * 128],
                            rhs=ikT[po:po + 64, c * 512:c * 512 + ncol], start=True, stop=True),
                            reads=[b_iqT[blk], b_ikT[c]], writes=[psb[bi]])
                        r_ = (h * nch + c) % 2
                        k.op("act", lambda e, r_=r_, ncol=ncol, bi=bi: e.activation(
                            out=rtmp[r_][:, 0:ncol], in_=ps[bi][:, 0:ncol], func=AF.Relu),
                            reads=[psb[bi]], writes=[b_rtmp[r_]])
                        cs = slice(c * 512, c * 512 + ncol)
                        if h == 0:
                            k.op("dve", lambda e, r_=r_, ncol=ncol, cs=cs, j=j: e.tensor_scalar(
                                out=Iacc[:, cs], in0=rtmp[r_][:, 0:ncol], scalar1=iw_t[:, j, 0:1],
                                scalar2=None, op0=ALU.mult),
                                reads=[b_rtmp[r_], b_iw[j]], writes=[b_I])
                        else:
                            k.op("dve", lambda e, r_=r_, ncol=ncol, cs=cs, j=j, h=h: e.scalar_tensor_tensor(
                                out=Iacc[:, cs], in0=rtmp[r_][:, 0:ncol], scalar=iw_t[:, j, h:h + 1],
                                in1=Iacc[:, cs], op0=ALU.mult, op1=ALU.add),
                                reads=[b_rtmp[r_], b_iw[j], b_I], writes=[b_I])
                        if work and (h % 2 == 1):
                            work.pop(0)()
                k.op("dve", lambda e, nk=nk: e.tensor_reduce(out=thr[:, 1:2], in_=Iacc[:, 0:nk],
                                                            axis=AX.X, op=ALU.max),
                     reads=[b_I], writes=[b_thr])
                k.op("dve", lambda e, nk=nk: e.tensor_reduce(out=thr[:, 0:1], in_=Iacc[:, 0:nk],
                                                            axis=AX.X, op=ALU.min),
                     reads=[b_I], writes=[b_thr])
                k.op("dve", lambda e, qb=qb: e.tensor_tensor(
                    out=Iacc[:, qb * 128:(qb + 1) * 128], in0=Iacc[:, qb * 128:(qb + 1) * 128],
                    in1=negtri[:, :], op=ALU.add), reads=[b_I, b_oc], writes=[b_I])
                if nk > 256:
                    k.op("dve", lambda e: e.tensor_tensor(
                        out=thr[:, 6:7], in0=thr[:, 1:2], in1=thr[:, 0:1], op=ALU.subtract),
                        reads=[b_thr], writes=[b_thr])
                    for it in range(20):
                        cit = 0.5 ** (it + 1)
                        k.op("dve", lambda e, cit=cit: e.scalar_tensor_tensor(
                            out=thr[:, 2:3], in0=thr[:, 6:7], scalar=cit, in1=thr[:, 0:1],
                            op0=ALU.mult, op1=ALU.add), reads=[b_thr], writes=[b_thr])
                        n0 = min(nk, 2048)
                        k.op("dve", lambda e, n0=n0: e.tensor_scalar(
                            out=hn[:, 0:n0], in0=Iacc[:, 0:n0], scalar1=thr[:, 2:3], scalar2=None,
                            op0=ALU.is_ge, op1=ALU.add, accum_out=thr[:, 3:4]),
                            reads=[b_I, b_thr], writes=[b_hn, b_thr])
                        if nk > 2048:
                            k.op("dve", lambda e, nk=nk: e.tensor_scalar(
                                out=hn[:, 0:nk - 2048], in0=Iacc[:, 2048:nk], scalar1=thr[:, 2:3],
                                scalar2=None, op0=ALU.is_ge, op1=ALU.add, accum_out=thr[:, 4:5]),
                                reads=[b_I, b_thr], writes=[b_hn, b_thr])
                            k.op("dve", lambda e: e.tensor_tensor(
                                out=thr[:, 3:4], in0=thr[:, 3:4], in1=thr[:, 4:5], op=ALU.add),
                                reads=[b_thr], writes=[b_thr])
                        k.op("dve", lambda e, cit=cit: e.tensor_scalar(
                            out=thr[:, 5:6], in0=thr[:, 3:4], scalar1=255.5, scalar2=cit,
                            op0=ALU.is_ge, op1=ALU.mult), reads=[b_thr], writes=[b_thr])
                        k.op("dve", lambda e: e.scalar_tensor_tensor(
                            out=thr[:, 0:1], in0=thr[:, 5:6], scalar=thr[:, 6:7], in1=thr[:, 0:1],
                            op0=ALU.mult, op1=ALU.add), reads=[b_thr], writes=[b_thr])
                for c in range(nch):
                    ncol = min(512, nk - c * 512)
                    nb = ncol // 128
                    k.op("dve", lambda e, c=c, ncol=ncol: e.tensor_scalar(
                        out=selq[:, 0:ncol], in0=Iacc[:, c * 512:c * 512 + ncol], scalar1=thr[:, 0:1],
                        scalar2=None, op0=ALU.is_ge), reads=[b_I, b_thr], writes=[b_selq])
                    bt = next_bank()
                    pt = ps[bt].bitcast(BF16)
                    for q in range(nb):
                        k.op("pe", lambda e, q=q, pt=pt: e.transpose(
                            out=pt[:, q * 128:(q + 1) * 128], in_=selq[:, q * 128:(q + 1) * 128],
                            identity=ident[:]), reads=[b_selq, b_ident], writes=[psb[bt]])
                    k.op("act", lambda e, c=c, nb=nb, pt=pt, j=j: e.copy(
                        out=selT[:, c * 4:c * 4 + nb, j * 128:(j + 1) * 128],
                        in_=pt[:, 0:nb * 128].rearrange("p (q t) -> p q t", q=nb)),
                        reads=[psb[bt]], writes=[b_selT[j]])
            while work:
                work.pop(0)()
            steps = []
            for hd in range(16):
                for c in range(st + 1):
                    for kq in range(4):
                        steps.append((hd, c, kq))
            nkb = st * 4 + 4
            nst_ = len(steps)
            LA = 2
            slot_of = {}
            bank_of = {}
            obank = {}

            def emit_load(hd, c):
                g = hd // 4
                if (g, c, hd) in slot_of:
                    return
                s_ = w2_rr[0]
                w2_rr[0] = (s_ + 1) % NW2
                kslot = w2s[s_][:, 0:1, :].rearrange("p a c -> p (a c)")
                vslot = w2s[s_][:, 1:2, :].rearrange("p a (q v) -> p (a q) v", q=4)
                k.dma("sp", kslot, kT_d[g, :, c * 512:(c + 1) * 512], reads=[b_kvd[c]],
                      writes=[b_w2s[s_]], chan=f"kv{s_}")
                k.dma("sp", vslot, v_d[c * 512:(c + 1) * 512, g * 128:(g + 1) * 128].rearrange(
                    "(q p) v -> p q v", p=128), reads=[b_kvd[c]], writes=[b_w2s[s_]], chan=f"kv{s_}")
                slot_of[(g, c, hd)] = (s_, kslot, vslot)

            def geom(c, kq):
                kb = c * 4 + kq
                col0 = kq * 128 if c == st else 0
                return kb, col0, 512 - col0

            def emit_qk(i):
                hd, c, kq = steps[i]
                g = hd // 4
                emit_load(hd, c)
                if kq == 0:
                    if c < st:
                        emit_load(hd, c + 1)
                    elif hd < 15:
                        emit_load(hd + 1, 0)
                s_, kslot, vslot = slot_of[(g, c, hd)]
                kb, col0, ncols = geom(c, kq)
                near = kb >= 4 * st - 1
                bi = next_bank()
                bank_of[i] = bi
                k.op("pe", lambda e: e.matmul(
                    ps[bi][:, 0:ncols], lhsT=kslot[:, kq * 128:(kq + 1) * 128],
                    rhs=qT[:, hd, col0:512], start=True, stop=(not near)),
                    reads=[b_w2s[s_], b_qT[hd]], writes=[psb[bi]])
                if near:
                    if c == st:
                        off, nbc = 0, min(256, ncols)
                    else:
                        off, nbc = 128, 128
                    k.op("pe", lambda e: e.matmul(
                        ps[bi][:, 0:nbc], lhsT=ident[:, :], rhs=BTn[:, hd, off:off + nbc],
                        start=False, stop=True),
                        reads=[b_BTn, b_ident], writes=[psb[bi]])

            def emit_softmax(i):
                hd, c, kq = steps[i]
                kb, col0, ncols = geom(c, kq)
                bi = bank_of[i]
                pr = i % 2
                k.op("act", lambda e: e.activation(
                    out=PTb[pr][:, 0:ncols], in_=ps[bi][:, 0:ncols], func=AF.Exp,
                    bias=cfarB[:, hd:hd + 1], scale=1.0),
                    reads=[psb[bi], b_oc], writes=[b_PT[pr]])
                k.op("dve", lambda e: e.tensor_tensor(
                    out=PMb[pr][:, 0:ncols], in0=PTb[pr][:, 0:ncols], in1=selT[:, kb, col0:512],
                    op=ALU.mult), reads=[b_PT[pr]] + b_selT, writes=[b_PM[pr]])

            def emit_pv(i):
                hd, c, kq = steps[i]
                g = hd // 4
                kb, col0, ncols = geom(c, kq)
                s_, kslot, vslot = slot_of[(g, c, hd)]
                pr = i % 2
                if kb == 0:
                    obank[hd] = (reserve_bank(), reserve_bank())
                bo, bl = obank[hd]
                k.op("pe", lambda e: e.matmul(
                    ps[bo][:, col0:512], lhsT=vslot[:, kq, :], rhs=PMb[pr][:, 0:ncols],
                    start=(kb == 0), stop=(kb == nkb - 1)),
                    reads=[b_w2s[s_], b_PM[pr]], writes=[psb[bo]])
                k.op("pe", lambda e: e.matmul(
                    ps[bl][:, col0:512], lhsT=ones128[:, :], rhs=PMb[pr][:, 0:ncols],
                    start=(kb == 0), stop=(kb == nkb - 1)),
                    reads=[b_oc, b_PM[pr]], writes=[psb[bl]])
                if kb == nkb - 1:
                    r_ = hd % 2
                    k.op("dve", lambda e: e.reciprocal(out=rtmp[r_][:, :], in_=ps[bl][:, :]),
                         reads=[psb[bl]], writes=[b_rtmp[r_]])
                    k.op("dve", lambda e: e.tensor_tensor(
                        out=qT[:, hd, :], in0=ps[bo][:, :], in1=rtmp[r_][:, :], op=ALU.mult),
                        reads=[psb[bo], b_rtmp[r_]], writes=[b_qT[hd]])
                    reserved.discard(bo)
                    reserved.discard(bl)

            for i in range(min(LA, nst_)):
                emit_qk(i)
            for i in range(nst_):
                emit_softmax(i)
                if i + LA < nst_:
                    emit_qk(i + LA)
                emit_pv(i)
            for cq in range(8):
                s_ = wchunk(w_out, cq * 256, 256, key=("out", cq))
                for j in range(4):
                    bi = next_bank()
                    for cb in range(16):
                        k.op("pe", lambda e, s_=s_, j=j, cb=cb, bi=bi: e.matmul(
                            ps[bi][:, 0:256], lhsT=qT[:, cb, j * 128:(j + 1) * 128],
                            rhs=w1s[s_][:, cb, 0:256], start=(cb == 0), stop=(cb == 15)),
                            reads=[b_w1s[s_], b_qT[cb]], writes=[psb[bi]])
                    k.op("dve", lambda e, j=j, bi=bi, cq=cq: e.tensor_tensor(
                        out=hs[:, j, cq * 256:(cq + 1) * 256], in0=ps[bi][:, 0:256],
                        in1=hs[:, j, cq * 256:(cq + 1) * 256], op=ALU.add),
                        reads=[psb[bi], b_hs[j]], writes=[b_hs[j]])
            alias_fence(odd_bufs, b_actT)

        first = True
        for li, l in enumerate(layers):
            last = (li == len(layers) - 1)
            if "mix" in parts and l % 2 == 0:
                alias_fence(odd2_bufs, even2_bufs + [b_lc])
                even_consts(l // 2)
            if "mix" in parts and l % 2 == 1:
                odd_consts(l // 2)
            for st in range(NST):
                ctx["l"], ctx["st"] = l, st
                src_h = x if first else hscr
                for j in range(TS // 128):
                    k.dma("sp", hs[:, j, :], src_h[st * TS + j * 128: st * TS + (j + 1) * 128, :],
                          reads=[b_hdram[st]] if not first else [], writes=[b_hs[j]], chan=f"hs{j}")
                if "mix" in parts:
                    load_gain(norm_mix_g[l:l + 1, :])
                    for j in range(TS // 128):
                        norm_tile(j)
                        norm_apply_T(j)
                    if l % 2 == 0:
                        even_mixer(l // 2)
                    else:
                        odd_mixer(l // 2, st)
                if "ffn" in parts:
                    load_gain(norm_ffn_g[l:l + 1, :])
                    for j in range(TS // 128):
                        norm_tile(j)
                        norm_apply_T(j)
                    ffn_supertile(l)
                if last and do_final:
                    load_gain(final_norm_g[0:1, :])
                    for j in range(TS // 128):
                        norm_tile(j)
                        k.op("dve", lambda e, j=j: e.scalar_tensor_tensor(
                            out=hs[:, j, :], in0=hs[:, j, :], scalar=stat[:, 2:3], in1=gB[:],
                            op0=ALU.mult, op1=ALU.mult),
                            reads=[b_hs[j], b_stat, b_gB], writes=[b_hs[j]])
                dst_h = out if last else hscr
                for j in range(TS // 128):
                    k.dma("sp", dst_h[st * TS + j * 128: st * TS + (j + 1) * 128, :], hs[:, j, :],
                          reads=[b_hs[j]], writes=[b_hdram[st]], chan=f"ho{j}")
            first = False
        k.wait_all("sp", b_hdram)

        with nc.Block() as block:
            @block.tensor
            def _(e):
                k.replay("pe", e)

            @block.scalar
            def _(e):
                k.replay("act", e)

            @block.vector
            def _(e):
                k.replay("dve", e)

            @block.gpsimd
            def _(e):
                k.replay("pool", e)

            @block.sync
            def _(e):
                k.replay("sp", e)
    return nc


def _bucket_table():
    d = np.arange(0, 257)
    dd = np.maximum(d, 1).astype(np.float32)
    large = 16 + (np.log(dd / np.float32(16)) / np.float32(math.log(128 / 16))
                  * np.float32(16)).astype(np.int32)
    large = np.minimum(large, 31)
    return np.where(d < 16, d, large)


def _oh_pad():
    bk = _bucket_table()
    oh = np.zeros((32, 384), np.float32)
    for m in range(127, 384):
        oh[bk[m - 127], m] = 1.0
    return oh


def make_in_map(inp, x):
    f = lambda a: np.ascontiguousarray(np.asarray(a, dtype=np.float32))
    return dict(
        x=f(x), norm_mix_g=f(inp["norm_mix_g"]), norm_ffn_g=f(inp["norm_ffn_g"]),
        final_norm_g=f(inp["final_norm_g"]).reshape(1, -1),
        ffn_w1=f(inp["ffn_w1"]), ffn_w2=f(inp["ffn_w2"]),
        ab_w_in=f(inp["ab_w_in"]), a_v_ln_g=f(inp["a_v_ln_g"]).reshape(2, 1, 1024),
        a_w_s=f(inp["a_w_s"]), a_b_s=f(inp["a_b_s"]).reshape(2, 1, 1024),
        b_gate_w2=f(inp["b_gate_w2"]), b_gate_b=f(inp["b_gate_b"]).reshape(2, 1, 512),
        b_out_norm_g=f(inp["b_out_norm_g"]).reshape(2, 1, 256), ab_w_out=f(inp["ab_w_out"]),
        c_w_in=f(inp["c_w_in"]), c_w_out=f(inp["c_w_out"]), rel_bias=f(inp["rel_bias"]),
        oh_pad=_oh_pad())


ACTIVE = (0, 1, 4, 5)


def kernel(**inputs):
    x = np.asarray(inputs["x"], dtype=np.float32)
    nc = build(dict(T=SEQ, layers=[0, 1, 2, 3]))
    real = [make_in_map(inputs, x[b]) for b in range(BATCH)]
    zero = {kk: (v if kk == "oh_pad" else np.zeros_like(v)) for kk, v in real[0].items()}
    in_maps = [zero] * 8
    in_maps = list(in_maps)
    for b, c in enumerate(ACTIVE):
        in_maps[c] = real[b]
    res = run_bass_kernel_spmd(nc, in_maps, core_ids=list(range(8)))
    out = np.stack([np.asarray(res.results[c]["out"]) for c in ACTIVE])
    return out.astype(np.float32)
```
